# Optimizing a Trainium2 kernel written in Bass

```python
import math
import jax, jax.numpy as jnp
from jax import lax
import numpy as np

D_MODEL = 1024
BATCH = 8
SEQ = 4096
DEPTH = 2

GRID_W = 64
CTX_LEN = 256
EPS = 1e-6
ROPE_BASE = 10000.0

ATT_HEADS = 4
ATT_QK = 64
ATT_V = 2 * ATT_QK
ATT_W = ATT_HEADS * ATT_V
ATT_BLOCK = 128

RET_HEADS = 4
RET_QK = 64
RET_V = 64
RET_W = RET_HEADS * RET_V
RET_CHUNK = 128

LRU_W = 256
LRU_BLOCKS = 4
LRU_BW = LRU_W // LRU_BLOCKS
LRU_CONV = 4
LRU_C = 8.0

MIX_W = ATT_W + RET_W + LRU_W
IN_SPLITS = (ATT_HEADS * 2 * ATT_QK, ATT_HEADS * 2 * ATT_QK, ATT_W,
             RET_HEADS * RET_QK, RET_HEADS * RET_QK, RET_W, RET_W, LRU_W, LRU_W)
IN_COLS = sum(IN_SPLITS)

N_EXPERTS = 16
EC_CAPACITY = 2
MOE_FF = 1024

kernel_name = 'hybrid_diffusion_diffattn_retention_rglru_ecmoe'


def _rms(x, g):
    xf = x.astype(jnp.float32)
    y = xf * lax.rsqrt(jnp.mean(xf * xf, axis=-1, keepdims=True) + EPS)
    return (y * g.astype(jnp.float32)).astype(x.dtype)


def _modulate(h, shift, scale):
    return h * (1 + scale) + shift


def _rope_tables(n_tokens):
    rows = n_tokens // GRID_W
    row = jnp.repeat(jnp.arange(rows), GRID_W).astype(jnp.float32)
    col = jnp.tile(jnp.arange(GRID_W), rows).astype(jnp.float32)
    axis_dim = ATT_QK // 2
    inv = 1.0 / (ROPE_BASE ** (jnp.arange(0, axis_dim, 2, dtype=jnp.float32) / axis_dim))
    ang = jnp.concatenate([row[:, None] * inv, col[:, None] * inv], axis=-1)
    return jnp.cos(ang), jnp.sin(ang)


def _rotate(t, cos, sin):
    n = t.shape[-1] // 2
    t1, t2 = t[..., :n], t[..., n:]
    return jnp.concatenate([t1 * cos - t2 * sin, t1 * sin + t2 * cos], axis=-1)


def _axial_rope(x, cos, sin):
    a = ATT_QK // 2
    f = a // 2
    out = jnp.concatenate([_rotate(x[..., :a], cos[:, :f], sin[:, :f]),
                           _rotate(x[..., a:], cos[:, f:], sin[:, f:])], axis=-1)
    return out.astype(x.dtype)


def _diff_attn_block(q, k, v, lam):
    s = jnp.einsum('bhmqd,bhmkd->bhmqk', q, k).astype(jnp.float32) * (ATT_QK ** -0.5)
    p = jax.nn.softmax(s, axis=-1)
    a = p[:, :, 0] - lam * p[:, :, 1]
    return jnp.einsum('bhqk,bhkv->bhqv', a, v.astype(jnp.float32))


def _diff_attn_latent(q, k, v, lam):
    b, h, _, n, d = q.shape
    nb = n // ATT_BLOCK
    qb = jnp.moveaxis(q.reshape(b, h, 2, nb, ATT_BLOCK, d), 3, 0)
    ob = lax.map(lambda qq: _diff_attn_block(qq, k, v, lam), qb)
    return jnp.moveaxis(ob, 0, 2).reshape(b, h, n, ATT_V)


def _retention_scan(q, k, v, log_g, s0, inclusive):
    b, h, n_tok, dk = q.shape
    dv = v.shape[-1]
    nc = n_tok // RET_CHUNK
    idx = jnp.arange(RET_CHUNK, dtype=jnp.float32)
    diff = idx[:, None] - idx[None, :]
    mask = (diff >= 0) if inclusive else (diff > 0)
    decay_mat = jnp.where(mask, jnp.exp(log_g[:, None, None] * jnp.where(mask, diff, 0.0)), 0.0)
    cross = jnp.exp(log_g[:, None] * (idx + 1.0))[..., None]
    inner = jnp.exp(log_g[:, None] * (RET_CHUNK - 1.0 - idx))[..., None]
    chunk_decay = jnp.exp(log_g * RET_CHUNK)[:, None, None]
    qs = jnp.moveaxis(q.reshape(b, h, nc, RET_CHUNK, dk), 2, 0)
    ks = jnp.moveaxis(k.reshape(b, h, nc, RET_CHUNK, dk), 2, 0)
    vs = jnp.moveaxis(v.reshape(b, h, nc, RET_CHUNK, dv), 2, 0)

    def step(s, inp):
        qc, kc, vc = inp
        sc = jnp.einsum('bhnd,bhmd->bhnm', qc, kc) * decay_mat
        o = jnp.einsum('bhnm,bhmv->bhnv', sc, vc) + jnp.einsum('bhnd,bhdv->bhnv', qc, s) * cross
        s = s * chunk_decay + jnp.einsum('bhmd,bhmv->bhdv', kc * inner, vc)
        return s, o

    s, o = lax.scan(step, s0, (qs, ks, vs))
    return jnp.moveaxis(o, 0, 2).reshape(b, h, n_tok, dv), s


def _retention_state(k, v, log_g):
    n_tok = k.shape[2]
    w = jnp.exp(log_g[:, None] * (n_tok - 1.0 - jnp.arange(n_tok, dtype=jnp.float32)))
    return jnp.einsum('bhld,hl,bhlv->bhdv', k, w, v)


def _short_conv(t, w, bias):
    y = lax.conv_general_dilated(t, w[:, None, :].astype(t.dtype), window_strides=(1,),
                                 padding=[(LRU_CONV // 2, LRU_CONV - 1 - LRU_CONV // 2)],
                                 dimension_numbers=('NWC', 'WIO', 'NWC'),
                                 feature_group_count=LRU_W)
    return y + bias


def _lin_combine(left, right):
    a1, b1 = left
    a2, b2 = right
    return a1 * a2, a2 * b1 + b2


def _rglru(x, gate_w, gate_b, lam, h0):
    x = x.astype(jnp.float32)
    b, n_tok, _ = x.shape
    xb = x.reshape(b, n_tok, LRU_BLOCKS, LRU_BW)
    gates = jnp.einsum('blnc,gncd->gblnd', xb, gate_w.astype(jnp.float32)).reshape(2, b, n_tok, LRU_W)
    gates = gates + gate_b.astype(jnp.float32)[:, None, None, :]
    r = jax.nn.sigmoid(gates[0])
    i = jax.nn.sigmoid(gates[1])
    log_a = -LRU_C * jax.nn.softplus(-lam.astype(jnp.float32)) * r
    a = jnp.exp(log_a)
    u = jnp.sqrt(-jnp.expm1(2.0 * log_a)) * (i * x)
    u = u.at[:, 0].add(a[:, 0] * h0)
    _, h = lax.associative_scan(_lin_combine, (a, u), axis=1)
    return h


def _mixer(hl, hc, cos, sin, layer_idx, need_ctx, w_in, w_out, q_norm_g, k_norm_g, lam_p, subln_g,
           ret_log_decay, ret_norm_g, conv_w, conv_b, gate_w, gate_b, lru_lam, lru_norm_g):
    f32 = jnp.float32
    cuts = np.cumsum(IN_SPLITS)[:-1].tolist()
    qa_l, ka_l, va_l, qr_l, kr_l, vr_l, gr_l, xu_l, gu_l = jnp.split(hl @ w_in, cuts, axis=-1)
    qa_c, ka_c, va_c, qr_c, kr_c, vr_c, gr_c, xu_c, gu_c = jnp.split(hc @ w_in, cuts, axis=-1)

    def qk_heads(t, g):
        b_, n_, _ = t.shape
        return _rms(t.reshape(b_, n_, ATT_HEADS, 2, ATT_QK).transpose(0, 2, 3, 1, 4), g)

    def heads(t, hd):
        b_, n_, _ = t.shape
        return t.reshape(b_, n_, -1, hd).transpose(0, 2, 1, 3)

    def merge(o):
        b_, h_, n_, hd = o.shape
        return o.transpose(0, 2, 1, 3).reshape(b_, n_, h_ * hd)

    lam_init = 0.8 - 0.6 * math.exp(-0.3 * layer_idx)
    lp = lam_p.astype(f32)
    lam = jnp.exp(jnp.sum(lp[0] * lp[1])) - jnp.exp(jnp.sum(lp[2] * lp[3])) + lam_init
    q_l = _axial_rope(qk_heads(qa_l, q_norm_g), cos, sin)
    k_l = _axial_rope(qk_heads(ka_l, k_norm_g), cos, sin)
    k_c = qk_heads(ka_c, k_norm_g)
    v_l = heads(va_l, ATT_V)
    v_c = heads(va_c, ATT_V)
    k_all = jnp.concatenate([k_c, k_l], axis=3)
    v_all = jnp.concatenate([v_c, v_l], axis=2)

    def att_out(o):
        return merge(_rms(o, subln_g) * (1.0 - lam_init))

    att_l = att_out(_diff_attn_latent(q_l, k_all, v_all, lam))

    flip = lambda t: t[:, :, ::-1]
    lg_f = ret_log_decay[0].astype(f32)
    lg_b = ret_log_decay[1].astype(f32)
    kscale = RET_QK ** -0.5
    rq_l = heads(qr_l, RET_QK).astype(f32)
    rk_l = (heads(kr_l, RET_QK) * kscale).astype(f32)
    rv_l = heads(vr_l, RET_V).astype(f32)
    rk_c = (heads(kr_c, RET_QK) * kscale).astype(f32)
    rv_c = heads(vr_c, RET_V).astype(f32)
    if need_ctx:
        rq_c = heads(qr_c, RET_QK).astype(f32)
        zero_s = jnp.zeros((hc.shape[0], RET_HEADS, RET_QK, RET_V), f32)
        o_cf, s_cf = _retention_scan(rq_c, rk_c, rv_c, lg_f, zero_s, True)
        o_cb, s_cb = _retention_scan(flip(rq_c), flip(rk_c), flip(rv_c), lg_b, zero_s, False)
    else:
        s_cf = _retention_state(rk_c, rv_c, lg_f)
        s_cb = _retention_state(flip(rk_c), flip(rv_c), lg_b)
    o_lf, _ = _retention_scan(rq_l, rk_l, rv_l, lg_f, s_cf, True)
    o_lb, _ = _retention_scan(flip(rq_l), flip(rk_l), flip(rv_l), lg_b, s_cb, False)

    def ret_out(o, g):
        return merge(_rms(o, ret_norm_g)) * jax.nn.silu(g.astype(f32))

    ret_l = ret_out(o_lf + flip(o_lb), gr_l)

    u_l = _short_conv(xu_l, conv_w, conv_b)
    u_c = _short_conv(xu_c, conv_w, conv_b)
    h0 = jnp.zeros((hc.shape[0], LRU_W), f32)
    lc_f = _rglru(u_c, gate_w[0], gate_b[0], lru_lam[0], h0)
    ll_f = _rglru(u_l, gate_w[0], gate_b[0], lru_lam[0], lc_f[:, -1])
    lc_b = _rglru(u_c[:, ::-1], gate_w[1], gate_b[1], lru_lam[1], h0)
    ll_b = _rglru(u_l[:, ::-1], gate_w[1], gate_b[1], lru_lam[1], lc_b[:, -1])

    def lru_out(hf, hb_rev, g):
        return _rms((hf + hb_rev[:, ::-1]) * jax.nn.gelu(g.astype(f32)), lru_norm_g)

    lru_l = lru_out(ll_f, ll_b, gu_l)

    mix_l = jnp.concatenate([att_l, ret_l, lru_l], axis=-1).astype(hl.dtype) @ w_out
    if not need_ctx:
        return mix_l, None
    att_c = att_out(_diff_attn_block(qk_heads(qa_c, q_norm_g), k_c, v_c, lam))
    ret_c = ret_out(o_cf + flip(o_cb), gr_c)
    lru_c = lru_out(lc_f, lc_b, gu_c)
    mix_c = jnp.concatenate([att_c, ret_c, lru_c], axis=-1).astype(hc.dtype) @ w_out
    return mix_l, mix_c


def _expert_choice_ffn(h, router_w, w_gate, w_up, w_down):
    b, n_tok, _ = h.shape
    cap = EC_CAPACITY * n_tok // N_EXPERTS
    probs = jax.nn.softmax((h @ router_w).astype(jnp.float32), axis=-1)
    aff, idx = lax.top_k(jnp.swapaxes(probs, 1, 2), cap)
    bidx = jnp.arange(b)[:, None, None]
    xs = h[bidx, idx]
    hid = jax.nn.silu(jnp.einsum('becd,edf->becf', xs, w_gate)) * jnp.einsum('becd,edf->becf', xs, w_up)
    out = jnp.einsum('becf,efd->becd', hid, w_down) * aff[..., None]
    return jnp.zeros_like(h).at[bidx, idx].add(out.astype(h.dtype))


def setup_inputs(seed: int = 0) -> dict:
    key = jax.random.key(seed)
    ks = jax.random.split(key, 32)
    f32 = jnp.float32
    D = D_MODEL

    def nrm(k, shape, scale):
        return jax.random.normal(k, shape, f32) * scale

    ret_base = jnp.asarray(np.log(1.0 - 2.0 ** (-5.0 - np.arange(RET_HEADS))), f32)
    u = jax.random.uniform(ks[20], (DEPTH, 2, LRU_W), f32, 0.9, 0.999)
    a0 = u ** (1.0 / LRU_C)
    return {
        'x': nrm(ks[0], (BATCH, SEQ, D), 1.0),
        'c': nrm(ks[1], (BATCH, D), 1.0),
        'ctx': nrm(ks[2], (BATCH, CTX_LEN, D), 1.0),
        'c_ctx': nrm(ks[3], (D,), 1.0),
        'mod_w': nrm(ks[4], (DEPTH, D, 6 * D), 0.5 * D ** -0.5),
        'mod_b': nrm(ks[5], (DEPTH, 6 * D), 0.01),
        'norm1_g': 1.0 + nrm(ks[6], (DEPTH, D), 0.02),
        'norm2_g': 1.0 + nrm(ks[7], (DEPTH, D), 0.02),
        'w_in': nrm(ks[8], (DEPTH, D, IN_COLS), D ** -0.5),
        'w_out': nrm(ks[9], (DEPTH, MIX_W, D), MIX_W ** -0.5),
        'att_q_norm_g': 1.0 + nrm(ks[10], (DEPTH, ATT_QK), 0.02),
        'att_k_norm_g': 1.0 + nrm(ks[11], (DEPTH, ATT_QK), 0.02),
        'att_lambda': nrm(ks[12], (DEPTH, 4, ATT_QK), 0.1),
        'att_subln_g': 1.0 + nrm(ks[13], (DEPTH, ATT_V), 0.02),
        'ret_log_decay': ret_base * jnp.exp(nrm(ks[14], (DEPTH, 2, RET_HEADS), 0.1)),
        'ret_norm_g': 1.0 + nrm(ks[15], (DEPTH, RET_V), 0.02),
        'lru_conv_w': nrm(ks[16], (DEPTH, LRU_CONV, LRU_W), LRU_CONV ** -0.5),
        'lru_conv_b': nrm(ks[17], (DEPTH, LRU_W), 0.01),
        'lru_gate_w': nrm(ks[18], (DEPTH, 2, 2, LRU_BLOCKS, LRU_BW, LRU_BW), LRU_BW ** -0.5),
        'lru_gate_b': nrm(ks[19], (DEPTH, 2, 2, LRU_W), 0.01),
        'lru_lambda': jnp.log(a0) - jnp.log1p(-a0),
        'lru_norm_g': 1.0 + nrm(ks[21], (DEPTH, LRU_W), 0.02),
        'router_w': nrm(ks[22], (DEPTH, D, N_EXPERTS), D ** -0.5),
        'exp_w_gate': nrm(ks[23], (DEPTH, N_EXPERTS, D, MOE_FF), D ** -0.5),
        'exp_w_up': nrm(ks[24], (DEPTH, N_EXPERTS, D, MOE_FF), D ** -0.5),
        'exp_w_down': nrm(ks[25], (DEPTH, N_EXPERTS, MOE_FF, D), MOE_FF ** -0.5),
    }


def reference(x, c, ctx, c_ctx, mod_w, mod_b, norm1_g, norm2_g, w_in, w_out, att_q_norm_g, att_k_norm_g,
              att_lambda, att_subln_g, ret_log_decay, ret_norm_g, lru_conv_w, lru_conv_b, lru_gate_w,
              lru_gate_b, lru_lambda, lru_norm_g, router_w, exp_w_gate, exp_w_up, exp_w_down):
    cos, sin = _rope_tables(x.shape[1])
    silu_c = jax.nn.silu(c)
    silu_cc = jax.nn.silu(c_ctx)
    xl, xc = x, ctx
    for i in range(DEPTH):
        need_ctx = i < DEPTH - 1
        sh1_l, sc1_l, g1_l, sh2_l, sc2_l, g2_l = jnp.split((silu_c @ mod_w[i] + mod_b[i])[:, None, :], 6, axis=-1)
        sh1_c, sc1_c, g1_c, sh2_c, sc2_c, g2_c = jnp.split(silu_cc @ mod_w[i] + mod_b[i], 6, axis=-1)
        hl = _modulate(_rms(xl, norm1_g[i]), sh1_l, sc1_l)
        hc = _modulate(_rms(xc, norm1_g[i]), sh1_c, sc1_c)
        mix_l, mix_c = _mixer(hl, hc, cos, sin, i, need_ctx, w_in[i], w_out[i], att_q_norm_g[i],
                              att_k_norm_g[i], att_lambda[i], att_subln_g[i], ret_log_decay[i], ret_norm_g[i],
                              lru_conv_w[i], lru_conv_b[i], lru_gate_w[i], lru_gate_b[i], lru_lambda[i],
                              lru_norm_g[i])
        xl = xl + g1_l * mix_l
        xl = xl + g2_l * _expert_choice_ffn(_modulate(_rms(xl, norm2_g[i]), sh2_l, sc2_l),
                                            router_w[i], exp_w_gate[i], exp_w_up[i], exp_w_down[i])
        if need_ctx:
            xc = xc + g1_c * mix_c
            xc = xc + g2_c * _expert_choice_ffn(_modulate(_rms(xc, norm2_g[i]), sh2_c, sc2_c),
                                                router_w[i], exp_w_gate[i], exp_w_up[i], exp_w_down[i])
    return xl
```

```python
import math
import threading
from contextlib import ExitStack
import numpy as np
import concourse.bass as bass
import concourse.mybir as mybir
from concourse.bass_utils import run_bass_kernel_spmd

F32 = mybir.dt.float32
BF16 = mybir.dt.bfloat16
I32 = mybir.dt.int32
ALU = mybir.AluOpType
AF = mybir.ActivationFunctionType
AX = mybir.AxisListType

D = 1024
TC = 256
TL = 4096
TT = TC + TL
NT = TT // 128
DEPTH = 2
EPS = 1e-6
NE = 16
N_CORES = 8


class Buf:
    __slots__ = ("w", "rs")

    def __init__(self):
        self.w = None
        self.rs = {}


class V:
    def __init__(self, ap, buf):
        self.ap = ap
        self.buf = buf

    def __getitem__(self, k):
        return V(self.ap[k], self.buf)

    def m(self, f):
        return V(f(self.ap), self.buf)


class T:
    def __init__(self, ap, nbuf=1):
        self.ap = ap
        self.bufs = [Buf() for _ in range(nbuf)]

    def __getitem__(self, k):
        return V(self.ap[k], self.bufs[0])

    def b(self, i):
        return V(self.ap, self.bufs[i % len(self.bufs)])


class SideRunner:
    def __init__(self, fn):
        self.go = threading.Semaphore(0)
        self.back = threading.Semaphore(0)
        self.budget = 0
        self.finished = False
        self.exc = None
        self.nops = 0

        def run():
            self.go.acquire()
            try:
                fn()
            except BaseException as e:
                self.exc = e
            self.finished = True
            self.back.release()
        self.thread = threading.Thread(target=run, daemon=True)
        self.thread.start()

    def tick(self):
        while self.budget <= 0:
            self.back.release()
            self.go.acquire()
        self.budget -= 1
        self.nops += 1

    def step(self, n):
        if self.finished:
            return
        self.budget = n
        self.go.release()
        self.back.acquire()
        if self.exc is not None:
            raise self.exc

    def flush(self):
        while not self.finished:
            self.step(10 ** 9)
        if self.exc is not None:
            raise self.exc


class Kern:
    CE = ("pe", "act", "dve", "pool")
    side = None

    def _gate(self):
        r = self.runners.get(threading.current_thread())
        if r is not None:
            r.tick()

    def _costep(self):
        r = self.co.get(threading.current_thread())
        if r is not None:
            r.step(1)

    def spawn(self, fn):
        r = SideRunner(fn)
        self.runners[r.thread] = r
        return r

    def corun(self, fn_a, fn_b):
        r = self.spawn(fn_b)
        me = threading.current_thread()
        self.co[me] = r
        try:
            fn_a()
        finally:
            del self.co[me]
        r.flush()
        del self.runners[r.thread]

    def __init__(self, nc):
        self.nc = nc
        self.eng = dict(pe=nc.tensor, act=nc.scalar, dve=nc.vector, pool=nc.gpsimd, sp=nc.sync)
        self.sems = []
        self.cur = {}
        self.cnt = {}
        self.seen = {e: {} for e in self.eng}
        self.nsem = 0
        for e in self.CE:
            self._new_eng_sem(e)
        self.dpool = {}
        self.dnext = {}
        self.dcnt = {}
        for q, n in (("sp", 16), ("pool", 16), ("act", 6)):
            self.dpool[q] = [self._new_sem() for _ in range(n)]
            self.dnext[q] = 0
            for s in self.dpool[q]:
                self.dcnt[s] = 0
        self.runners = {}
        self.co = {}
        self.stacks = [ExitStack()]
        self.caches = [{}]
        self.uid = 0

    def _new_sem(self):
        h = self.nc.alloc_semaphore(name=f"s{self.nsem}")
        self.nsem += 1
        self.sems.append(h)
        return len(self.sems) - 1

    def _new_eng_sem(self, e):
        self.cur[e] = self._new_sem()
        self.cnt[e] = 0

    def _wait(self, e, ev):
        if ev is None:
            return
        s, v = ev
        if v <= 0 or self.seen[e].get(s, 0) >= v:
            return
        self.eng[e].wait_ge(self.sems[s], v)
        self.seen[e][s] = v

    def _deps(self, e, reads, writes):
        for b in reads:
            self._wait(e, b.w)
        for b in writes:
            self._wait(e, b.w)
            for s, v in list(b.rs.items()):
                self._wait(e, (s, v))

    def _post(self, ev, reads, writes):
        for b in reads:
            if b.rs.get(ev[0], 0) < ev[1]:
                b.rs[ev[0]] = ev[1]
        for b in writes:
            b.w = ev
            b.rs = {}

    @staticmethod
    def _bufs(vs):
        out = []
        for v in vs:
            if isinstance(v, V) and v.buf not in out:
                out.append(v.buf)
        return out

    def op(self, e, fn, outs, ins):
        self._gate()
        reads = self._bufs(ins)
        writes = self._bufs(outs)
        self._deps(e, reads, writes)
        inst = fn(self.eng[e])
        s = self.cur[e]
        self.cnt[e] += 1
        c = self.cnt[e]
        inst.then_inc(self.sems[s], 1)
        if e == "pe":
            self.seen[e][s] = c
        self._post((s, c), reads, writes)
        if c >= 30000:
            self._new_eng_sem(e)
        self._costep()

    def dma(self, q, out, in_, extra_in=(), fn=None, **kw):
        self._gate()
        reads = self._bufs([in_] + list(extra_in))
        writes = self._bufs([out])
        pool = self.dpool[q]
        si = pool[self.dnext[q] % len(pool)]
        self.dnext[q] += 1
        self._wait(q, (si, self.dcnt[si]))
        self._deps(q, reads, writes)
        if fn is None:
            inst = self.eng[q].dma_start(out=out.ap, in_=in_.ap, **kw)
        else:
            inst = fn(self.eng[q])
        self.dcnt[si] += 16
        inst.then_inc(self.sems[si], 16)
        self._post((si, self.dcnt[si]), reads, writes)
        self._costep()

    def barrier(self, engines=None):
        for e in (engines or list(self.eng)):
            for q in self.dpool:
                for si in self.dpool[q]:
                    self._wait(e, (si, self.dcnt[si]))
            for en in self.CE:
                if en != e or en != "pe":
                    self._wait(e, (self.cur[en], self.cnt[en]))

    def push(self):
        self.stacks.append(ExitStack())
        self.caches.append({})

    def pop(self):
        self.barrier()
        self.stacks.pop().close()
        self.caches.pop()

    def sb(self, shape, dtype, nbuf=1, name=None):
        self.uid += 1
        h = self.stacks[-1].enter_context(self.nc.sbuf_tensor(f"{name or 'sb'}_{self.uid}", list(shape), dtype))
        return T(h, nbuf)

    def ps(self, shape, dtype=F32, nbuf=1, name=None):
        self.uid += 1
        h = self.stacks[-1].enter_context(self.nc.psum_tensor(f"{name or 'ps'}_{self.uid}", list(shape), dtype))
        return T(h, nbuf)

    def dr(self, name, shape, dtype, kind=None, nbuf=1):
        if kind is None:
            t = self.nc.dram_tensor(name, list(shape), dtype)
        else:
            t = self.nc.dram_tensor(name, list(shape), dtype, kind=kind)
        return T(t.ap(), nbuf)

    @staticmethod
    def _a(x):
        return x.ap if isinstance(x, V) else x

    def act(self, out, in_, func, bias=None, scale=None, accum=None):
        kw = {}
        if bias is not None:
            kw["bias"] = self._a(bias)
        if scale is not None:
            kw["scale"] = self._a(scale)
        if accum is not None:
            kw["accum_out"] = accum.ap
        self.op("act", lambda E: E.activation(out=out.ap, in_=in_.ap, func=func, **kw),
                [out, accum], [in_, bias, scale])

    def tt(self, e, out, a, b, op):
        self.op(e, lambda E: E.tensor_tensor(out=out.ap, in0=a.ap, in1=b.ap, op=op), [out], [a, b])

    def ts(self, e, out, a, s1, s2=None, op0=ALU.mult, op1=None, accum=None):
        kw = {}
        if op1 is not None:
            kw["op1"] = op1
        if accum is not None:
            kw["accum_out"] = accum.ap
        self.op(e, lambda E: E.tensor_scalar(out=out.ap, in0=a.ap, scalar1=self._a(s1), scalar2=self._a(s2),
                                              op0=op0, **kw), [out, accum], [a, s1, s2])

    def stt(self, out, a, s, b, op0, op1):
        self.op("dve", lambda E: E.scalar_tensor_tensor(out=out.ap, in0=a.ap, scalar=self._a(s), in1=b.ap,
                                                       op0=op0, op1=op1), [out], [a, s, b])

    def copy(self, e, out, in_):
        if e == "act":
            self.op(e, lambda E: E.copy(out=out.ap, in_=in_.ap), [out], [in_])
        else:
            self.op(e, lambda E: E.tensor_copy(out=out.ap, in_=in_.ap), [out], [in_])

    def memset(self, e, out, val):
        self.op(e, lambda E: E.memset(out.ap, val), [out], [])

    def recip(self, out, in_):
        self.op("dve", lambda E: E.reciprocal(out=out.ap, in_=in_.ap), [out], [in_])

    def reduce(self, out, in_, op, axis=AX.X):
        self.op("dve", lambda E: E.tensor_reduce(out=out.ap, in_=in_.ap, axis=axis, op=op), [out], [in_])

    def scan(self, out, d0, d1, init, op0=ALU.mult, op1=ALU.add):
        self.op("dve", lambda E: E.tensor_tensor_scan(out=out.ap, data0=d0.ap, data1=d1.ap, initial=self._a(init),
                                                     op0=op0, op1=op1), [out], [d0, d1, init])

    def mm(self, out, lhsT, rhs, start=True, stop=True):
        self.op("pe", lambda E: E.matmul(out.ap, lhsT.ap, rhs.ap, start=start, stop=stop), [out], [lhsT, rhs])

    def tr(self, out, in_, ident):
        self.op("pe", lambda E: E.transpose(out.ap, in_.ap, ident.ap), [out], [in_, ident])

    def rstd(self, out, ss, n, tmp):
        self.act(tmp, ss, AF.Ln, bias=self.epsc[: ss.ap.shape[0]], scale=1.0 / n)
        self.act(out, tmp, AF.Exp, scale=-0.5)

    def sigmoid(self, out, in_, tmp, nbias=None, scale=1.0):
        if nbias is None:
            self.act(tmp, in_, AF.Exp, scale=-scale)
        else:
            self.act(tmp, in_, AF.Exp, bias=nbias, scale=-scale)
        self.act(tmp, tmp, AF.Ln, bias=self.onec[: in_.ap.shape[0]])
        self.act(out, tmp, AF.Exp, scale=-1.0)


FM_ROWS = [0, 128, 256, 384, 512, 640, 768, 896, 1536, 1664, 1792, 1920, 2560, 2688, 2816, 2944]
NCONST = 128 + 128 + 128 + 512 + 64 + 1
import os
SIDE_N, SIDE_A, SIDE_B = [int(v) for v in os.environ.get('SIDE_CFG', '1,1,1').split(',')]


def host_consts():
    c = np.zeros((128, NCONST), np.float32)
    c[:, 0:128] = np.eye(128, dtype=np.float32)
    for i in range(128):
        d = i % 64
        p = d + 16 if (d % 32) < 16 else d - 16
        c[(i // 64) * 64 + p, 128 + i] = 1.0
    c[0:64, 256:320] = 1.0
    c[64:128, 320:384] = 1.0
    c[:, 384:896] = np.arange(512, dtype=np.float32)[None, :]
    c[:, 896:960] = np.arange(64, dtype=np.float32)[None, :]
    c[:, 960] = np.arange(128, dtype=np.float32)
    return c


def host_rope():
    rows = TL // 64
    row = np.repeat(np.arange(rows), 64).astype(np.float32)
    col = np.tile(np.arange(64), rows).astype(np.float32)
    inv = (1.0 / (np.float32(10000.0) ** (np.arange(0, 32, 2, dtype=np.float32) / np.float32(32)))).astype(np.float32)
    ang = np.concatenate([row[:, None] * inv, col[:, None] * inv], axis=-1).astype(np.float32)
    cos = np.cos(ang).astype(np.float32)
    sin = np.sin(ang).astype(np.float32)
    cosT = np.ones((128, TT), np.float32)
    sinT = np.zeros((128, TT), np.float32)
    for r in range(128):
        d = r % 64
        f = d % 16
        a = f if d < 32 else 16 + f
        cosT[r, TC:] = cos[:, a]
        sinT[r, TC:] = -sin[:, a] if (d % 32) < 16 else sin[:, a]
    return cosT, sinT


def build(stop_after=None, dbg=()):
    nc = bass.Bass("TRN2", target_bir_lowering=False)
    K = Kern(nc)

    def dk(name):
        return "ExternalOutput" if name in dbg else None

    xin = K.dr("xin", [TT, D], F32, "ExternalInput")
    cvec = K.dr("cvec", [D, 2], F32, "ExternalInput")
    consts = K.dr("consts", [128, NCONST], F32, "ExternalInput")
    cosD = K.dr("cosT", [128, TT], F32, "ExternalInput")
    sinD = K.dr("sinT", [128, TT], F32, "ExternalInput")
    W = {}
    for n, shp in (("mod_w", [DEPTH, D, 6 * D]), ("mod_b", [DEPTH, 6 * D]), ("norm1_g", [DEPTH, D]), ("norm2_g", [DEPTH, D]),
                   ("w_in", [DEPTH, D, 3072]), ("w_out", [DEPTH, D, D]), ("att_q_norm_g", [DEPTH, 64]),
                   ("att_k_norm_g", [DEPTH, 64]), ("att_lambda", [DEPTH, 256]), ("att_subln_g", [DEPTH, 128]),
                   ("ret_log_decay", [DEPTH, 8]), ("ret_norm_g", [DEPTH, 64]), ("lru_conv_w", [DEPTH, 4, 256]),
                   ("lru_conv_b", [DEPTH, 256]), ("lru_gate_w", [DEPTH, 2, 2, 4, 64, 64]), ("lru_gate_b", [DEPTH, 4, 256]),
                   ("lru_lambda", [DEPTH, 2, 256]), ("lru_norm_g", [DEPTH, 256]), ("router_w", [DEPTH, D, NE]),
                   ("exp_w_gate", [DEPTH, NE, D, D]), ("exp_w_up", [DEPTH, NE, D, D]), ("exp_w_down", [DEPTH, NE, D, D])):
        W[n] = K.dr(n, shp, F32, "ExternalInput")
    yout = K.dr("y", [TL, D], F32, "ExternalOutput", nbuf=NT)
    S1 = K.dr("S1", [TT, D], F32, dk("S1"), nbuf=NT)
    QT = K.dr("QT", [1024, TT], BF16, dk("QT"), nbuf=64)
    KT = K.dr("KT", [1024, TT], BF16, dk("KT"), nbuf=64)
    Vd = K.dr("Vd", [TT, 512], BF16, dk("Vd"), nbuf=NT)
    RQT = K.dr("RQT", [256, TT], BF16, dk("RQT"), nbuf=32)
    RKT = K.dr("RKT", [256, TT], BF16, dk("RKT"), nbuf=32)
    RKd = K.dr("RKd", [TT, 256], BF16, dk("RKd"), nbuf=NT)
    RVd = K.dr("RVd", [TT, 256], BF16, dk("RVd"), nbuf=NT)
    SGd = K.dr("SGd", [TT, 256], F32, dk("SGd"), nbuf=NT)
    XUd = K.dr("XUd", [256, TT], F32, dk("XUd"), nbuf=32)
    GUd = K.dr("GUd", [256, TT], F32, dk("GUd"), nbuf=32)
    MIXT = K.dr("MIXT", [1024, TT], BF16, dk("MIXT"), nbuf=64)
    H2d = K.dr("H2d", [TT, D], BF16, dk("H2d"), nbuf=NT)
    PRd = K.dr("PRd", [TT, NE], F32, dk("PRd"), nbuf=NT) if "PRd" in dbg else None

    cst = K.sb([128, NCONST], F32, name="cst")
    K.dma("sp", cst[:, :], consts[:, :])
    ident = cst[:, 0:128]
    permc = cst[:, 128:256]
    blk1 = cst[:, 256:384]
    iota = cst[:, 384:896]
    tidx = cst[:, 896:960]
    pidx = cst[:, 960:961]
    identb_t = K.sb([128, 128], BF16, name="identb")
    K.copy("dve", identb_t[:, :], ident)
    identb = identb_t[:, :]
    ones_t = K.sb([128, 128], F32, name="ones")
    K.memset("dve", ones_t[:, :], 1.0)
    ones = ones_t[:, :]
    onesb_t = K.sb([128, 512], BF16, name="onesb")
    K.memset("dve", onesb_t[:, :], 1.0)
    onesb = onesb_t[:, :]
    epsc_t = K.sb([128, 1], F32, name="epsc")
    K.memset("dve", epsc_t[:, :], EPS)
    K.epsc = epsc_t[:, :]
    onec_t = K.sb([128, 1], F32, name="onec")
    K.memset("dve", onec_t[:, :], 1.0)
    K.onec = onec_t[:, :]
    csil = K.sb([128, 8, 2], F32, name="csil")
    K.dma("sp", csil[:, :, :], cvec.b(0).m(lambda a: a.rearrange("(k p) r -> p k r", p=128)))
    K.act(csil[:, :, :], csil[:, :, :], AF.Silu)
    crep = K.sb([128, 2, 8, 128], BF16, name="crep")
    for r in range(2):
        K.copy("dve", crep[:, r, :, :], csil[:, :, r:r + 1].m(lambda a: a.to_broadcast([128, 8, 128])))
    MODS = K.dr("MODS", [DEPTH, 2, 6 * D], F32, dk("MODS"), nbuf=DEPTH)
    PROBS = K.sb([128, NT, NE], F32, name="PROBS")
    PROBSp = T(PROBS.ap, nbuf=NT)

    def col(dst, src_ap):
        K.dma("sp", dst, V(src_ap.rearrange("(p o) -> p o", o=1), Buf()))

    def bc(dst, src_ap, n=128):
        K.dma("sp", dst, V(src_ap.partition_broadcast(n), Buf()))

    def stage_done(name):
        return stop_after == name

    for L in range(DEPTH):
        need_ctx = L < DEPTH - 1
        Xsrc = xin if L == 0 else S1
        lam_init = 0.8 - 0.6 * math.exp(-0.3 * L)

        def xrows(t0, n):
            if need_ctx:
                return S1[t0:t0 + n, :]
            return yout[t0 - TC:t0 - TC + n, :]

        K.push()
        winb = K.sb([128, 8, 3072], BF16, name="winb", nbuf=1)
        for cgi in range(3):
            K.dma("pool", winb[:, :, cgi * 1024:(cgi + 1) * 1024],
                  W["w_in"].b(0).m(lambda a: a[L, :, cgi * 1024:(cgi + 1) * 1024].rearrange("(k p) c -> p k c", p=128)), max_dma_last_dim=4096)
        K.push()
        modb = K.sb([128, 6 * D], F32, name="modb")
        bc(modb[:, :], W["mod_b"].ap[L])
        n1g = K.sb([128, D], F32, name="n1g")
        bc(n1g[:, :], W["norm1_g"].ap[L])
        n2g = K.sb([128, D], F32, name="n2g")
        bc(n2g[:, :], W["norm2_g"].ap[L])
        MOD = K.sb([128, 2, 6 * D], F32, name="MOD", nbuf=2)
        mw = K.sb([128, 2, 8, 512], BF16, name="mw", nbuf=2)
        pm = K.ps([128, 2, 512], F32, name="pm", nbuf=2)
        for cg in range(12):
            s = cg % 2
            K.dma("pool", mw.b(s)[:, s, :, :],
                  W["mod_w"].b(0).m(lambda a: a[L, :, cg * 512:(cg + 1) * 512].rearrange("(k p) c -> p k c", p=128)), max_dma_last_dim=2048)
            for r in range(2):
                for k in range(8):
                    K.mm(pm.b(r)[:, r, :], crep[:, r, k, :], mw.b(s)[:, s, k, :], start=(k == 0), stop=(k == 7))
                K.tt("dve", MOD.b(r)[:, r, cg * 512:(cg + 1) * 512], pm.b(r)[:, r, :], modb[:, cg * 512:(cg + 1) * 512], ALU.add)
        for r in range(2):
            K.stt(MOD.b(r)[:, r, 1024:2048], MOD.b(r)[:, r, 1024:2048], 1.0, n1g[:, :], ALU.add, ALU.mult)
            K.stt(MOD.b(r)[:, r, 4096:5120], MOD.b(r)[:, r, 4096:5120], 1.0, n2g[:, :], ALU.add, ALU.mult)
        for r in range(2):
            K.dma("sp", MODS.b(L).m(lambda a: a[L, r:r + 1, :]), MOD.b(r)[0:1, r, :])
        K.pop()
        if stage_done(f"mod{L}"):
            break

        def MODv(r, j):
            for cch in reversed(K.caches):
                if ("mod", r, j) in cch:
                    return cch[("mod", r, j)]
            t_ = K.sb([128, D], F32, name="modv")
            K.dma("sp", t_[:, :], MODS.b(L).m(lambda a: a[L, r, j * 1024:(j + 1) * 1024].partition_broadcast(128)))
            K.caches[-1][("mod", r, j)] = t_[:, :]
            return t_[:, :]

        cosS = K.sb([128, 512], F32, name="cosS")
        sinS = K.sb([128, 512], F32, name="sinS")
        gq = K.sb([128, 2], F32, name="gq")
        for j, nm in enumerate(("att_q_norm_g", "att_k_norm_g")):
            for hh in range(2):
                col(gq[hh * 64:(hh + 1) * 64, j:j + 1], W[nm].ap[L])
        permg = K.sb([128, 2, 128], F32, name="permg")
        for j in range(2):
            K.ts("dve", permg[:, j, :], permc, gq[:, j:j + 1], None, ALU.mult)
        xt = K.sb([128, 2, D], F32, name="xt", nbuf=2)
        st = K.sb([128, 8], F32, name="st", nbuf=4)
        tmpn = K.sb([128, D], F32, name="tmpn", nbuf=1)
        hb2 = K.sb([128, 2, 4, D], BF16, name="hb", nbuf=8)
        hT2 = K.sb([128, 2, 8, 512], BF16, name="hT", nbuf=2)
        pT = K.ps([128, 2, 512], F32, name="pT", nbuf=2)
        pF = K.ps([128, 2, 512], F32, name="pF", nbuf=2)
        pF2 = K.ps([128, 2, 512], F32, name="pF2", nbuf=2)
        pS = K.ps([128, 512], F32, name="pS")
        pR = K.ps([128, 512], F32, name="pR")
        raw = K.sb([128, 512], F32, name="raw")
        sq = K.sb([128, 512], F32, name="sq")
        t1 = K.sb([128, 512], F32, name="t1")
        t2 = K.sb([128, 512], F32, name="t2")
        rs = K.sb([128, 512], F32, name="rs")
        ob = K.sb([128, 2, 512], BF16, name="ob", nbuf=2)
        of = K.sb([128, 2, 512], F32, name="of", nbuf=2)
        obt = K.sb([128, 2, 1024], BF16, name="obt", nbuf=2)
        sgt = K.sb([128, 2, 256], F32, name="sgt", nbuf=2)
        tmB = K.sb([128, 256], F32, name="tmB")
        blocks = [(0, TC, 1)] + [(TC + 512 * i, 512, 0) for i in range(8)]
        for r_ in (0, 1):
            MODv(r_, 0)
            MODv(r_, 1)
        cnts = {"fm": 0, "tm": 0}

        def norm_part(bi):
            (t0, Wd, r) = blocks[bi]
            par = bi % 2
            nti = Wd // 128
            for i in range(nti):
                xv = xt.b(i % 2)[:, i % 2, :]
                K.dma("sp", xv, Xsrc.b(0)[t0 + i * 128:t0 + (i + 1) * 128, :])
                sv = st.b(i)
                K.act(tmpn[:, :], xv, AF.Square, accum=sv[:, 0:1])
                K.rstd(sv[:, 2:3], sv[:, 0:1], D, sv[:, 1:2])
                K.stt(tmpn[:, :], xv, sv[:, 2:3], MODv(r, 1), ALU.mult, ALU.mult)
                K.tt("pool", hb2.b(par * 4 + i)[:, par, i, :], tmpn[:, :], MODv(r, 0), ALU.add)
            for k in range(8):
                for i in range(nti):
                    K.mm(pT.b(k)[:, k % 2, i * 128:(i + 1) * 128], hb2.b(par * 4 + i)[:, par, i, k * 128:(k + 1) * 128], identb)
                K.copy("act", hT2.b(par)[:, par, k, 0:Wd], pT.b(k)[:, k % 2, 0:Wd])

        def fm_part(bi):
            (t0, Wd, r) = blocks[bi]
            par = bi % 2
            K.dma("sp", cosS[:, 0:Wd], cosD[:, t0:t0 + Wd])
            K.dma("sp", sinS[:, 0:Wd], sinD[:, t0:t0 + Wd])
            for rc, c0 in enumerate(FM_ROWS):
                s = cnts["fm"] % 2
                cnts["fm"] += 1
                cf = cnts["fm"]
                pf = pF.b(s)[:, s, 0:Wd]
                for k in range(8):
                    K.mm(pf, winb[:, k, c0:c0 + 128], hT2.b(par)[:, par, k, 0:Wd], start=(k == 0), stop=(k == 7))
                if rc < 8:
                    j = 0 if rc < 4 else 1
                    K.copy("act", raw[:, 0:Wd], pf)
                    K.tt("pool", sq[:, 0:Wd], raw[:, 0:Wd], raw[:, 0:Wd], ALU.mult)
                    K.mm(pS[:, 0:Wd], blk1, sq[:, 0:Wd])
                    K.mm(pR[:, 0:Wd], permg[:, j, :], raw[:, 0:Wd])
                    K.rstd(rs[:, 0:Wd], pS[:, 0:Wd], 64, sq[:, 0:Wd])
                    K.stt(t1[:, 0:Wd], raw[:, 0:Wd], gq[:, j:j + 1], cosS[:, 0:Wd], ALU.mult, ALU.mult)
                    K.tt("dve", t2[:, 0:Wd], pR[:, 0:Wd], sinS[:, 0:Wd], ALU.mult)
                    K.tt("pool", t1[:, 0:Wd], t1[:, 0:Wd], t2[:, 0:Wd], ALU.add)
                    K.tt("dve", ob.b(s)[:, s, 0:Wd], t1[:, 0:Wd], rs[:, 0:Wd], ALU.mult)
                    dst = (QT if j == 0 else KT)
                    rr = (rc % 4) * 128
                    K.dma("sp", dst.b(cf)[rr:rr + 128, t0:t0 + Wd], ob.b(s)[:, s, 0:Wd])
                elif rc < 12:
                    j = (rc - 8) // 2
                    K.act(ob.b(s)[:, s, 0:Wd], pf, AF.Copy, scale=(1.0 if j == 0 else 0.125))
                    dst = RQT if j == 0 else RKT
                    rr = ((rc - 8) % 2) * 128
                    K.dma("sp", dst.b(cf)[rr:rr + 128, t0:t0 + Wd], ob.b(s)[:, s, 0:Wd])
                else:
                    j = (rc - 12) // 2
                    K.copy("act", of.b(s)[:, s, 0:Wd], pf)
                    dst = XUd if j == 0 else GUd
                    rr = ((rc - 12) % 2) * 128
                    K.dma("sp", dst.b(cf)[rr:rr + 128, t0:t0 + Wd], of.b(s)[:, s, 0:Wd])

        def tm_part(bi):
            (t0, Wd, r) = blocks[bi]
            par = bi % 2
            for i in range(Wd // 128):
                tok = t0 + i * 128
                ti = tok // 128
                s = cnts["tm"] % 2
                cnts["tm"] += 1
                for g, (c0, cw) in enumerate(((1024, 512), (1792, 512), (2304, 256))):
                    pf = pF2.b(g)[:, g % 2, 0:cw]
                    for k in range(8):
                        K.mm(pf, hT2.b(par)[:, par, k, i * 128:(i + 1) * 128], winb[:, k, c0:c0 + cw], start=(k == 0), stop=(k == 7))
                    if g == 0:
                        K.copy("act", obt.b(s)[:, s, 0:512], pf)
                        K.dma("sp", Vd.b(ti)[tok:tok + 128, :], obt.b(s)[:, s, 0:512])
                    elif g == 1:
                        K.act(obt.b(s)[:, s, 512:768], pF2.b(g)[:, g % 2, 0:256], AF.Copy, scale=0.125)
                        K.copy("dve", obt.b(s)[:, s, 768:1024], pF2.b(g)[:, g % 2, 256:512])
                        K.dma("sp", RKd.b(ti)[tok:tok + 128, :], obt.b(s)[:, s, 512:768])
                        K.dma("sp", RVd.b(ti)[tok:tok + 128, :], obt.b(s)[:, s, 768:1024])
                    else:
                        K.sigmoid(sgt.b(s)[:, s, :], pf, tmB[:, 0:256])
                        K.tt("dve", sgt.b(s)[:, s, :], sgt.b(s)[:, s, :], pf, ALU.mult)
                        K.dma("sp", SGd.b(ti)[tok:tok + 128, :], sgt.b(s)[:, s, :])

        def tm_and_next(bi):
            tm_part(bi)
            if bi + 1 < len(blocks):
                norm_part(bi + 1)

        norm_part(0)
        for bi in range(len(blocks)):
            K.corun(lambda: fm_part(bi), lambda: tm_and_next(bi))
        K.pop()
        if stage_done(f"proj{L}"):
            break

        def side_fn():
            K.push()
            lgr = K.sb([128, 8], F32, name="lgr")
            bc(lgr[:, :], W["ret_log_decay"].ap[L])
            lgc = K.sb([128, 4], F32, name="lgc")
            for dr_ in range(2):
                for hp in range(2):
                    for e in range(2):
                        hd = dr_ * 4 + hp * 2 + e
                        bc(lgc[e * 64:(e + 1) * 64, dr_ * 2 + hp:dr_ * 2 + hp + 1], W["ret_log_decay"].ap[L, hd:hd + 1], n=64)
            rng = K.sb([128, 64], F32, name="rng")
            bc(rng[:, :], W["ret_norm_g"].ap[L])
            pcol = K.sb([128, 4], F32, name="pcol")
            K.copy("dve", pcol[:, 0:1], pidx)
            K.ts("dve", pcol[:, 1:2], pidx, -1.0, 127.0, ALU.mult, ALU.add)
            inner = K.sb([128, 2, 4], F32, name="inner")
            K.act(inner[:, 0, :], lgr[:, 0:4], AF.Exp, scale=pcol[:, 1:2])
            K.act(inner[:, 1, :], lgr[:, 4:8], AF.Exp, scale=pcol[:, 0:1])
            cdt = K.sb([128, 2, 4], F32, name="cdt")
            K.act(cdt[:, :, :].m(lambda a: a.rearrange("p a b -> p (a b)")), lgr[:, :], AF.Exp, scale=128.0)
            crossT = K.sb([128, 2, 2, 128], F32, name="crossT")
            jrow = K.sb([128, 2, 128], F32, name="jrow")
            K.ts("dve", jrow[:, 0, :], iota[:, 0:128], 1.0, None, ALU.add)
            K.ts("dve", jrow[:, 1, :], iota[:, 0:128], -1.0, 128.0, ALU.mult, ALU.add)
            for dr_ in range(2):
                for hp in range(2):
                    K.act(crossT[:, dr_, hp, :], jrow[:, dr_, :], AF.Exp, scale=lgc[:, dr_ * 2 + hp:dr_ * 2 + hp + 1])
            dif = K.sb([128, 128], F32, name="dif")
            K.ts("dve", dif[:, :], iota[:, 0:128], pidx, None, ALU.subtract)
            dpos = K.sb([128, 128], F32, name="dpos")
            dneg = K.sb([128, 128], F32, name="dneg")
            K.ts("dve", dpos[:, :], dif[:, :], 0.0, None, ALU.max)
            K.ts("dve", dneg[:, :], dif[:, :], -1.0, 0.0, ALU.mult, ALU.max)
            mge = K.sb([128, 128], F32, name="mge")
            mlt = K.sb([128, 128], F32, name="mlt")
            K.ts("dve", mge[:, :], dif[:, :], 0.0, None, ALU.is_ge)
            K.ts("dve", mlt[:, :], dif[:, :], 0.0, None, ALU.is_lt)
            Dfb = K.sb([128, 4, 128], F32, name="Dfb")
            dtmp = K.sb([128, 128], F32, name="dtmp")
            for h in range(4):
                K.act(dtmp[:, :], dpos[:, :], AF.Exp, scale=lgr[:, h:h + 1])
                K.tt("dve", Dfb[:, h, :], dtmp[:, :], mge[:, :], ALU.mult)
                K.act(dtmp[:, :], dneg[:, :], AF.Exp, scale=lgr[:, 4 + h:5 + h])
                K.tt("dve", dtmp[:, :], dtmp[:, :], mlt[:, :], ALU.mult)
                K.tt("dve", Dfb[:, h, :], Dfb[:, h, :], dtmp[:, :], ALU.add)
            cdtab = K.sb([128, 2, 256], F32, name="cdtab")
            for dr_ in range(2):
                K.copy("dve", cdtab[:, dr_, :].m(lambda a: a.rearrange("p (h v) -> p h v", v=64)),
                       cdt[:, dr_, :].m(lambda a: a.unsqueeze(2).to_broadcast([128, 4, 64])))
            rk = K.sb([128, 2, 256], BF16, name="rk", nbuf=2)
            rv = K.sb([128, 2, 256], BF16, name="rv", nbuf=2)
            rvi = K.sb([128, 2, 2, 256], BF16, name="rvi", nbuf=4)
            PB = K.ps([128, 2, 512], F32, name="PB", nbuf=6)
            KVb = K.sb([128, NT, 256], F32, name="KVb", nbuf=NT)
            SF = K.sb([128, 2, NT + 1, 256], BF16, name="SF", nbuf=2 * (NT + 1))
            s32 = K.sb([128, 2, 256], F32, name="s32", nbuf=2)
            stmp = K.sb([128, 256], F32, name="stmp")
            K.memset("dve", s32[:, 0, :], 0.0)
            K.memset("dve", s32.b(1)[:, 1, :], 0.0)

            def sfv(dr_, c):
                return SF.b(dr_ * (NT + 1) + c)[:, dr_, c, :]
            K.memset("pool", sfv(0, 0), 0.0)
            for c in range(NT):
                s = c % 2
                K.dma("sp", rk.b(s)[:, s, :], RKd.b(c)[c * 128:(c + 1) * 128, :])
                K.dma("sp", rv.b(s)[:, s, :], RVd.b(c)[c * 128:(c + 1) * 128, :])
                for dr_ in range(2):
                    K.tt("pool" if dr_ == 0 else "dve",
                         rvi.b(s * 2 + dr_)[:, s, dr_, :].m(lambda a: a.rearrange("p (h v) -> p h v", v=64)),
                         rv.b(s)[:, s, :].m(lambda a: a.rearrange("p (h v) -> p h v", v=64)),
                         inner[:, dr_, :].m(lambda a: a.unsqueeze(2).to_broadcast([128, 4, 64])), ALU.mult)
                    for hp in range(2):
                        K.mm(PB.b(dr_)[:, dr_, hp * 128:(hp + 1) * 128], rk.b(s)[:, s, hp * 128:(hp + 1) * 128],
                             rvi.b(s * 2 + dr_)[:, s, dr_, hp * 128:(hp + 1) * 128])
                K.tt("pool", stmp[:, :], s32[:, 0, :], cdtab[:, 0, :], ALU.mult)
                K.tt("dve", s32[:, 0, :], stmp[:, :], PB.b(0)[:, 0, 0:256], ALU.add)
                K.copy("act", sfv(0, c + 1), s32[:, 0, :])
                K.copy("act", KVb.b(c)[:, c, :], PB.b(1)[:, 1, 0:256])
            border = [1, 0] + list(range(NT - 1, 1, -1))
            K.memset("pool", sfv(1, border[0]), 0.0)
            for i in range(len(border) - 1):
                c, cn = border[i], border[i + 1]
                K.tt("pool", stmp[:, :], s32.b(1)[:, 1, :], cdtab[:, 1, :], ALU.mult)
                K.tt("dve", s32.b(1)[:, 1, :], stmp[:, :], KVb.b(c)[:, c, :], ALU.add)
                K.copy("act", sfv(1, cn), s32.b(1)[:, 1, :])
            rq = K.sb([128, 2, 2, 128], BF16, name="rq", nbuf=2)
            rkt = K.sb([128, 2, 2, 128], BF16, name="rkt", nbuf=2)
            sg = K.sb([128, 2, 256], F32, name="sg", nbuf=2)
            qc = K.sb([128, 2, 2, 2, 128], BF16, name="qc", nbuf=4)
            AT = K.sb([128, 2, 4, 128], BF16, name="AT", nbuf=2)
            osb = K.sb([128, 256], F32, name="osb")
            rsq2 = K.sb([128, 2, 256], F32, name="rsq", nbuf=2)
            rss2 = K.sb([128, 2, 12], F32, name="rss", nbuf=2)
            gs2 = K.sb([128, 2, 256], F32, name="gs", nbuf=2)
            ro12 = K.sb([128, 2, 256], F32, name="ro1", nbuf=2)
            osb2 = K.sb([128, 2, 256], F32, name="osb2", nbuf=2)
            rob = K.sb([128, 2, 256], BF16, name="rob", nbuf=2)
            rT = K.sb([128, 2, 256], BF16, name="rT", nbuf=2)
            chunks = list(range(NT)) if need_ctx else list(range(2, NT))
            def ret_chunk(c):
                s = c % 2
                rsq, rss, gs, ro1, osb = rsq2.b(s)[:, s], rss2.b(s)[:, s], gs2.b(s)[:, s], ro12.b(s)[:, s], osb2.b(s)[:, s]
                K.dma("sp", rq.b(s)[:, s, :, :], RQT.b(0).m(lambda a: a[:, c * 128:(c + 1) * 128].rearrange("(k p) t -> p k t", p=128)))
                K.dma("sp", rkt.b(s)[:, s, :, :], RKT.b(0).m(lambda a: a[:, c * 128:(c + 1) * 128].rearrange("(k p) t -> p k t", p=128)))
                K.dma("sp", rv.b(s)[:, s, :], RVd.b(c)[c * 128:(c + 1) * 128, :])
                K.dma("sp", sg.b(s)[:, s, :], SGd.b(c)[c * 128:(c + 1) * 128, :])
                for dr_ in range(2):
                    K.tt("dve" if dr_ == 0 else "pool", qc.b(s * 2 + dr_)[:, s, dr_, :, :], rq.b(s)[:, s, :, :], crossT[:, dr_, :, :], ALU.mult)
                for h in range(4):
                    e, hp = h % 2, h // 2
                    K.mm(PB.b(e)[:, e, hp * 128:(hp + 1) * 128], rkt.b(s)[e * 64:(e + 1) * 64, s, hp, :], rq.b(s)[e * 64:(e + 1) * 64, s, hp, :])
                for e in range(2):
                    K.tt("dve", AT.b(s)[:, s, :, :].m(lambda a: a.rearrange("p (hp e) n -> p hp e n", e=2)[:, :, e, :]),
                         PB.b(e)[:, e, 0:256].m(lambda a: a.rearrange("p (hp n) -> p hp n", n=128)),
                         Dfb[:, :, :].m(lambda a: a.rearrange("p (hp e) n -> p hp e n", e=2)[:, :, e, :]), ALU.mult)
                for h in range(4):
                    e, hp = h % 2, h // 2
                    po = PB.b(2 + e)[:, e, 256 + hp * 64:256 + (hp + 1) * 64]
                    K.mm(po, AT.b(s)[:, s, h, :], rv.b(s)[:, s, h * 64:(h + 1) * 64], start=True, stop=False)
                    for dr_ in range(2):
                        K.mm(po, qc.b(s * 2 + dr_)[e * 64:(e + 1) * 64, s, dr_, hp, :],
                             sfv(dr_, c)[e * 64:(e + 1) * 64, hp * 128 + e * 64:hp * 128 + (e + 1) * 64], start=False, stop=(dr_ == 1))
                for e in range(2):
                    K.copy("act", osb[:, :].m(lambda a: a.rearrange("p (hp e v) -> p hp e v", e=2, v=64)[:, :, e, :]),
                           PB.b(2 + e)[:, e, 256:384].m(lambda a: a.rearrange("p (hp v) -> p hp v", v=64)))
                K.act(rsq[:, :], osb[:, :], AF.Square)
                K.reduce(rss[:, 0:4], rsq[:, :].m(lambda a: a.rearrange("p (h v) -> p h v", v=64)), ALU.add)
                K.rstd(rss[:, 8:12], rss[:, 0:4], 64, rss[:, 4:8])
                K.tt("pool", gs[:, :].m(lambda a: a.rearrange("p (h v) -> p h v", v=64)),
                     sg.b(s)[:, s, :].m(lambda a: a.rearrange("p (h v) -> p h v", v=64)),
                     rng[:, :].m(lambda a: a.unsqueeze(1).to_broadcast([128, 4, 64])), ALU.mult)
                K.tt("dve", ro1[:, :].m(lambda a: a.rearrange("p (h v) -> p h v", v=64)),
                     osb[:, :].m(lambda a: a.rearrange("p (h v) -> p h v", v=64)),
                     rss[:, 8:12].m(lambda a: a.unsqueeze(2).to_broadcast([128, 4, 64])), ALU.mult)
                K.tt("pool", rob.b(s)[:, s, :], ro1[:, :], gs[:, :], ALU.mult)
                for j in range(2):
                    K.mm(PB.b(4 + j)[:, j, 384:512], rob.b(s)[:, s, j * 128:(j + 1) * 128], identb)
                for j in range(2):
                    K.copy("act", rT.b(s)[:, s, j * 128:(j + 1) * 128], PB.b(4 + j)[:, j, 384:512])
                for j in range(2):
                    K.dma("sp", MIXT.b(c * 2 + j)[512 + j * 128:512 + (j + 1) * 128, c * 128:(c + 1) * 128], rT.b(s)[:, s, j * 128:(j + 1) * 128])

            def run_par(par):
                for c in chunks:
                    if c % 2 == par:
                        ret_chunk(c)
            for c in chunks:
                ret_chunk(c)
            K.pop()

            K.push()
            cw = K.sb([128, 2, 4], F32, name="cw")
            cb = K.sb([128, 2], F32, name="cb")
            gb = K.sb([128, 4, 2], F32, name="gb")
            lam = K.sb([128, 4], F32, name="lam")
            lng = K.sb([128, 2], F32, name="lng")
            for c in range(2):
                for j in range(4):
                    col(cw[:, c, j:j + 1], W["lru_conv_w"].ap[L, j, c * 128:(c + 1) * 128])
                col(cb[:, c:c + 1], W["lru_conv_b"].ap[L, c * 128:(c + 1) * 128])
                col(lng[:, c:c + 1], W["lru_norm_g"].ap[L, c * 128:(c + 1) * 128])
                for dg in range(4):
                    col(gb[:, dg, c:c + 1], W["lru_gate_b"].ap[L, dg, c * 128:(c + 1) * 128])
                for dr_ in range(2):
                    col(lam[:, dr_ * 2 + c:dr_ * 2 + c + 1], W["lru_lambda"].ap[L, dr_, c * 128:(c + 1) * 128])
            wbd = K.sb([128, 8, 128], F32, name="wbd")
            K.memset("dve", wbd[:, :, :], 0.0)
            for dr_ in range(2):
                for g in range(2):
                    for c in range(2):
                        for e in range(2):
                            K.dma("sp", wbd[e * 64:(e + 1) * 64, (dr_ * 2 + g) * 2 + c, e * 64:(e + 1) * 64],
                                  W["lru_gate_w"].b(0).m(lambda a: a[L, dr_, g, 2 * c + e, :, :]))
            sp = K.sb([128, 8, 4], F32, name="sp")
            K.ts("dve", sp[:, 5, :], lam[:, :], -1.0, None, ALU.mult)
            K.tt("dve", sp[:, 0, :], lam[:, :], sp[:, 5, :], ALU.max)
            K.act(sp[:, 1, :], sp[:, 0, :], AF.Exp, scale=-1.0)
            K.ts("dve", sp[:, 2, :], sp[:, 1, :], 2.0, None, ALU.add)
            K.recip(sp[:, 2, :], sp[:, 2, :])
            K.tt("dve", sp[:, 2, :], sp[:, 2, :], sp[:, 1, :], ALU.mult)
            K.tt("dve", sp[:, 3, :], sp[:, 2, :], sp[:, 2, :], ALU.mult)
            K.memset("dve", sp[:, 4, :], 1.0 / 15.0)
            for n_ in (13, 11, 9, 7, 5, 3, 1):
                K.tt("dve", sp[:, 4, :], sp[:, 4, :], sp[:, 3, :], ALU.mult)
                K.ts("dve", sp[:, 4, :], sp[:, 4, :], 1.0 / n_, None, ALU.add)
            K.tt("dve", sp[:, 4, :], sp[:, 4, :], sp[:, 2, :], ALU.mult)
            K.ts("dve", sp[:, 5, :], lam[:, :], -1.0, 0.0, ALU.mult, ALU.max)
            K.stt(sp[:, 6, :], sp[:, 4, :], 2.0, sp[:, 5, :], ALU.mult, ALU.add)
            K.ts("dve", sp[:, 7, :], sp[:, 6, :], -8.0, None, ALU.mult)
            ccoef = sp[:, 7, :]
            PADL = TC + 3
            xu = K.sb([128, 2, TT + 6], F32, name="xu")
            K.memset("pool", xu[:, :, :], 0.0)
            for c in range(2):
                K.dma("sp", xu[:, c, 2:2 + TC], XUd.b(0)[c * 128:(c + 1) * 128, 0:TC])
                K.dma("sp", xu[:, c, PADL + 2:PADL + 2 + TL], XUd.b(0)[c * 128:(c + 1) * 128, TC:TT])
            u = K.sb([128, 2, TT], F32, name="u")
            for c in range(2):
                for (pb, t0, Ln) in ((2, 0, TC), (PADL + 2, TC, TL)):
                    e = "dve"
                    K.ts(e, u[:, c, t0:t0 + Ln], xu[:, c, pb - 2:pb - 2 + Ln], cw[:, c, 0:1], cb[:, c:c + 1], ALU.mult, ALU.add)
                    for j in range(1, 4):
                        K.stt(u[:, c, t0:t0 + Ln], xu[:, c, pb - 2 + j:pb - 2 + j + Ln], cw[:, c, j:j + 1], u[:, c, t0:t0 + Ln], ALU.mult, ALU.add)
            hf = xu
            pG = K.ps([128, 2, 512], F32, name="pG", nbuf=2)
            gr_2 = K.sb([128, 2, 512], F32, name="gr", nbuf=2)
            gtmp_2 = K.sb([128, 2, 512], F32, name="gtmp", nbuf=2)
            ngb = K.sb([128, 4, 2], F32, name="ngb")
            K.ts("dve", ngb[:, :, :], gb[:, :, :], -1.0, None, ALU.mult)
            gi_2 = K.sb([128, 2, 512], F32, name="gi", nbuf=2)
            ga_2 = K.sb([128, 2, 512], F32, name="ga", nbuf=2)
            gw_2 = K.sb([128, 2, 512], F32, name="gw", nbuf=2)
            gbv_2 = K.sb([128, 2, 512], F32, name="gbv", nbuf=2)
            ar_2 = K.sb([128, 2, 512], F32, name="ar", nbuf=2)
            br_2 = K.sb([128, 2, 512], F32, name="br", nbuf=2)
            hbr = K.sb([128, 2, 512], F32, name="hbr", nbuf=2)
            hst = K.sb([128, 2], F32, name="hst", nbuf=2)
            yc = K.sb([128, 2, 512], F32, name="yc", nbuf=2)
            gu = K.sb([128, 2, 512], F32, name="gu", nbuf=2)
            g2_2 = K.sb([128, 2, 512], F32, name="g2", nbuf=2)
            ysq = K.sb([128, 2, 512], F32, name="ysq", nbuf=2)
            yrs = K.sb([128, 512], F32, name="yrs")
            yob = K.sb([128, 2, 512], BF16, name="yob", nbuf=2)

            def tmps(c):
                return [t_.b(c)[:, c] for t_ in (gr_2, gtmp_2, gi_2, ga_2, gw_2, gbv_2, ar_2, br_2, g2_2)]

            def gates(dr_, c, t0, Wd):
                gr, gtmp, gi, ga, gw, gbv, ar, br, g2 = tmps(c)
                for g, dst in ((0, gr), (1, gi)):
                    K.mm(pG.b(c)[:, c, 0:Wd], wbd[:, (dr_ * 2 + g) * 2 + c, :], u[:, c, t0:t0 + Wd])
                    K.sigmoid(dst[:, 0:Wd], pG.b(c)[:, c, 0:Wd], gtmp[:, 0:Wd], nbias=ngb[:, dr_ * 2 + g, c:c + 1])
                K.act(ga[:, 0:Wd], gr[:, 0:Wd], AF.Exp, scale=ccoef[:, dr_ * 2 + c:dr_ * 2 + c + 1])
                K.tt("pool", gw[:, 0:Wd], ga[:, 0:Wd], ga[:, 0:Wd], ALU.mult)
                K.ts("dve", gw[:, 0:Wd], gw[:, 0:Wd], -1.0, 1.0, ALU.mult, ALU.add)
                K.act(gw[:, 0:Wd], gw[:, 0:Wd], AF.Ln)
                K.act(gw[:, 0:Wd], gw[:, 0:Wd], AF.Exp, scale=0.5)
                K.tt("pool", gbv[:, 0:Wd], gi[:, 0:Wd], u[:, c, t0:t0 + Wd], ALU.mult)
                K.tt("dve", gbv[:, 0:Wd], gbv[:, 0:Wd], gw[:, 0:Wd], ALU.mult)

            lblocks = [(0, TC)] + [(TC + 512 * i, 512) for i in range(8)]

            def fwd_chain(c):
                gr, gtmp, gi, ga, gw, gbv, ar, br, g2 = tmps(c)
                for bi, (t0, Wd) in enumerate(lblocks):
                    gates(0, c, t0, Wd)
                    init = 0.0 if bi == 0 else hf.b(0)[:, c, t0 - 1:t0]
                    K.scan(hf.b(0)[:, c, t0:t0 + Wd], ga[:, 0:Wd], gbv[:, 0:Wd], init)
            K.corun(lambda: fwd_chain(0), lambda: fwd_chain(1))
            bblocks = [lblocks[0]] + lblocks[:0:-1]
            nyo = 0

            def bwd_blk(c, bi, t0, Wd, emit):
                gr, gtmp, gi, ga, gw, gbv, ar, br, g2 = tmps(c)
                gates(1, c, t0, Wd)
                K.copy("dve", ar[:, 0:Wd], ga[:, 0:Wd].m(lambda a: a[:, ::-1]))
                K.copy("dve", br[:, 0:Wd], gbv[:, 0:Wd].m(lambda a: a[:, ::-1]))
                init = 0.0 if bi == 0 else hst.b(c)[:, c:c + 1]
                K.scan(hbr.b(c)[:, c, 0:Wd], ar[:, 0:Wd], br[:, 0:Wd], init)
                K.copy("pool", hst.b(c)[:, c:c + 1], hbr.b(c)[:, c, Wd - 1:Wd])
                if not emit:
                    return
                K.dma("sp", gu.b(c)[:, c, 0:Wd], GUd.b(0)[c * 128:(c + 1) * 128, t0:t0 + Wd])
                K.tt("dve", yc.b(c)[:, c, 0:Wd], hf.b(0)[:, c, t0:t0 + Wd], hbr.b(c)[:, c, 0:Wd].m(lambda a: a[:, ::-1]), ALU.add)
                guv = gu.b(c)[:, c, 0:Wd]
                K.tt("pool", g2[:, 0:Wd], guv, guv, ALU.mult)
                K.ts("dve", g2[:, 0:Wd], g2[:, 0:Wd], 0.044715, 1.0, ALU.mult, ALU.add)
                K.tt("pool", g2[:, 0:Wd], g2[:, 0:Wd], guv, ALU.mult)
                K.sigmoid(g2[:, 0:Wd], g2[:, 0:Wd], gtmp[:, 0:Wd], scale=2.0 * math.sqrt(2.0 / math.pi))
                K.tt("pool", g2[:, 0:Wd], g2[:, 0:Wd], guv, ALU.mult)
                K.tt("dve", yc.b(c)[:, c, 0:Wd], yc.b(c)[:, c, 0:Wd], g2[:, 0:Wd], ALU.mult)
                K.tt("pool", ysq.b(c)[:, c, 0:Wd], yc.b(c)[:, c, 0:Wd], yc.b(c)[:, c, 0:Wd], ALU.mult)

            for bi, (t0, Wd) in enumerate(bblocks):
                emit = need_ctx or t0 >= TC
                K.corun(lambda: bwd_blk(0, bi, t0, Wd, emit), lambda: bwd_blk(1, bi, t0, Wd, emit))
                if not emit:
                    continue
                for c in range(2):
                    K.mm(pG.b(0)[:, 0, 0:Wd], ones, ysq.b(c)[:, c, 0:Wd], start=(c == 0), stop=(c == 1))
                K.rstd(yrs[:, 0:Wd], pG.b(0)[:, 0, 0:Wd], 256, ysq.b(0)[:, 0, 0:Wd])
                for c in range(2):
                    K.stt(yob.b(c)[:, c, 0:Wd], yc.b(c)[:, c, 0:Wd], lng[:, c:c + 1], yrs[:, 0:Wd], ALU.mult, ALU.mult)
                    nyo += 1
                    K.dma("sp", MIXT.b(nyo)[768 + c * 128:768 + (c + 1) * 128, t0:t0 + Wd], yob.b(c)[:, c, 0:Wd])
            K.pop()


        K.push()
        lamb = K.sb([128, 256], F32, name="lamb")
        bc(lamb[:, :], W["att_lambda"].ap[L])
        lt = K.sb([128, 8], F32, name="lt")
        lj = K.sb([128, 64], F32, name="lj")
        K.tt("dve", lj[:, :], lamb[:, 0:64], lamb[:, 64:128], ALU.mult)
        K.reduce(lt[:, 0:1], lj[:, :], ALU.add)
        K.tt("dve", lj[:, :], lamb[:, 128:192], lamb[:, 192:256], ALU.mult)
        K.reduce(lt[:, 1:2], lj[:, :], ALU.add)
        K.act(lt[:, 2:4], lt[:, 0:2], AF.Exp)
        K.tt("dve", lt[:, 4:5], lt[:, 3:4], lt[:, 2:3], ALU.subtract)
        K.ts("dve", lt[:, 5:6], lt[:, 4:5], -lam_init, None, ALU.add)
        neglam = lt[:, 5:6]
        gsub = K.sb([128, 1], F32, name="gsub")
        col(gsub[:, :], W["att_subln_g"].ap[L])
        K.ts("dve", gsub[:, :], gsub[:, :], 1.0 - lam_init, None, ALU.mult)
        QTh = K.sb([128, TT], BF16, name="QTh")
        KTh = K.sb([128, TT], BF16, name="KTh")
        Vh = K.sb([128, NT, 128], BF16, name="Vh")
        pSs = K.ps([128, 4, 512], F32, name="pSs", nbuf=4)
        pO = K.ps([128, 2, 512], F32, name="pO", nbuf=2)
        PTt = K.sb([128, 4, 512], BF16, name="PTt", nbuf=4)
        rl = K.sb([128, 2, 512], F32, name="rl", nbuf=2)
        pacc = K.sb([128, 2, 512], F32, name="pacc", nbuf=2)
        o0 = K.sb([128, 512], F32, name="o0")
        o1 = K.sb([128, 512], F32, name="o1")
        osq = K.sb([128, 512], F32, name="osq")
        ors = K.sb([128, 512], F32, name="ors")
        aob = K.sb([128, 2, 512], BF16, name="aob", nbuf=2)
        qblocks = [(TC + 512 * i, 512, list(range(NT))) for i in range(8)]
        if need_ctx:
            qblocks = [(0, TC, [0, 1])] + qblocks
        nqb = 0
        K.side = K.spawn(side_fn)
        for h in range(4):
            K.dma("sp", QTh[:, :], QT.b(0)[h * 128:(h + 1) * 128, :])
            K.dma("sp", KTh[:, :], KT.b(0)[h * 128:(h + 1) * 128, :])
            K.dma("sp", Vh[:, :, :], Vd.b(0).m(lambda a: a[:, h * 128:(h + 1) * 128].rearrange("(c p) v -> p c v", p=128)))
            for (t0, Wd, kcs) in qblocks:
                def st_mm(ci):
                    kc = kcs[ci]
                    for m in range(2):
                        sl = (ci % 2) * 2 + m
                        K.mm(pSs.b(sl)[:, sl, 0:Wd], KTh[m * 64:(m + 1) * 64, kc * 128:(kc + 1) * 128],
                             QTh[m * 64:(m + 1) * 64, t0:t0 + Wd])
                st_mm(0)
                for ci, kc in enumerate(kcs):
                    if ci + 1 < len(kcs):
                        st_mm(ci + 1)
                    K.side.step(SIDE_A)
                    par = ci % 2
                    K.op("act", lambda E, par=par: E.activation(out=PTt.ap[:, par * 2:par * 2 + 2, 0:Wd], in_=pSs.ap[:, par * 2:par * 2 + 2, 0:Wd],
                                                                func=AF.Exp, scale=0.125),
                         [PTt.b(par * 2), PTt.b(par * 2 + 1)], [pSs.b(par * 2), pSs.b(par * 2 + 1)])
                    K.side.step(SIDE_B)
                    for m in range(2):
                        sl = par * 2 + m
                        K.mm(pO.b(m)[:, m, 0:Wd], Vh[:, kc, :], PTt.b(sl)[:, sl, 0:Wd], start=(ci == 0), stop=(ci == len(kcs) - 1))
                    if ci == 0:
                        K.op("dve", lambda E, par=par: E.tensor_copy(out=pacc.ap[:, :, 0:Wd], in_=PTt.ap[:, par * 2:par * 2 + 2, 0:Wd]),
                             [pacc.b(0), pacc.b(1)], [PTt.b(par * 2), PTt.b(par * 2 + 1)])
                    else:
                        K.op("dve", lambda E, par=par: E.tensor_tensor(out=pacc.ap[:, :, 0:Wd], in0=pacc.ap[:, :, 0:Wd],
                                                                      in1=PTt.ap[:, par * 2:par * 2 + 2, 0:Wd], op=ALU.add),
                             [pacc.b(0), pacc.b(1)], [pacc.b(0), pacc.b(1), PTt.b(par * 2), PTt.b(par * 2 + 1)])
                    K.side.step(SIDE_N)
                for m in range(2):
                    K.mm(pSs.b(m)[:, m, 0:Wd], ones, pacc.b(m)[:, m, 0:Wd])
                K.op("act", lambda E: E.activation(out=rl.ap[:, :, 0:Wd], in_=pSs.ap[:, 0:2, 0:Wd], func=AF.Ln),
                     [rl.b(0), rl.b(1)], [pSs.b(0), pSs.b(1)])
                K.op("act", lambda E: E.activation(out=rl.ap[:, :, 0:Wd], in_=rl.ap[:, :, 0:Wd], func=AF.Exp, scale=-1.0),
                     [rl.b(0), rl.b(1)], [rl.b(0), rl.b(1)])
                K.tt("dve", o0[:, 0:Wd], pO.b(0)[:, 0, 0:Wd], rl.b(0)[:, 0, 0:Wd], ALU.mult)
                K.tt("dve", o1[:, 0:Wd], pO.b(1)[:, 1, 0:Wd], rl.b(1)[:, 1, 0:Wd], ALU.mult)
                K.stt(o0[:, 0:Wd], o1[:, 0:Wd], neglam, o0[:, 0:Wd], ALU.mult, ALU.add)
                K.tt("pool", osq[:, 0:Wd], o0[:, 0:Wd], o0[:, 0:Wd], ALU.mult)
                K.mm(pSs.b(2)[:, 2, 0:Wd], ones, osq[:, 0:Wd])
                K.rstd(ors[:, 0:Wd], pSs.b(2)[:, 2, 0:Wd], 128, osq[:, 0:Wd])
                s = nqb % 2
                nqb += 1
                K.stt(aob.b(s)[:, s, 0:Wd], o0[:, 0:Wd], gsub[:, 0:1], ors[:, 0:Wd], ALU.mult, ALU.mult)
                K.dma("sp", MIXT.b(nqb)[h * 128:(h + 1) * 128, t0:t0 + Wd], aob.b(s)[:, s, 0:Wd])
        K.side.flush()
        del K.runners[K.side.thread]
        K.side = None
        K.pop()
        if stage_done(f"att{L}") or stage_done(f"ret{L}") or stage_done(f"lru{L}"):
            break

        K.push()
        woutb = K.sb([128, 8, D], BF16, name="woutb", nbuf=1)
        K.dma("pool", woutb[:, :, :], W["w_out"].b(0).m(lambda a: a[L].rearrange("(k p) c -> p k c", p=128)), max_dma_last_dim=4096)
        rw = K.sb([128, 8, NE], F32, name="rw")
        K.dma("sp", rw[:, :, :], W["router_w"].b(0).m(lambda a: a[L].rearrange("(k p) e -> p k e", p=128)))
        mtp = K.sb([128, 2, 8, 128], BF16, name="mtp", nbuf=2)
        x0 = K.sb([128, 2, D], F32, name="x0", nbuf=2)
        x1 = K.sb([128, 2, D], F32, name="x1", nbuf=2)
        pXp = K.ps([128, 2, 512], F32, name="pXp", nbuf=2)
        junk2 = K.sb([128, 2, D], F32, name="junk2", nbuf=2)
        st2p = K.sb([128, 2, 8], F32, name="st2", nbuf=2)
        tmp2 = K.sb([128, 2, D], F32, name="tmp2", nbuf=2)
        h2f2 = K.sb([128, 2, D], F32, name="h2f", nbuf=2)
        h2b = K.sb([128, 2, D], BF16, name="h2b", nbuf=2)
        pHp = K.ps([128, 2, 512], F32, name="pHp", nbuf=2)
        h2Tp = K.sb([128, 2, 8, 128], F32, name="h2T", nbuf=2)
        pRtp = K.ps([128, 2, 512], F32, name="pRt", nbuf=2)
        exp_ = K.sb([128, 2, NE], F32, name="ex", nbuf=2)
        oblocks = [(TC + 512 * i, 512, 0) for i in range(8)]
        if need_ctx:
            oblocks = [(0, TC, 1)] + oblocks
        for r_ in ([0, 1] if need_ctx else [0]):
            for j_ in (2, 3, 4):
                MODv(r_, j_)

        def out_tile(tok, ti, r, p):
            st2 = st2p.b(p)[:, p]
            h2f = h2f2.b(p)[:, p]
            h2T = h2Tp.b(p)[:, p]
            K.dma("sp", mtp.b(p)[:, p, :, :], MIXT.b(0).m(lambda a: a[:, tok:tok + 128].rearrange("(k p) t -> p k t", p=128)))
            K.dma("sp", x0.b(p)[:, p, :], Xsrc.b(0)[tok:tok + 128, :])
            for hf_ in range(2):
                for k in range(8):
                    K.mm(pXp.b(p)[:, p, :], mtp.b(p)[:, p, k, :], woutb[:, k, hf_ * 512:(hf_ + 1) * 512], start=(k == 0), stop=(k == 7))
                K.tt("dve", x1.b(p)[:, p, hf_ * 512:(hf_ + 1) * 512], pXp.b(p)[:, p, :], MODv(r, 2)[:, hf_ * 512:(hf_ + 1) * 512], ALU.mult)
            K.tt("pool", x1.b(p)[:, p, :], x1.b(p)[:, p, :], x0.b(p)[:, p, :], ALU.add)
            dst = (S1.b(ti)[tok:tok + 128, :] if need_ctx else yout.b(ti)[tok - TC:tok - TC + 128, :])
            K.dma("sp", dst, x1.b(p)[:, p, :])
            K.act(junk2.b(p)[:, p, :], x1.b(p)[:, p, :], AF.Square, accum=st2[:, 0:1])
            K.rstd(st2[:, 2:3], st2[:, 0:1], D, st2[:, 1:2])
            K.stt(tmp2.b(p)[:, p, :], x1.b(p)[:, p, :], st2[:, 2:3], MODv(r, 4), ALU.mult, ALU.mult)
            K.tt("pool", h2f[:, :], tmp2.b(p)[:, p, :], MODv(r, 3), ALU.add)
            K.copy("act", h2b.b(p)[:, p, :], h2f[:, :])
            K.dma("sp", H2d.b(ti)[tok:tok + 128, :], h2b.b(p)[:, p, :])
            for q in range(2):
                for k4 in range(4):
                    k = q * 4 + k4
                    K.tr(pHp.b(p)[:, p, k4 * 128:(k4 + 1) * 128], h2f[:, k * 128:(k + 1) * 128], ident)
                K.copy("act" if q == 0 else "dve", h2T[:, q * 4:(q + 1) * 4, :].m(lambda a: a.rearrange("p k t -> p (k t)")), pHp.b(p)[:, p, :])
            for k in range(8):
                K.mm(pRtp.b(p)[:, p, 0:NE], h2T[:, k, :], rw[:, k, :], start=(k == 0), stop=(k == 7))
            K.reduce(st2[:, 3:4], pRtp.b(p)[:, p, 0:NE], ALU.max)
            K.ts("dve", st2[:, 4:5], st2[:, 3:4], -1.0, None, ALU.mult)
            K.act(exp_.b(p)[:, p, :], pRtp.b(p)[:, p, 0:NE], AF.Exp, bias=st2[:, 4:5], accum=st2[:, 5:6])
            K.recip(st2[:, 6:7], st2[:, 5:6])
            K.ts("dve", PROBSp.b(ti)[:, ti, :], exp_.b(p)[:, p, :], st2[:, 6:7], None, ALU.mult)
            if PRd is not None and L == 0:
                K.dma("sp", PRd.b(ti)[tok:tok + 128, :], PROBSp.b(ti)[:, ti, :])

        def out_tiles(par):
            for (t0, Wd, r) in oblocks:
                for i in range(Wd // 128):
                    tok = t0 + i * 128
                    if (tok // 128) % 2 == par:
                        out_tile(tok, tok // 128, r, par)
        K.corun(lambda: out_tiles(0), lambda: out_tiles(1))
        K.pop()
        if stage_done(f"out{L}"):
            break

        def moe(groups, stream_rows):
            K.push()
            pTp = K.ps([NE, 512], F32, name="pTp")
            pPM = K.ps([128, 32, NE], F32, name="pPM")
            wg = K.sb([128, 2, 2, 8, D], BF16, name="wg", nbuf=4)
            wd = K.sb([128, 8, D], BF16, name="wd")
            wn = ("exp_w_gate", "exp_w_up", "exp_w_down")

            def load_w(e):
                s = e % 2
                for j in range(2):
                    K.dma("pool", wg.b(s * 2 + j)[:, s, j, :, :],
                          W[wn[j]].b(0).m(lambda a: a[L, e].rearrange("(k p) c -> p k c", p=128)), max_dma_last_dim=4096)

            def load_wd(e):
                K.dma("pool", wd[:, :, :], W[wn[2]].b(0).m(lambda a: a[L, e].rearrange("(k p) c -> p k c", p=128)),
                      max_dma_last_dim=4096)

            load_w(0)
            for g in groups:
                row0, ntk, cap = g["row0"], g["ntk"], g["cap"]
                Tn = ntk * 128
                ti0 = row0 // 128
                g["ncj"] = (cap + 127) // 128
                g["cwj"] = min(cap, 128)
                PM = K.sb([128, ntk, NE], F32, name="PM")
                g["PM"] = PM
                K.push()
                PTm = K.sb([NE, Tn], F32, name="PTm")
                for i in range(ntk):
                    K.tr(pTp[:, (i % 4) * 128:(i % 4 + 1) * 128], PROBS[:, ti0 + i, :], ident)
                    if i % 4 == 3 or i == ntk - 1:
                        n = (i % 4 + 1) * 128
                        K.copy("act", PTm[:, (i // 4) * 512:(i // 4) * 512 + n], pTp[:, 0:n])
                bs = K.sb([NE, 8], F32, name="bs")
                K.memset("dve", bs[:, 0:1], 0.0)
                K.memset("dve", bs[:, 1:2], 1.0)
                bj = K.sb([NE, Tn], BF16, name="bj")
                for it in range(30):
                    K.tt("dve", bs[:, 2:3], bs[:, 0:1], bs[:, 1:2], ALU.add)
                    K.ts("dve", bs[:, 2:3], bs[:, 2:3], 0.5, None, ALU.mult)
                    K.ts("dve", bj[:, :], PTm[:, :], bs[:, 2:3], 0.0, ALU.is_ge, ALU.add, accum=bs[:, 3:4])
                    K.ts("dve", bs[:, 4:5], bs[:, 3:4], float(cap), None, ALU.is_ge)
                    K.tt("dve", bs[:, 5:6], bs[:, 2:3], bs[:, 0:1], ALU.subtract)
                    K.stt(bs[:, 0:1], bs[:, 5:6], bs[:, 4:5], bs[:, 0:1], ALU.mult, ALU.add)
                    K.tt("dve", bs[:, 5:6], bs[:, 1:2], bs[:, 2:3], ALU.subtract)
                    K.stt(bs[:, 1:2], bs[:, 5:6], bs[:, 4:5], bs[:, 2:3], ALU.mult, ALU.add)
                K.ts("dve", bj[:, :], PTm[:, :], bs[:, 0:1], None, ALU.is_ge)
                pos = K.sb([NE, Tn], F32, name="pos")
                K.scan(pos[:, :], bj[:, :], bj[:, :], 0.0, op0=ALU.add, op1=ALU.max)
                K.tt("dve", pos[:, :], pos[:, :], bj[:, :], ALU.mult)
                K.ts("dve", pos[:, :], pos[:, :], -1.0, None, ALU.add)
                for i in range(ntk):
                    K.tr(pPM[:, i, :], pos[:, i * 128:(i + 1) * 128], ident[0:NE, 0:NE])
                K.copy("act", PM[:, :, :], pPM[:, 0:ntk, :])
                K.pop()
                vals = K.sb([128, ntk, NE, 5], BF16, name="vals")
                g["vals"] = vals
                K.copy("dve", vals[:, :, :, 0], tidx[:, 0:ntk].m(lambda a: a.unsqueeze(2).to_broadcast([128, ntk, NE])))
                K.copy("dve", vals[:, :, :, 1], pidx.m(lambda a: a.unsqueeze(2).to_broadcast([128, ntk, NE])))
                pr = PROBS[:, ti0:ti0 + ntk, :]
                vf = K.sb([128, ntk, NE], F32, name="vf")
                r1 = K.sb([128, ntk, NE], F32, name="r1")
                K.copy("dve", vals[:, :, :, 2], pr)
                K.copy("dve", vf[:, :, :], vals[:, :, :, 2])
                K.tt("dve", r1[:, :, :], pr, vf[:, :, :], ALU.subtract)
                K.copy("dve", vals[:, :, :, 3], r1[:, :, :])
                K.copy("dve", vf[:, :, :], vals[:, :, :, 3])
                K.tt("dve", r1[:, :, :], r1[:, :, :], vf[:, :, :], ALU.subtract)
                K.copy("dve", vals[:, :, :, 4], r1[:, :, :])
                ncj = g["ncj"]
                g["idxi"] = K.sb([128, 2, 4], I32, name="idxi", nbuf=2)
                g["idxs"] = K.sb([128, 2, 4], I32, name="idxs", nbuf=2)
                g["aff"] = K.sb([128, 2, 4], F32, name="aff", nbuf=2)
                g["Xg"] = K.sb([128, ncj, D], BF16, name="Xg", nbuf=ncj)
                g["XT"] = K.sb([128, 2, 8, cap], BF16, name="XT", nbuf=2)
                g["hid"] = K.sb([128, 8, cap], BF16, name="hid", nbuf=8)
            NSEL = 6
            sel = K.sb([128, NSEL, 512], BF16, name="sel", nbuf=NSEL)
            pPMf = pPM.ap.rearrange("p t e -> p (t e)")
            pIg = [T(pTp.ap[0:5, :]), T(pPMf[0:5, 64:64 + 64])]
            pITg = [T(pPMf[:, 0:32]), T(pPMf[:, 32:40])]
            ilg = [K.sb([8, 512], F32, name="il"), K.sb([8, 64], F32, name="il2")]
            ilT = K.sb([128, 2, 4, 8], F32, name="ilT", nbuf=2)
            idxf = K.sb([128, 2, 4], F32, name="idxf", nbuf=2)
            pXT = K.ps([128, 512], F32, name="pXT")
            pGU = K.ps([128, 2, 512], F32, name="pGU", nbuf=2)
            pC = K.ps([128, 512], F32, name="pC")
            sgl = K.sb([128, 512], F32, name="sgl")
            pY = K.ps([128, 2, 512], F32, name="pY", nbuf=2)
            ysb = K.sb([128, 2, D], F32, name="ysb", nbuf=2)
            cnt = {"sel": 0, "y": 0}
            scb = [[Buf() for _ in range(8)] for _ in range(2)]
            for b_ in scb[0] + scb[1]:
                b_.w = stream_rows.buf.w

            def sel_op(e, gi, i, slot):
                g = groups[gi]
                cap = g["cap"]
                K.ts("dve", sel.b(slot)[:, slot, 0:cap], iota[:, 0:cap], g["PM"][:, i, e:e + 1], None, ALU.is_equal)

            def idx_mm(e, gi, i, slot):
                g = groups[gi]
                cap, ntk = g["cap"], g["ntk"]
                K.mm(pIg[gi][:, 0:cap], g["vals"][:, i, e, :], sel.b(slot)[:, slot, 0:cap], start=(i == 0), stop=(i == ntk - 1))

            def prep_fin(e):
                s = e % 2
                for gi, g in enumerate(groups):
                    cap, ncj, cwj = g["cap"], g["ncj"], g["cwj"]
                    il = ilg[gi]
                    K.copy("act", il[0:5, 0:cap], pIg[gi][:, 0:cap])
                    for jc in range(ncj):
                        K.tr(pITg[gi][0:cwj, jc * 8:jc * 8 + 5], il[0:5, jc * 128:jc * 128 + cwj], ident[0:5, 0:5])
                    iT = ilT.b(gi)[0:cwj, gi, 0:ncj, 0:5]
                    K.copy("act", iT, pITg[gi][0:cwj, 0:ncj * 8].m(lambda a: a.rearrange("p (j f) -> p j f", f=8)[:, :, 0:5]))
                    iF = idxf.b(gi)[0:cwj, gi, 0:ncj]
                    K.stt(iF, ilT.b(gi)[0:cwj, gi, 0:ncj, 0], 128.0, ilT.b(gi)[0:cwj, gi, 0:ncj, 1], ALU.mult, ALU.add)
                    K.ts("dve", iF, iF, float(g["srow0"]), None, ALU.add)
                    K.copy("dve", g["idxs"].b(s)[0:cwj, s, 0:ncj], iF)
                    K.ts("dve", iF, iF, float(g["row0"] - g["srow0"]), None, ALU.add)
                    K.copy("dve", g["idxi"].b(s)[0:cwj, s, 0:ncj], iF)
                    K.tt("dve", g["aff"].b(s)[0:cwj, s, 0:ncj], ilT.b(gi)[0:cwj, gi, 0:ncj, 2], ilT.b(gi)[0:cwj, gi, 0:ncj, 3], ALU.add)
                    K.tt("dve", g["aff"].b(s)[0:cwj, s, 0:ncj], g["aff"].b(s)[0:cwj, s, 0:ncj], ilT.b(gi)[0:cwj, gi, 0:ncj, 4], ALU.add)
                    for jc in range(ncj):
                        iv = g["idxi"].b(s)[0:cwj, s, jc:jc + 1]
                        Xg = g["Xg"]
                        K.dma("pool", Xg.b(jc)[0:cwj, jc, :], H2d.b(0)[:, :], extra_in=[iv],
                              fn=lambda E, jc=jc, iv=iv, Xg=Xg, cwj=cwj: E.indirect_dma_start(
                                  out=Xg.ap[0:cwj, jc, :], out_offset=None, in_=H2d.ap[:, :],
                                  in_offset=bass.IndirectOffsetOnAxis(ap=iv.ap, axis=0)))

            def sel_list(e):
                return [(gi, i) for gi, g in enumerate(groups) for i in range(g["ntk"])]

            def prep_xt(e):
                s = e % 2
                for g in groups:
                    cap, ncj, cwj = g["cap"], g["ncj"], g["cwj"]
                    for k in range(8):
                        tgt = (pXT[:, :], pY.b(0)[:, 0, :], pY.b(1)[:, 1, :])[k % 3]
                        for jc in range(ncj):
                            K.mm(tgt[:, jc * 128:jc * 128 + cwj], g["Xg"].b(jc)[0:cwj, jc, k * 128:(k + 1) * 128], identb[0:cwj, 0:cwj])
                        K.copy("act" if k % 2 == 0 else "dve", g["XT"].b(s)[:, s, k, 0:cap], tgt[:, 0:cap])

            def gate_up(e, pend):
                s = e % 2
                per = (len(pend) + 7) // 8
                for fc in range(8):
                    batch = pend[fc * per:(fc + 1) * per]
                    slots = []
                    for (gi, i) in batch:
                        slot = cnt["sel"] % NSEL
                        cnt["sel"] += 1
                        slots.append(slot)
                        sel_op(e + 1, gi, i, slot)
                    for gi, g in enumerate(groups):
                        cap = g["cap"]
                        for j in range(2):
                            po = pGU.b(j)[:, j, 0:cap] if gi == 0 else pC[:, j * 64:j * 64 + cap]
                            for k in range(8):
                                K.mm(po, wg.b(s * 2 + j)[:, s, j, k, fc * 128:(fc + 1) * 128], g["XT"].b(s)[:, s, k, 0:cap],
                                     start=(k == 0), stop=(k == 7))
                    for (gi, i), slot in zip(batch, slots):
                        idx_mm(e + 1, gi, i, slot)
                    for gi, g in enumerate(groups):
                        cap = g["cap"]
                        pg = pGU.b(0)[:, 0, 0:cap] if gi == 0 else pC[:, 0:cap]
                        pu = pGU.b(1)[:, 1, 0:cap] if gi == 0 else pC[:, 64:64 + cap]
                        K.act(sgl[:, 0:cap], pg, AF.Silu)
                        K.tt("dve", g["hid"].b(fc)[:, fc, 0:cap], sgl[:, 0:cap], pu, ALU.mult)

            def down(e):
                s = e % 2
                nsc = 0
                prevb = [V(None, b_) for b_ in scb[(e + 1) % 2]]
                for g in groups:
                    ncj, cwj = g["ncj"], g["cwj"]
                    g2b = MODv(g["r"], 5)
                    for jc in range(ncj):
                        ys = cnt["y"] % 2
                        cnt["y"] += 1
                        for hf_ in range(2):
                            for fc in range(8):
                                K.mm(pY.b(hf_)[0:cwj, hf_, :], g["hid"].b(fc)[:, fc, jc * 128:jc * 128 + cwj],
                                     wd[:, fc, hf_ * 512:(hf_ + 1) * 512], start=(fc == 0), stop=(fc == 7))
                            K.stt(ysb.b(ys)[0:cwj, ys, hf_ * 512:(hf_ + 1) * 512], pY.b(hf_)[0:cwj, hf_, :],
                                  g["aff"].b(s)[0:cwj, s, jc:jc + 1], g2b[0:cwj, hf_ * 512:(hf_ + 1) * 512], ALU.mult, ALU.mult)
                        iv = g["idxs"].b(s)[0:cwj, s, jc:jc + 1]
                        sv_ = V(stream_rows.ap, scb[e % 2][nsc])
                        nsc += 1
                        K.dma("pool", sv_, ysb.b(ys)[0:cwj, ys, :], extra_in=[iv] + prevb,
                              fn=lambda E, ys=ys, iv=iv, cwj=cwj: E.indirect_dma_start(
                                  out=stream_rows.ap, out_offset=bass.IndirectOffsetOnAxis(ap=iv.ap, axis=0),
                                  in_=ysb.ap[0:cwj, ys, :], in_offset=None, compute_op=ALU.add))

            for (gi, i) in sel_list(0):
                slot = cnt["sel"] % NSEL
                cnt["sel"] += 1
                sel_op(0, gi, i, slot)
                idx_mm(0, gi, i, slot)
            prep_fin(0)
            prep_xt(0)
            for e in range(NE):
                load_wd(e)
                if e + 1 < NE:
                    load_w(e + 1)
                gate_up(e, sel_list(e + 1) if e + 1 < NE else [])
                if e + 1 < NE:
                    prep_fin(e + 1)
                down(e)
                if e + 1 < NE:
                    prep_xt(e + 1)
            K.pop()

        glat = dict(row0=TC, ntk=TL // 128, cap=2 * TL // NE, r=0, srow0=(TC if need_ctx else 0))
        if need_ctx:
            gctx = dict(row0=0, ntk=TC // 128, cap=2 * TC // NE, r=1, srow0=0)
            moe([glat, gctx], S1[:, :])
        else:
            moe([glat], yout[:, :])
        if stage_done(f"moe{L}"):
            break

    K.barrier(["sp"])
    return nc


def _prep_inputs(inputs):
    f = lambda a: np.ascontiguousarray(np.asarray(a, dtype=np.float32))
    x, c, ctx, c_ctx = f(inputs["x"]), f(inputs["c"]), f(inputs["ctx"]), f(inputs["c_ctx"])
    cosT, sinT = host_rope()
    consts = host_consts()
    shared = dict(consts=consts, cosT=cosT, sinT=sinT)
    for n in ("mod_w", "mod_b", "norm1_g", "norm2_g", "w_in", "w_out", "att_q_norm_g", "att_k_norm_g", "att_subln_g",
              "ret_norm_g", "lru_conv_w", "lru_conv_b", "lru_gate_w", "lru_lambda", "lru_norm_g", "router_w",
              "exp_w_gate", "exp_w_up", "exp_w_down"):
        shared[n] = f(inputs[n])
    shared["att_lambda"] = f(inputs["att_lambda"]).reshape(DEPTH, 256)
    shared["ret_log_decay"] = f(inputs["ret_log_decay"]).reshape(DEPTH, 8)
    shared["lru_gate_b"] = f(inputs["lru_gate_b"]).reshape(DEPTH, 4, 256)
    maps = []
    for b in range(x.shape[0]):
        m = dict(shared)
        m["xin"] = np.ascontiguousarray(np.concatenate([ctx[b], x[b]], axis=0))
        m["cvec"] = np.ascontiguousarray(np.stack([c[b], c_ctx], axis=1))
        maps.append(m)
    return maps


_NC_CACHE = {}


def kernel(**inputs):
    maps = _prep_inputs(inputs)
    if "nc" not in _NC_CACHE:
        _NC_CACHE["nc"] = build()
    nc = _NC_CACHE["nc"]
    res = run_bass_kernel_spmd(nc, maps, core_ids=list(range(N_CORES)))
    return np.stack([np.asarray(r["y"], dtype=np.float32) for r in res.results], axis=0)
```

```python
import math
import threading
from contextlib import ExitStack
import numpy as np
import concourse.bass as bass
import concourse.mybir as mybir
from concourse.bass_utils import run_bass_kernel_spmd

F32 = mybir.dt.float32
BF16 = mybir.dt.bfloat16
I32 = mybir.dt.int32
ALU = mybir.AluOpType
AF = mybir.ActivationFunctionType
AX = mybir.AxisListType

D = 1024
TC = 256
TL = 4096
TT = TC + TL
NT = TT // 128
DEPTH = 2
EPS = 1e-6
NE = 16
N_CORES = 8


class Buf:
    __slots__ = ("w", "rs")

    def __init__(self):
        self.w = None
        self.rs = {}


class V:
    def __init__(self, ap, buf):
        self.ap = ap
        self.buf = buf

    def __getitem__(self, k):
        return V(self.ap[k], self.buf)

    def m(self, f):
        return V(f(self.ap), self.buf)


class T:
    def __init__(self, ap, nbuf=1):
        self.ap = ap
        self.bufs = [Buf() for _ in range(nbuf)]

    def __getitem__(self, k):
        return V(self.ap[k], self.bufs[0])

    def b(self, i):
        return V(self.ap, self.bufs[i % len(self.bufs)])


class SideRunner:
    def __init__(self, fn):
        self.go = threading.Semaphore(0)
        self.back = threading.Semaphore(0)
        self.budget = 0
        self.finished = False
        self.exc = None
        self.nops = 0

        def run():
            self.go.acquire()
            try:
                fn()
            except BaseException as e:
                self.exc = e
            self.finished = True
            self.back.release()
        self.thread = threading.Thread(target=run, daemon=True)
        self.thread.start()

    def tick(self):
        while self.budget <= 0:
            self.back.release()
            self.go.acquire()
        self.budget -= 1
        self.nops += 1

    def step(self, n):
        if self.finished:
            return
        self.budget = n
        self.go.release()
        self.back.acquire()
        if self.exc is not None:
            raise self.exc

    def flush(self):
        while not self.finished:
            self.step(10 ** 9)
        if self.exc is not None:
            raise self.exc


class Kern:
    CE = ("pe", "act", "dve", "pool")
    side = None

    def _gate(self):
        r = self.runners.get(threading.current_thread())
        if r is not None:
            r.tick()

    def _costep(self):
        r = self.co.get(threading.current_thread())
        if r is not None:
            r.step(1)

    def spawn(self, fn):
        r = SideRunner(fn)
        self.runners[r.thread] = r
        return r

    def corun(self, fn_a, fn_b):
        r = self.spawn(fn_b)
        me = threading.current_thread()
        self.co[me] = r
        try:
            fn_a()
        finally:
            del self.co[me]
        r.flush()
        del self.runners[r.thread]

    def __init__(self, nc):
        self.nc = nc
        self.eng = dict(pe=nc.tensor, act=nc.scalar, dve=nc.vector, pool=nc.gpsimd, sp=nc.sync)
        self.sems = []
        self.cur = {}
        self.cnt = {}
        self.seen = {e: {} for e in self.eng}
        self.nsem = 0
        for e in self.CE:
            self._new_eng_sem(e)
        self.dpool = {}
        self.dnext = {}
        self.dcnt = {}
        for q, n in (("sp", 16), ("pool", 16), ("act", 6)):
            self.dpool[q] = [self._new_sem() for _ in range(n)]
            self.dnext[q] = 0
            for s in self.dpool[q]:
                self.dcnt[s] = 0
        self.runners = {}
        self.co = {}
        self.stacks = [ExitStack()]
        self.caches = [{}]
        self.uid = 0

    def _new_sem(self):
        h = self.nc.alloc_semaphore(name=f"s{self.nsem}")
        self.nsem += 1
        self.sems.append(h)
        return len(self.sems) - 1

    def _new_eng_sem(self, e):
        self.cur[e] = self._new_sem()
        self.cnt[e] = 0

    def _wait(self, e, ev):
        if ev is None:
            return
        s, v = ev
        if v <= 0 or self.seen[e].get(s, 0) >= v:
            return
        self.eng[e].wait_ge(self.sems[s], v)
        self.seen[e][s] = v

    def _deps(self, e, reads, writes):
        for b in reads:
            self._wait(e, b.w)
        for b in writes:
            self._wait(e, b.w)
            for s, v in list(b.rs.items()):
                self._wait(e, (s, v))

    def _post(self, ev, reads, writes):
        for b in reads:
            if b.rs.get(ev[0], 0) < ev[1]:
                b.rs[ev[0]] = ev[1]
        for b in writes:
            b.w = ev
            b.rs = {}

    @staticmethod
    def _bufs(vs):
        out = []
        for v in vs:
            if isinstance(v, V) and v.buf not in out:
                out.append(v.buf)
        return out

    def op(self, e, fn, outs, ins):
        self._gate()
        reads = self._bufs(ins)
        writes = self._bufs(outs)
        self._deps(e, reads, writes)
        inst = fn(self.eng[e])
        s = self.cur[e]
        self.cnt[e] += 1
        c = self.cnt[e]
        inst.then_inc(self.sems[s], 1)
        if e == "pe":
            self.seen[e][s] = c
        self._post((s, c), reads, writes)
        if c >= 30000:
            self._new_eng_sem(e)
        self._costep()

    def dma(self, q, out, in_, extra_in=(), fn=None, **kw):
        self._gate()
        reads = self._bufs([in_] + list(extra_in))
        writes = self._bufs([out])
        pool = self.dpool[q]
        si = pool[self.dnext[q] % len(pool)]
        self.dnext[q] += 1
        self._wait(q, (si, self.dcnt[si]))
        self._deps(q, reads, writes)
        if fn is None:
            inst = self.eng[q].dma_start(out=out.ap, in_=in_.ap, **kw)
        else:
            inst = fn(self.eng[q])
        self.dcnt[si] += 16
        inst.then_inc(self.sems[si], 16)
        self._post((si, self.dcnt[si]), reads, writes)
        self._costep()

    def barrier(self, engines=None):
        for e in (engines or list(self.eng)):
            for q in self.dpool:
                for si in self.dpool[q]:
                    self._wait(e, (si, self.dcnt[si]))
            for en in self.CE:
                if en != e or en != "pe":
                    self._wait(e, (self.cur[en], self.cnt[en]))

    def push(self):
        self.stacks.append(ExitStack())
        self.caches.append({})

    def pop(self):
        self.barrier()
        self.stacks.pop().close()
        self.caches.pop()

    def sb(self, shape, dtype, nbuf=1, name=None):
        self.uid += 1
        h = self.stacks[-1].enter_context(self.nc.sbuf_tensor(f"{name or 'sb'}_{self.uid}", list(shape), dtype))
        return T(h, nbuf)

    def ps(self, shape, dtype=F32, nbuf=1, name=None):
        self.uid += 1
        h = self.stacks[-1].enter_context(self.nc.psum_tensor(f"{name or 'ps'}_{self.uid}", list(shape), dtype))
        return T(h, nbuf)

    def dr(self, name, shape, dtype, kind=None, nbuf=1):
        if kind is None:
            t = self.nc.dram_tensor(name, list(shape), dtype)
        else:
            t = self.nc.dram_tensor(name, list(shape), dtype, kind=kind)
        return T(t.ap(), nbuf)

    @staticmethod
    def _a(x):
        return x.ap if isinstance(x, V) else x

    def act(self, out, in_, func, bias=None, scale=None, accum=None):
        kw = {}
        if bias is not None:
            kw["bias"] = self._a(bias)
        if scale is not None:
            kw["scale"] = self._a(scale)
        if accum is not None:
            kw["accum_out"] = accum.ap
        self.op("act", lambda E: E.activation(out=out.ap, in_=in_.ap, func=func, **kw),
                [out, accum], [in_, bias, scale])

    def tt(self, e, out, a, b, op):
        self.op(e, lambda E: E.tensor_tensor(out=out.ap, in0=a.ap, in1=b.ap, op=op), [out], [a, b])

    def ts(self, e, out, a, s1, s2=None, op0=ALU.mult, op1=None, accum=None):
        kw = {}
        if op1 is not None:
            kw["op1"] = op1
        if accum is not None:
            kw["accum_out"] = accum.ap
        self.op(e, lambda E: E.tensor_scalar(out=out.ap, in0=a.ap, scalar1=self._a(s1), scalar2=self._a(s2),
                                              op0=op0, **kw), [out, accum], [a, s1, s2])

    def stt(self, out, a, s, b, op0, op1):
        self.op("dve", lambda E: E.scalar_tensor_tensor(out=out.ap, in0=a.ap, scalar=self._a(s), in1=b.ap,
                                                       op0=op0, op1=op1), [out], [a, s, b])

    def copy(self, e, out, in_):
        if e == "act":
            self.op(e, lambda E: E.copy(out=out.ap, in_=in_.ap), [out], [in_])
        else:
            self.op(e, lambda E: E.tensor_copy(out=out.ap, in_=in_.ap), [out], [in_])

    def memset(self, e, out, val):
        self.op(e, lambda E: E.memset(out.ap, val), [out], [])

    def recip(self, out, in_):
        self.op("dve", lambda E: E.reciprocal(out=out.ap, in_=in_.ap), [out], [in_])

    def reduce(self, out, in_, op, axis=AX.X):
        self.op("dve", lambda E: E.tensor_reduce(out=out.ap, in_=in_.ap, axis=axis, op=op), [out], [in_])

    def scan(self, out, d0, d1, init, op0=ALU.mult, op1=ALU.add):
        self.op("dve", lambda E: E.tensor_tensor_scan(out=out.ap, data0=d0.ap, data1=d1.ap, initial=self._a(init),
                                                     op0=op0, op1=op1), [out], [d0, d1, init])

    def mm(self, out, lhsT, rhs, start=True, stop=True):
        self.op("pe", lambda E: E.matmul(out.ap, lhsT.ap, rhs.ap, start=start, stop=stop), [out], [lhsT, rhs])

    def tr(self, out, in_, ident):
        self.op("pe", lambda E: E.transpose(out.ap, in_.ap, ident.ap), [out], [in_, ident])

    def rstd(self, out, ss, n, tmp):
        self.act(tmp, ss, AF.Ln, bias=self.epsc[: ss.ap.shape[0]], scale=1.0 / n)
        self.act(out, tmp, AF.Exp, scale=-0.5)

    def sigmoid(self, out, in_, tmp, nbias=None, scale=1.0):
        if nbias is None:
            self.act(tmp, in_, AF.Exp, scale=-scale)
        else:
            self.act(tmp, in_, AF.Exp, bias=nbias, scale=-scale)
        self.act(tmp, tmp, AF.Ln, bias=self.onec[: in_.ap.shape[0]])
        self.act(out, tmp, AF.Exp, scale=-1.0)


FM_ROWS = [0, 128, 256, 384, 512, 640, 768, 896, 1536, 1664, 1792, 1920, 2560, 2688, 2816, 2944]
NCONST = 128 + 128 + 128 + 512 + 64 + 1 + 1 + 128
import os
SIDE_N, SIDE_A, SIDE_B = [int(v) for v in os.environ.get('SIDE_CFG', '1,1,1').split(',')]


def host_consts():
    c = np.zeros((128, NCONST), np.float32)
    c[:, 0:128] = np.eye(128, dtype=np.float32)
    for i in range(128):
        d = i % 64
        p = d + 16 if (d % 32) < 16 else d - 16
        c[(i // 64) * 64 + p, 128 + i] = 1.0
    c[0:64, 256:320] = 1.0
    c[64:128, 320:384] = 1.0
    c[:, 384:896] = np.arange(512, dtype=np.float32)[None, :]
    c[:, 896:960] = np.arange(64, dtype=np.float32)[None, :]
    c[:, 960] = np.arange(128, dtype=np.float32)
    c[:, 961] = (np.arange(128) // 16 + 1).astype(np.float32) / 8.0
    pp = np.arange(128)
    c[:, 962:1090] = (pp[:, None] % 16 == pp[None, :] % 16).astype(np.float32)
    return c


def host_rope():
    rows = TL // 64
    row = np.repeat(np.arange(rows), 64).astype(np.float32)
    col = np.tile(np.arange(64), rows).astype(np.float32)
    inv = (1.0 / (np.float32(10000.0) ** (np.arange(0, 32, 2, dtype=np.float32) / np.float32(32)))).astype(np.float32)
    ang = np.concatenate([row[:, None] * inv, col[:, None] * inv], axis=-1).astype(np.float32)
    cos = np.cos(ang).astype(np.float32)
    sin = np.sin(ang).astype(np.float32)
    cosT = np.ones((128, TT), np.float32)
    sinT = np.zeros((128, TT), np.float32)
    for r in range(128):
        d = r % 64
        f = d % 16
        a = f if d < 32 else 16 + f
        cosT[r, TC:] = cos[:, a]
        sinT[r, TC:] = -sin[:, a] if (d % 32) < 16 else sin[:, a]
    return cosT, sinT


def build(stop_after=None, dbg=()):
    nc = bass.Bass("TRN2", target_bir_lowering=False)
    K = Kern(nc)

    def dk(name):
        return "ExternalOutput" if name in dbg else None

    xin = K.dr("xin", [TT, D], F32, "ExternalInput")
    cvec = K.dr("cvec", [D, 2], F32, "ExternalInput")
    consts = K.dr("consts", [128, NCONST], F32, "ExternalInput")
    cosD = K.dr("cosT", [128, TT], F32, "ExternalInput")
    sinD = K.dr("sinT", [128, TT], F32, "ExternalInput")
    W = {}
    for n, shp in (("mod_w", [DEPTH, D, 6 * D]), ("mod_b", [DEPTH, 6 * D]), ("norm1_g", [DEPTH, D]), ("norm2_g", [DEPTH, D]),
                   ("w_in", [DEPTH, D, 3072]), ("w_out", [DEPTH, D, D]), ("att_q_norm_g", [DEPTH, 64]),
                   ("att_k_norm_g", [DEPTH, 64]), ("att_lambda", [DEPTH, 256]), ("att_subln_g", [DEPTH, 128]),
                   ("ret_log_decay", [DEPTH, 8]), ("ret_norm_g", [DEPTH, 64]), ("lru_conv_w", [DEPTH, 4, 256]),
                   ("lru_conv_b", [DEPTH, 256]), ("lru_gate_w", [DEPTH, 2, 2, 4, 64, 64]), ("lru_gate_b", [DEPTH, 4, 256]),
                   ("lru_lambda", [DEPTH, 2, 256]), ("lru_norm_g", [DEPTH, 256]), ("router_w", [DEPTH, D, NE]),
                   ("exp_w_gate", [DEPTH, NE, D, D]), ("exp_w_up", [DEPTH, NE, D, D]), ("exp_w_down", [DEPTH, NE, D, D])):
        W[n] = K.dr(n, shp, F32, "ExternalInput")
    yout = K.dr("y", [TL, D], F32, "ExternalOutput", nbuf=NT)
    S1 = K.dr("S1", [TT, D], F32, dk("S1"), nbuf=NT)
    QT = K.dr("QT", [1024, TT], BF16, dk("QT"), nbuf=64)
    KT = K.dr("KT", [1024, TT], BF16, dk("KT"), nbuf=64)
    Vd = K.dr("Vd", [TT, 512], BF16, dk("Vd"), nbuf=NT)
    RQT = K.dr("RQT", [256, TT], BF16, dk("RQT"), nbuf=32)
    RKT = K.dr("RKT", [256, TT], BF16, dk("RKT"), nbuf=32)
    RKd = K.dr("RKd", [TT, 256], BF16, dk("RKd"), nbuf=NT)
    RVd = K.dr("RVd", [TT, 256], BF16, dk("RVd"), nbuf=NT)
    SGd = K.dr("SGd", [TT, 256], F32, dk("SGd"), nbuf=NT)
    XUd = K.dr("XUd", [256, TT], F32, dk("XUd"), nbuf=32)
    GUd = K.dr("GUd", [256, TT], F32, dk("GUd"), nbuf=32)
    MIXT = K.dr("MIXT", [1024, TT], BF16, dk("MIXT"), nbuf=64)
    H2d = K.dr("H2d", [TT, D], BF16, dk("H2d"), nbuf=NT)
    PRd = K.dr("PRd", [TT, NE], F32, dk("PRd"), nbuf=NT) if "PRd" in dbg else None

    cst = K.sb([128, NCONST], F32, name="cst")
    K.dma("sp", cst[:, :], consts[:, :])
    ident = cst[:, 0:128]
    permc = cst[:, 128:256]
    blk1 = cst[:, 256:384]
    iota = cst[:, 384:896]
    tidx = cst[:, 896:960]
    pidx = cst[:, 960:961]
    cfrac = cst[:, 961:962]
    sameE = cst[:, 962:1090]
    PTd = K.dr("PTd", [NE, TL], F32)
    identb_t = K.sb([128, 128], BF16, name="identb")
    K.copy("dve", identb_t[:, :], ident)
    identb = identb_t[:, :]
    ones_t = K.sb([128, 128], F32, name="ones")
    K.memset("dve", ones_t[:, :], 1.0)
    ones = ones_t[:, :]
    onesb_t = K.sb([128, 512], BF16, name="onesb")
    K.memset("dve", onesb_t[:, :], 1.0)
    onesb = onesb_t[:, :]
    epsc_t = K.sb([128, 1], F32, name="epsc")
    K.memset("dve", epsc_t[:, :], EPS)
    K.epsc = epsc_t[:, :]
    onec_t = K.sb([128, 1], F32, name="onec")
    K.memset("dve", onec_t[:, :], 1.0)
    K.onec = onec_t[:, :]
    csil = K.sb([128, 8, 2], F32, name="csil")
    K.dma("sp", csil[:, :, :], cvec.b(0).m(lambda a: a.rearrange("(k p) r -> p k r", p=128)))
    K.act(csil[:, :, :], csil[:, :, :], AF.Silu)
    crep = K.sb([128, 2, 8, 128], BF16, name="crep")
    for r in range(2):
        K.copy("dve", crep[:, r, :, :], csil[:, :, r:r + 1].m(lambda a: a.to_broadcast([128, 8, 128])))
    MODS = K.dr("MODS", [DEPTH, 2, 6 * D], F32, dk("MODS"), nbuf=DEPTH)
    PROBS = K.sb([128, NT, NE], F32, name="PROBS")
    PROBSp = T(PROBS.ap, nbuf=NT)

    def col(dst, src_ap):
        K.dma("sp", dst, V(src_ap.rearrange("(p o) -> p o", o=1), Buf()))

    def bc(dst, src_ap, n=128):
        K.dma("sp", dst, V(src_ap.partition_broadcast(n), Buf()))

    def stage_done(name):
        return stop_after == name

    for L in range(DEPTH):
        need_ctx = L < DEPTH - 1
        Xsrc = xin if L == 0 else S1
        lam_init = 0.8 - 0.6 * math.exp(-0.3 * L)

        def xrows(t0, n):
            if need_ctx:
                return S1[t0:t0 + n, :]
            return yout[t0 - TC:t0 - TC + n, :]

        K.push()
        winb = K.sb([128, 8, 3072], BF16, name="winb", nbuf=1)
        for cgi in range(3):
            K.dma("pool", winb[:, :, cgi * 1024:(cgi + 1) * 1024],
                  W["w_in"].b(0).m(lambda a: a[L, :, cgi * 1024:(cgi + 1) * 1024].rearrange("(k p) c -> p k c", p=128)), max_dma_last_dim=4096)
        K.push()
        modb = K.sb([128, 6 * D], F32, name="modb")
        bc(modb[:, :], W["mod_b"].ap[L])
        n1g = K.sb([128, D], F32, name="n1g")
        bc(n1g[:, :], W["norm1_g"].ap[L])
        n2g = K.sb([128, D], F32, name="n2g")
        bc(n2g[:, :], W["norm2_g"].ap[L])
        MOD = K.sb([128, 2, 6 * D], F32, name="MOD", nbuf=2)
        mw = K.sb([128, 2, 8, 512], BF16, name="mw", nbuf=2)
        pm = K.ps([128, 2, 512], F32, name="pm", nbuf=2)
        for cg in range(12):
            s = cg % 2
            K.dma("pool", mw.b(s)[:, s, :, :],
                  W["mod_w"].b(0).m(lambda a: a[L, :, cg * 512:(cg + 1) * 512].rearrange("(k p) c -> p k c", p=128)), max_dma_last_dim=2048)
            for r in range(2):
                for k in range(8):
                    K.mm(pm.b(r)[:, r, :], crep[:, r, k, :], mw.b(s)[:, s, k, :], start=(k == 0), stop=(k == 7))
                K.tt("dve", MOD.b(r)[:, r, cg * 512:(cg + 1) * 512], pm.b(r)[:, r, :], modb[:, cg * 512:(cg + 1) * 512], ALU.add)
        for r in range(2):
            K.stt(MOD.b(r)[:, r, 1024:2048], MOD.b(r)[:, r, 1024:2048], 1.0, n1g[:, :], ALU.add, ALU.mult)
            K.stt(MOD.b(r)[:, r, 4096:5120], MOD.b(r)[:, r, 4096:5120], 1.0, n2g[:, :], ALU.add, ALU.mult)
        for r in range(2):
            K.dma("sp", MODS.b(L).m(lambda a: a[L, r:r + 1, :]), MOD.b(r)[0:1, r, :])
        K.pop()
        if stage_done(f"mod{L}"):
            break

        def MODv(r, j):
            for cch in reversed(K.caches):
                if ("mod", r, j) in cch:
                    return cch[("mod", r, j)]
            t_ = K.sb([128, D], F32, name="modv")
            K.dma("sp", t_[:, :], MODS.b(L).m(lambda a: a[L, r, j * 1024:(j + 1) * 1024].partition_broadcast(128)))
            K.caches[-1][("mod", r, j)] = t_[:, :]
            return t_[:, :]

        cosS = K.sb([128, 512], F32, name="cosS")
        sinS = K.sb([128, 512], F32, name="sinS")
        gq = K.sb([128, 2], F32, name="gq")
        for j, nm in enumerate(("att_q_norm_g", "att_k_norm_g")):
            for hh in range(2):
                col(gq[hh * 64:(hh + 1) * 64, j:j + 1], W[nm].ap[L])
        permg = K.sb([128, 2, 128], F32, name="permg")
        for j in range(2):
            K.ts("dve", permg[:, j, :], permc, gq[:, j:j + 1], None, ALU.mult)
        xt = K.sb([128, 2, D], F32, name="xt", nbuf=2)
        st = K.sb([128, 8], F32, name="st", nbuf=4)
        tmpn = K.sb([128, D], F32, name="tmpn", nbuf=1)
        hb2 = K.sb([128, 2, 4, D], BF16, name="hb", nbuf=8)
        hT2 = K.sb([128, 2, 8, 512], BF16, name="hT", nbuf=2)
        pT = K.ps([128, 2, 512], F32, name="pT", nbuf=2)
        pF = K.ps([128, 2, 512], F32, name="pF", nbuf=2)
        pF2 = K.ps([128, 2, 512], F32, name="pF2", nbuf=2)
        pS = K.ps([128, 512], F32, name="pS")
        pR = K.ps([128, 512], F32, name="pR")
        raw = K.sb([128, 512], F32, name="raw")
        sq = K.sb([128, 512], F32, name="sq")
        t1 = K.sb([128, 512], F32, name="t1")
        t2 = K.sb([128, 512], F32, name="t2")
        rs = K.sb([128, 512], F32, name="rs")
        ob = K.sb([128, 2, 512], BF16, name="ob", nbuf=2)
        of = K.sb([128, 2, 512], F32, name="of", nbuf=2)
        obt = K.sb([128, 2, 1024], BF16, name="obt", nbuf=2)
        sgt = K.sb([128, 2, 256], F32, name="sgt", nbuf=2)
        tmB = K.sb([128, 256], F32, name="tmB")
        blocks = [(0, TC, 1)] + [(TC + 512 * i, 512, 0) for i in range(8)]
        for r_ in (0, 1):
            MODv(r_, 0)
            MODv(r_, 1)
        cnts = {"fm": 0, "tm": 0}

        def norm_part(bi):
            (t0, Wd, r) = blocks[bi]
            par = bi % 2
            nti = Wd // 128
            for i in range(nti):
                xv = xt.b(i % 2)[:, i % 2, :]
                K.dma("sp", xv, Xsrc.b(0)[t0 + i * 128:t0 + (i + 1) * 128, :])
                sv = st.b(i)
                K.act(tmpn[:, :], xv, AF.Square, accum=sv[:, 0:1])
                K.rstd(sv[:, 2:3], sv[:, 0:1], D, sv[:, 1:2])
                K.stt(tmpn[:, :], xv, sv[:, 2:3], MODv(r, 1), ALU.mult, ALU.mult)
                K.tt("pool", hb2.b(par * 4 + i)[:, par, i, :], tmpn[:, :], MODv(r, 0), ALU.add)
            for k in range(8):
                for i in range(nti):
                    K.mm(pT.b(k)[:, k % 2, i * 128:(i + 1) * 128], hb2.b(par * 4 + i)[:, par, i, k * 128:(k + 1) * 128], identb)
                K.copy("act", hT2.b(par)[:, par, k, 0:Wd], pT.b(k)[:, k % 2, 0:Wd])

        def fm_part(bi):
            (t0, Wd, r) = blocks[bi]
            par = bi % 2
            K.dma("sp", cosS[:, 0:Wd], cosD[:, t0:t0 + Wd])
            K.dma("sp", sinS[:, 0:Wd], sinD[:, t0:t0 + Wd])
            for rc, c0 in enumerate(FM_ROWS):
                s = cnts["fm"] % 2
                cnts["fm"] += 1
                cf = cnts["fm"]
                pf = pF.b(s)[:, s, 0:Wd]
                for k in range(8):
                    K.mm(pf, winb[:, k, c0:c0 + 128], hT2.b(par)[:, par, k, 0:Wd], start=(k == 0), stop=(k == 7))
                if rc < 8:
                    j = 0 if rc < 4 else 1
                    K.copy("act", raw[:, 0:Wd], pf)
                    K.tt("pool", sq[:, 0:Wd], raw[:, 0:Wd], raw[:, 0:Wd], ALU.mult)
                    K.mm(pS[:, 0:Wd], blk1, sq[:, 0:Wd])
                    K.mm(pR[:, 0:Wd], permg[:, j, :], raw[:, 0:Wd])
                    K.rstd(rs[:, 0:Wd], pS[:, 0:Wd], 64, sq[:, 0:Wd])
                    K.stt(t1[:, 0:Wd], raw[:, 0:Wd], gq[:, j:j + 1], cosS[:, 0:Wd], ALU.mult, ALU.mult)
                    K.tt("dve", t2[:, 0:Wd], pR[:, 0:Wd], sinS[:, 0:Wd], ALU.mult)
                    K.tt("pool", t1[:, 0:Wd], t1[:, 0:Wd], t2[:, 0:Wd], ALU.add)
                    K.tt("dve", ob.b(s)[:, s, 0:Wd], t1[:, 0:Wd], rs[:, 0:Wd], ALU.mult)
                    dst = (QT if j == 0 else KT)
                    rr = (rc % 4) * 128
                    K.dma("sp", dst.b(cf)[rr:rr + 128, t0:t0 + Wd], ob.b(s)[:, s, 0:Wd])
                elif rc < 12:
                    j = (rc - 8) // 2
                    K.act(ob.b(s)[:, s, 0:Wd], pf, AF.Copy, scale=(1.0 if j == 0 else 0.125))
                    dst = RQT if j == 0 else RKT
                    rr = ((rc - 8) % 2) * 128
                    K.dma("sp", dst.b(cf)[rr:rr + 128, t0:t0 + Wd], ob.b(s)[:, s, 0:Wd])
                else:
                    j = (rc - 12) // 2
                    K.copy("act", of.b(s)[:, s, 0:Wd], pf)
                    dst = XUd if j == 0 else GUd
                    rr = ((rc - 12) % 2) * 128
                    K.dma("sp", dst.b(cf)[rr:rr + 128, t0:t0 + Wd], of.b(s)[:, s, 0:Wd])

        def tm_part(bi):
            (t0, Wd, r) = blocks[bi]
            par = bi % 2
            for i in range(Wd // 128):
                tok = t0 + i * 128
                ti = tok // 128
                s = cnts["tm"] % 2
                cnts["tm"] += 1
                for g, (c0, cw) in enumerate(((1024, 512), (1792, 512), (2304, 256))):
                    pf = pF2.b(g)[:, g % 2, 0:cw]
                    for k in range(8):
                        K.mm(pf, hT2.b(par)[:, par, k, i * 128:(i + 1) * 128], winb[:, k, c0:c0 + cw], start=(k == 0), stop=(k == 7))
                    if g == 0:
                        K.copy("act", obt.b(s)[:, s, 0:512], pf)
                        K.dma("sp", Vd.b(ti)[tok:tok + 128, :], obt.b(s)[:, s, 0:512])
                    elif g == 1:
                        K.act(obt.b(s)[:, s, 512:768], pF2.b(g)[:, g % 2, 0:256], AF.Copy, scale=0.125)
                        K.copy("dve", obt.b(s)[:, s, 768:1024], pF2.b(g)[:, g % 2, 256:512])
                        K.dma("sp", RKd.b(ti)[tok:tok + 128, :], obt.b(s)[:, s, 512:768])
                        K.dma("sp", RVd.b(ti)[tok:tok + 128, :], obt.b(s)[:, s, 768:1024])
                    else:
                        K.sigmoid(sgt.b(s)[:, s, :], pf, tmB[:, 0:256])
                        K.tt("dve", sgt.b(s)[:, s, :], sgt.b(s)[:, s, :], pf, ALU.mult)
                        K.dma("sp", SGd.b(ti)[tok:tok + 128, :], sgt.b(s)[:, s, :])

        def tm_and_next(bi):
            tm_part(bi)
            if bi + 1 < len(blocks):
                norm_part(bi + 1)

        norm_part(0)
        for bi in range(len(blocks)):
            K.corun(lambda: fm_part(bi), lambda: tm_and_next(bi))
        K.pop()
        if stage_done(f"proj{L}"):
            break

        def side_fn():
            K.push()
            lgr = K.sb([128, 8], F32, name="lgr")
            bc(lgr[:, :], W["ret_log_decay"].ap[L])
            lgc = K.sb([128, 4], F32, name="lgc")
            for dr_ in range(2):
                for hp in range(2):
                    for e in range(2):
                        hd = dr_ * 4 + hp * 2 + e
                        bc(lgc[e * 64:(e + 1) * 64, dr_ * 2 + hp:dr_ * 2 + hp + 1], W["ret_log_decay"].ap[L, hd:hd + 1], n=64)
            rng = K.sb([128, 64], F32, name="rng")
            bc(rng[:, :], W["ret_norm_g"].ap[L])
            pcol = K.sb([128, 4], F32, name="pcol")
            K.copy("dve", pcol[:, 0:1], pidx)
            K.ts("dve", pcol[:, 1:2], pidx, -1.0, 127.0, ALU.mult, ALU.add)
            inner = K.sb([128, 2, 4], F32, name="inner")
            K.act(inner[:, 0, :], lgr[:, 0:4], AF.Exp, scale=pcol[:, 1:2])
            K.act(inner[:, 1, :], lgr[:, 4:8], AF.Exp, scale=pcol[:, 0:1])
            cdt = K.sb([128, 2, 4], F32, name="cdt")
            K.act(cdt[:, :, :].m(lambda a: a.rearrange("p a b -> p (a b)")), lgr[:, :], AF.Exp, scale=128.0)
            crossT = K.sb([128, 2, 2, 128], F32, name="crossT")
            jrow = K.sb([128, 2, 128], F32, name="jrow")
            K.ts("dve", jrow[:, 0, :], iota[:, 0:128], 1.0, None, ALU.add)
            K.ts("dve", jrow[:, 1, :], iota[:, 0:128], -1.0, 128.0, ALU.mult, ALU.add)
            for dr_ in range(2):
                for hp in range(2):
                    K.act(crossT[:, dr_, hp, :], jrow[:, dr_, :], AF.Exp, scale=lgc[:, dr_ * 2 + hp:dr_ * 2 + hp + 1])
            dif = K.sb([128, 128], F32, name="dif")
            K.ts("dve", dif[:, :], iota[:, 0:128], pidx, None, ALU.subtract)
            dpos = K.sb([128, 128], F32, name="dpos")
            dneg = K.sb([128, 128], F32, name="dneg")
            K.ts("dve", dpos[:, :], dif[:, :], 0.0, None, ALU.max)
            K.ts("dve", dneg[:, :], dif[:, :], -1.0, 0.0, ALU.mult, ALU.max)
            mge = K.sb([128, 128], F32, name="mge")
            mlt = K.sb([128, 128], F32, name="mlt")
            K.ts("dve", mge[:, :], dif[:, :], 0.0, None, ALU.is_ge)
            K.ts("dve", mlt[:, :], dif[:, :], 0.0, None, ALU.is_lt)
            Dfb = K.sb([128, 4, 128], F32, name="Dfb")
            dtmp = K.sb([128, 128], F32, name="dtmp")
            for h in range(4):
                K.act(dtmp[:, :], dpos[:, :], AF.Exp, scale=lgr[:, h:h + 1])
                K.tt("dve", Dfb[:, h, :], dtmp[:, :], mge[:, :], ALU.mult)
                K.act(dtmp[:, :], dneg[:, :], AF.Exp, scale=lgr[:, 4 + h:5 + h])
                K.tt("dve", dtmp[:, :], dtmp[:, :], mlt[:, :], ALU.mult)
                K.tt("dve", Dfb[:, h, :], Dfb[:, h, :], dtmp[:, :], ALU.add)
            cdtab = K.sb([128, 2, 256], F32, name="cdtab")
            for dr_ in range(2):
                K.copy("dve", cdtab[:, dr_, :].m(lambda a: a.rearrange("p (h v) -> p h v", v=64)),
                       cdt[:, dr_, :].m(lambda a: a.unsqueeze(2).to_broadcast([128, 4, 64])))
            rk = K.sb([128, 2, 256], BF16, name="rk", nbuf=2)
            rv = K.sb([128, 2, 256], BF16, name="rv", nbuf=2)
            rvi = K.sb([128, 2, 2, 256], BF16, name="rvi", nbuf=4)
            PB = K.ps([128, 2, 512], F32, name="PB", nbuf=6)
            KVb = K.sb([128, NT, 256], F32, name="KVb", nbuf=NT)
            SF = K.sb([128, 2, NT + 1, 256], BF16, name="SF", nbuf=2 * (NT + 1))
            s32 = K.sb([128, 2, 256], F32, name="s32", nbuf=2)
            stmp = K.sb([128, 256], F32, name="stmp")
            K.memset("dve", s32[:, 0, :], 0.0)
            K.memset("dve", s32.b(1)[:, 1, :], 0.0)

            def sfv(dr_, c):
                return SF.b(dr_ * (NT + 1) + c)[:, dr_, c, :]
            K.memset("pool", sfv(0, 0), 0.0)
            for c in range(NT):
                s = c % 2
                K.dma("sp", rk.b(s)[:, s, :], RKd.b(c)[c * 128:(c + 1) * 128, :])
                K.dma("sp", rv.b(s)[:, s, :], RVd.b(c)[c * 128:(c + 1) * 128, :])
                for dr_ in range(2):
                    K.tt("pool" if dr_ == 0 else "dve",
                         rvi.b(s * 2 + dr_)[:, s, dr_, :].m(lambda a: a.rearrange("p (h v) -> p h v", v=64)),
                         rv.b(s)[:, s, :].m(lambda a: a.rearrange("p (h v) -> p h v", v=64)),
                         inner[:, dr_, :].m(lambda a: a.unsqueeze(2).to_broadcast([128, 4, 64])), ALU.mult)
                    for hp in range(2):
                        K.mm(PB.b(dr_)[:, dr_, hp * 128:(hp + 1) * 128], rk.b(s)[:, s, hp * 128:(hp + 1) * 128],
                             rvi.b(s * 2 + dr_)[:, s, dr_, hp * 128:(hp + 1) * 128])
                K.tt("pool", stmp[:, :], s32[:, 0, :], cdtab[:, 0, :], ALU.mult)
                K.tt("dve", s32[:, 0, :], stmp[:, :], PB.b(0)[:, 0, 0:256], ALU.add)
                K.copy("act", sfv(0, c + 1), s32[:, 0, :])
                K.copy("act", KVb.b(c)[:, c, :], PB.b(1)[:, 1, 0:256])
            border = [1, 0] + list(range(NT - 1, 1, -1))
            K.memset("pool", sfv(1, border[0]), 0.0)
            for i in range(len(border) - 1):
                c, cn = border[i], border[i + 1]
                K.tt("pool", stmp[:, :], s32.b(1)[:, 1, :], cdtab[:, 1, :], ALU.mult)
                K.tt("dve", s32.b(1)[:, 1, :], stmp[:, :], KVb.b(c)[:, c, :], ALU.add)
                K.copy("act", sfv(1, cn), s32.b(1)[:, 1, :])
            rq = K.sb([128, 2, 2, 128], BF16, name="rq", nbuf=2)
            rkt = K.sb([128, 2, 2, 128], BF16, name="rkt", nbuf=2)
            sg = K.sb([128, 2, 256], F32, name="sg", nbuf=2)
            qc = K.sb([128, 2, 2, 2, 128], BF16, name="qc", nbuf=4)
            AT = K.sb([128, 2, 4, 128], BF16, name="AT", nbuf=2)
            osb = K.sb([128, 256], F32, name="osb")
            rsq2 = K.sb([128, 2, 256], F32, name="rsq", nbuf=2)
            rss2 = K.sb([128, 2, 12], F32, name="rss", nbuf=2)
            gs2 = K.sb([128, 2, 256], F32, name="gs", nbuf=2)
            ro12 = K.sb([128, 2, 256], F32, name="ro1", nbuf=2)
            osb2 = K.sb([128, 2, 256], F32, name="osb2", nbuf=2)
            rob = K.sb([128, 2, 256], BF16, name="rob", nbuf=2)
            rT = K.sb([128, 2, 256], BF16, name="rT", nbuf=2)
            chunks = list(range(NT)) if need_ctx else list(range(2, NT))
            def ret_chunk(c):
                s = c % 2
                rsq, rss, gs, ro1, osb = rsq2.b(s)[:, s], rss2.b(s)[:, s], gs2.b(s)[:, s], ro12.b(s)[:, s], osb2.b(s)[:, s]
                K.dma("sp", rq.b(s)[:, s, :, :], RQT.b(0).m(lambda a: a[:, c * 128:(c + 1) * 128].rearrange("(k p) t -> p k t", p=128)))
                K.dma("sp", rkt.b(s)[:, s, :, :], RKT.b(0).m(lambda a: a[:, c * 128:(c + 1) * 128].rearrange("(k p) t -> p k t", p=128)))
                K.dma("sp", rv.b(s)[:, s, :], RVd.b(c)[c * 128:(c + 1) * 128, :])
                K.dma("sp", sg.b(s)[:, s, :], SGd.b(c)[c * 128:(c + 1) * 128, :])
                for dr_ in range(2):
                    K.tt("dve" if dr_ == 0 else "pool", qc.b(s * 2 + dr_)[:, s, dr_, :, :], rq.b(s)[:, s, :, :], crossT[:, dr_, :, :], ALU.mult)
                for h in range(4):
                    e, hp = h % 2, h // 2
                    K.mm(PB.b(e)[:, e, hp * 128:(hp + 1) * 128], rkt.b(s)[e * 64:(e + 1) * 64, s, hp, :], rq.b(s)[e * 64:(e + 1) * 64, s, hp, :])
                for e in range(2):
                    K.tt("dve", AT.b(s)[:, s, :, :].m(lambda a: a.rearrange("p (hp e) n -> p hp e n", e=2)[:, :, e, :]),
                         PB.b(e)[:, e, 0:256].m(lambda a: a.rearrange("p (hp n) -> p hp n", n=128)),
                         Dfb[:, :, :].m(lambda a: a.rearrange("p (hp e) n -> p hp e n", e=2)[:, :, e, :]), ALU.mult)
                for h in range(4):
                    e, hp = h % 2, h // 2
                    po = PB.b(2 + e)[:, e, 256 + hp * 64:256 + (hp + 1) * 64]
                    K.mm(po, AT.b(s)[:, s, h, :], rv.b(s)[:, s, h * 64:(h + 1) * 64], start=True, stop=False)
                    for dr_ in range(2):
                        K.mm(po, qc.b(s * 2 + dr_)[e * 64:(e + 1) * 64, s, dr_, hp, :],
                             sfv(dr_, c)[e * 64:(e + 1) * 64, hp * 128 + e * 64:hp * 128 + (e + 1) * 64], start=False, stop=(dr_ == 1))
                for e in range(2):
                    K.copy("act", osb[:, :].m(lambda a: a.rearrange("p (hp e v) -> p hp e v", e=2, v=64)[:, :, e, :]),
                           PB.b(2 + e)[:, e, 256:384].m(lambda a: a.rearrange("p (hp v) -> p hp v", v=64)))
                K.act(rsq[:, :], osb[:, :], AF.Square)
                K.reduce(rss[:, 0:4], rsq[:, :].m(lambda a: a.rearrange("p (h v) -> p h v", v=64)), ALU.add)
                K.rstd(rss[:, 8:12], rss[:, 0:4], 64, rss[:, 4:8])
                K.tt("pool", gs[:, :].m(lambda a: a.rearrange("p (h v) -> p h v", v=64)),
                     sg.b(s)[:, s, :].m(lambda a: a.rearrange("p (h v) -> p h v", v=64)),
                     rng[:, :].m(lambda a: a.unsqueeze(1).to_broadcast([128, 4, 64])), ALU.mult)
                K.tt("dve", ro1[:, :].m(lambda a: a.rearrange("p (h v) -> p h v", v=64)),
                     osb[:, :].m(lambda a: a.rearrange("p (h v) -> p h v", v=64)),
                     rss[:, 8:12].m(lambda a: a.unsqueeze(2).to_broadcast([128, 4, 64])), ALU.mult)
                K.tt("pool", rob.b(s)[:, s, :], ro1[:, :], gs[:, :], ALU.mult)
                for j in range(2):
                    K.mm(PB.b(4 + j)[:, j, 384:512], rob.b(s)[:, s, j * 128:(j + 1) * 128], identb)
                for j in range(2):
                    K.copy("act", rT.b(s)[:, s, j * 128:(j + 1) * 128], PB.b(4 + j)[:, j, 384:512])
                for j in range(2):
                    K.dma("sp", MIXT.b(c * 2 + j)[512 + j * 128:512 + (j + 1) * 128, c * 128:(c + 1) * 128], rT.b(s)[:, s, j * 128:(j + 1) * 128])

            def run_par(par):
                for c in chunks:
                    if c % 2 == par:
                        ret_chunk(c)
            for c in chunks:
                ret_chunk(c)
            K.pop()

            K.push()
            cw = K.sb([128, 2, 4], F32, name="cw")
            cb = K.sb([128, 2], F32, name="cb")
            gb = K.sb([128, 4, 2], F32, name="gb")
            lam = K.sb([128, 4], F32, name="lam")
            lng = K.sb([128, 2], F32, name="lng")
            for c in range(2):
                for j in range(4):
                    col(cw[:, c, j:j + 1], W["lru_conv_w"].ap[L, j, c * 128:(c + 1) * 128])
                col(cb[:, c:c + 1], W["lru_conv_b"].ap[L, c * 128:(c + 1) * 128])
                col(lng[:, c:c + 1], W["lru_norm_g"].ap[L, c * 128:(c + 1) * 128])
                for dg in range(4):
                    col(gb[:, dg, c:c + 1], W["lru_gate_b"].ap[L, dg, c * 128:(c + 1) * 128])
                for dr_ in range(2):
                    col(lam[:, dr_ * 2 + c:dr_ * 2 + c + 1], W["lru_lambda"].ap[L, dr_, c * 128:(c + 1) * 128])
            wbd = K.sb([128, 8, 128], F32, name="wbd")
            K.memset("dve", wbd[:, :, :], 0.0)
            for dr_ in range(2):
                for g in range(2):
                    for c in range(2):
                        for e in range(2):
                            K.dma("sp", wbd[e * 64:(e + 1) * 64, (dr_ * 2 + g) * 2 + c, e * 64:(e + 1) * 64],
                                  W["lru_gate_w"].b(0).m(lambda a: a[L, dr_, g, 2 * c + e, :, :]))
            sp = K.sb([128, 8, 4], F32, name="sp")
            K.ts("dve", sp[:, 5, :], lam[:, :], -1.0, None, ALU.mult)
            K.tt("dve", sp[:, 0, :], lam[:, :], sp[:, 5, :], ALU.max)
            K.act(sp[:, 1, :], sp[:, 0, :], AF.Exp, scale=-1.0)
            K.ts("dve", sp[:, 2, :], sp[:, 1, :], 2.0, None, ALU.add)
            K.recip(sp[:, 2, :], sp[:, 2, :])
            K.tt("dve", sp[:, 2, :], sp[:, 2, :], sp[:, 1, :], ALU.mult)
            K.tt("dve", sp[:, 3, :], sp[:, 2, :], sp[:, 2, :], ALU.mult)
            K.memset("dve", sp[:, 4, :], 1.0 / 15.0)
            for n_ in (13, 11, 9, 7, 5, 3, 1):
                K.tt("dve", sp[:, 4, :], sp[:, 4, :], sp[:, 3, :], ALU.mult)
                K.ts("dve", sp[:, 4, :], sp[:, 4, :], 1.0 / n_, None, ALU.add)
            K.tt("dve", sp[:, 4, :], sp[:, 4, :], sp[:, 2, :], ALU.mult)
            K.ts("dve", sp[:, 5, :], lam[:, :], -1.0, 0.0, ALU.mult, ALU.max)
            K.stt(sp[:, 6, :], sp[:, 4, :], 2.0, sp[:, 5, :], ALU.mult, ALU.add)
            K.ts("dve", sp[:, 7, :], sp[:, 6, :], -8.0, None, ALU.mult)
            ccoef = sp[:, 7, :]
            PADL = TC + 3
            xu = K.sb([128, 2, TT + 6], F32, name="xu")
            K.memset("pool", xu[:, :, :], 0.0)
            for c in range(2):
                K.dma("sp", xu[:, c, 2:2 + TC], XUd.b(0)[c * 128:(c + 1) * 128, 0:TC])
                K.dma("sp", xu[:, c, PADL + 2:PADL + 2 + TL], XUd.b(0)[c * 128:(c + 1) * 128, TC:TT])
            u = K.sb([128, 2, TT], F32, name="u")
            for c in range(2):
                for (pb, t0, Ln) in ((2, 0, TC), (PADL + 2, TC, TL)):
                    e = "dve"
                    K.ts(e, u[:, c, t0:t0 + Ln], xu[:, c, pb - 2:pb - 2 + Ln], cw[:, c, 0:1], cb[:, c:c + 1], ALU.mult, ALU.add)
                    for j in range(1, 4):
                        K.stt(u[:, c, t0:t0 + Ln], xu[:, c, pb - 2 + j:pb - 2 + j + Ln], cw[:, c, j:j + 1], u[:, c, t0:t0 + Ln], ALU.mult, ALU.add)
            hf = xu
            pG = K.ps([128, 2, 512], F32, name="pG", nbuf=2)
            gr_2 = K.sb([128, 2, 512], F32, name="gr", nbuf=2)
            gtmp_2 = K.sb([128, 2, 512], F32, name="gtmp", nbuf=2)
            ngb = K.sb([128, 4, 2], F32, name="ngb")
            K.ts("dve", ngb[:, :, :], gb[:, :, :], -1.0, None, ALU.mult)
            gi_2 = K.sb([128, 2, 512], F32, name="gi", nbuf=2)
            ga_2 = K.sb([128, 2, 512], F32, name="ga", nbuf=2)
            gw_2 = K.sb([128, 2, 512], F32, name="gw", nbuf=2)
            gbv_2 = K.sb([128, 2, 512], F32, name="gbv", nbuf=2)
            ar_2 = K.sb([128, 2, 512], F32, name="ar", nbuf=2)
            br_2 = K.sb([128, 2, 512], F32, name="br", nbuf=2)
            hbr = K.sb([128, 2, 512], F32, name="hbr", nbuf=2)
            hst = K.sb([128, 2], F32, name="hst", nbuf=2)
            yc = K.sb([128, 2, 512], F32, name="yc", nbuf=2)
            gu = K.sb([128, 2, 512], F32, name="gu", nbuf=2)
            g2_2 = K.sb([128, 2, 512], F32, name="g2", nbuf=2)
            ysq = K.sb([128, 2, 512], F32, name="ysq", nbuf=2)
            yrs = K.sb([128, 512], F32, name="yrs")
            yob = K.sb([128, 2, 512], BF16, name="yob", nbuf=2)

            def tmps(c):
                return [t_.b(c)[:, c] for t_ in (gr_2, gtmp_2, gi_2, ga_2, gw_2, gbv_2, ar_2, br_2, g2_2)]

            def gates(dr_, c, t0, Wd):
                gr, gtmp, gi, ga, gw, gbv, ar, br, g2 = tmps(c)
                for g, dst in ((0, gr), (1, gi)):
                    K.mm(pG.b(c)[:, c, 0:Wd], wbd[:, (dr_ * 2 + g) * 2 + c, :], u[:, c, t0:t0 + Wd])
                    K.sigmoid(dst[:, 0:Wd], pG.b(c)[:, c, 0:Wd], gtmp[:, 0:Wd], nbias=ngb[:, dr_ * 2 + g, c:c + 1])
                K.act(ga[:, 0:Wd], gr[:, 0:Wd], AF.Exp, scale=ccoef[:, dr_ * 2 + c:dr_ * 2 + c + 1])
                K.tt("pool", gw[:, 0:Wd], ga[:, 0:Wd], ga[:, 0:Wd], ALU.mult)
                K.ts("dve", gw[:, 0:Wd], gw[:, 0:Wd], -1.0, 1.0, ALU.mult, ALU.add)
                K.act(gw[:, 0:Wd], gw[:, 0:Wd], AF.Ln)
                K.act(gw[:, 0:Wd], gw[:, 0:Wd], AF.Exp, scale=0.5)
                K.tt("pool", gbv[:, 0:Wd], gi[:, 0:Wd], u[:, c, t0:t0 + Wd], ALU.mult)
                K.tt("dve", gbv[:, 0:Wd], gbv[:, 0:Wd], gw[:, 0:Wd], ALU.mult)

            lblocks = [(0, TC)] + [(TC + 512 * i, 512) for i in range(8)]

            def fwd_chain(c):
                gr, gtmp, gi, ga, gw, gbv, ar, br, g2 = tmps(c)
                for bi, (t0, Wd) in enumerate(lblocks):
                    gates(0, c, t0, Wd)
                    init = 0.0 if bi == 0 else hf.b(0)[:, c, t0 - 1:t0]
                    K.scan(hf.b(0)[:, c, t0:t0 + Wd], ga[:, 0:Wd], gbv[:, 0:Wd], init)
            K.corun(lambda: fwd_chain(0), lambda: fwd_chain(1))
            bblocks = [lblocks[0]] + lblocks[:0:-1]
            nyo = 0

            def bwd_blk(c, bi, t0, Wd, emit):
                gr, gtmp, gi, ga, gw, gbv, ar, br, g2 = tmps(c)
                gates(1, c, t0, Wd)
                K.copy("dve", ar[:, 0:Wd], ga[:, 0:Wd].m(lambda a: a[:, ::-1]))
                K.copy("dve", br[:, 0:Wd], gbv[:, 0:Wd].m(lambda a: a[:, ::-1]))
                init = 0.0 if bi == 0 else hst.b(c)[:, c:c + 1]
                K.scan(hbr.b(c)[:, c, 0:Wd], ar[:, 0:Wd], br[:, 0:Wd], init)
                K.copy("pool", hst.b(c)[:, c:c + 1], hbr.b(c)[:, c, Wd - 1:Wd])
                if not emit:
                    return
                K.dma("sp", gu.b(c)[:, c, 0:Wd], GUd.b(0)[c * 128:(c + 1) * 128, t0:t0 + Wd])
                K.tt("dve", yc.b(c)[:, c, 0:Wd], hf.b(0)[:, c, t0:t0 + Wd], hbr.b(c)[:, c, 0:Wd].m(lambda a: a[:, ::-1]), ALU.add)
                guv = gu.b(c)[:, c, 0:Wd]
                K.tt("pool", g2[:, 0:Wd], guv, guv, ALU.mult)
                K.ts("dve", g2[:, 0:Wd], g2[:, 0:Wd], 0.044715, 1.0, ALU.mult, ALU.add)
                K.tt("pool", g2[:, 0:Wd], g2[:, 0:Wd], guv, ALU.mult)
                K.sigmoid(g2[:, 0:Wd], g2[:, 0:Wd], gtmp[:, 0:Wd], scale=2.0 * math.sqrt(2.0 / math.pi))
                K.tt("pool", g2[:, 0:Wd], g2[:, 0:Wd], guv, ALU.mult)
                K.tt("dve", yc.b(c)[:, c, 0:Wd], yc.b(c)[:, c, 0:Wd], g2[:, 0:Wd], ALU.mult)
                K.tt("pool", ysq.b(c)[:, c, 0:Wd], yc.b(c)[:, c, 0:Wd], yc.b(c)[:, c, 0:Wd], ALU.mult)

            for bi, (t0, Wd) in enumerate(bblocks):
                emit = need_ctx or t0 >= TC
                K.corun(lambda: bwd_blk(0, bi, t0, Wd, emit), lambda: bwd_blk(1, bi, t0, Wd, emit))
                if not emit:
                    continue
                for c in range(2):
                    K.mm(pG.b(0)[:, 0, 0:Wd], ones, ysq.b(c)[:, c, 0:Wd], start=(c == 0), stop=(c == 1))
                K.rstd(yrs[:, 0:Wd], pG.b(0)[:, 0, 0:Wd], 256, ysq.b(0)[:, 0, 0:Wd])
                for c in range(2):
                    K.stt(yob.b(c)[:, c, 0:Wd], yc.b(c)[:, c, 0:Wd], lng[:, c:c + 1], yrs[:, 0:Wd], ALU.mult, ALU.mult)
                    nyo += 1
                    K.dma("sp", MIXT.b(nyo)[768 + c * 128:768 + (c + 1) * 128, t0:t0 + Wd], yob.b(c)[:, c, 0:Wd])
            K.pop()


        K.push()
        lamb = K.sb([128, 256], F32, name="lamb")
        bc(lamb[:, :], W["att_lambda"].ap[L])
        lt = K.sb([128, 8], F32, name="lt")
        lj = K.sb([128, 64], F32, name="lj")
        K.tt("dve", lj[:, :], lamb[:, 0:64], lamb[:, 64:128], ALU.mult)
        K.reduce(lt[:, 0:1], lj[:, :], ALU.add)
        K.tt("dve", lj[:, :], lamb[:, 128:192], lamb[:, 192:256], ALU.mult)
        K.reduce(lt[:, 1:2], lj[:, :], ALU.add)
        K.act(lt[:, 2:4], lt[:, 0:2], AF.Exp)
        K.tt("dve", lt[:, 4:5], lt[:, 3:4], lt[:, 2:3], ALU.subtract)
        K.ts("dve", lt[:, 5:6], lt[:, 4:5], -lam_init, None, ALU.add)
        neglam = lt[:, 5:6]
        gsub = K.sb([128, 1], F32, name="gsub")
        col(gsub[:, :], W["att_subln_g"].ap[L])
        K.ts("dve", gsub[:, :], gsub[:, :], 1.0 - lam_init, None, ALU.mult)
        QTh = K.sb([128, TT], BF16, name="QTh")
        KTh = K.sb([128, TT], BF16, name="KTh")
        Vh = K.sb([128, NT, 128], BF16, name="Vh")
        pSs = K.ps([128, 4, 512], F32, name="pSs", nbuf=4)
        pO = K.ps([128, 2, 512], F32, name="pO", nbuf=2)
        PTt = K.sb([128, 4, 512], BF16, name="PTt", nbuf=4)
        rl = K.sb([128, 2, 512], F32, name="rl", nbuf=2)
        pacc = K.sb([128, 2, 512], F32, name="pacc", nbuf=2)
        o0 = K.sb([128, 512], F32, name="o0")
        o1 = K.sb([128, 512], F32, name="o1")
        osq = K.sb([128, 512], F32, name="osq")
        ors = K.sb([128, 512], F32, name="ors")
        aob = K.sb([128, 2, 512], BF16, name="aob", nbuf=2)
        qblocks = [(TC + 512 * i, 512, list(range(NT))) for i in range(8)]
        if need_ctx:
            qblocks = [(0, TC, [0, 1])] + qblocks
        nqb = 0
        K.side = K.spawn(side_fn)
        for h in range(4):
            K.dma("sp", QTh[:, :], QT.b(0)[h * 128:(h + 1) * 128, :])
            K.dma("sp", KTh[:, :], KT.b(0)[h * 128:(h + 1) * 128, :])
            K.dma("sp", Vh[:, :, :], Vd.b(0).m(lambda a: a[:, h * 128:(h + 1) * 128].rearrange("(c p) v -> p c v", p=128)))
            for (t0, Wd, kcs) in qblocks:
                def st_mm(ci):
                    kc = kcs[ci]
                    for m in range(2):
                        sl = (ci % 2) * 2 + m
                        K.mm(pSs.b(sl)[:, sl, 0:Wd], KTh[m * 64:(m + 1) * 64, kc * 128:(kc + 1) * 128],
                             QTh[m * 64:(m + 1) * 64, t0:t0 + Wd])
                st_mm(0)
                for ci, kc in enumerate(kcs):
                    if ci + 1 < len(kcs):
                        st_mm(ci + 1)
                    K.side.step(SIDE_A)
                    par = ci % 2
                    K.op("act", lambda E, par=par: E.activation(out=PTt.ap[:, par * 2:par * 2 + 2, 0:Wd], in_=pSs.ap[:, par * 2:par * 2 + 2, 0:Wd],
                                                                func=AF.Exp, scale=0.125),
                         [PTt.b(par * 2), PTt.b(par * 2 + 1)], [pSs.b(par * 2), pSs.b(par * 2 + 1)])
                    K.side.step(SIDE_B)
                    for m in range(2):
                        sl = par * 2 + m
                        K.mm(pO.b(m)[:, m, 0:Wd], Vh[:, kc, :], PTt.b(sl)[:, sl, 0:Wd], start=(ci == 0), stop=(ci == len(kcs) - 1))
                    if ci == 0:
                        K.op("dve", lambda E, par=par: E.tensor_copy(out=pacc.ap[:, :, 0:Wd], in_=PTt.ap[:, par * 2:par * 2 + 2, 0:Wd]),
                             [pacc.b(0), pacc.b(1)], [PTt.b(par * 2), PTt.b(par * 2 + 1)])
                    else:
                        K.op("dve", lambda E, par=par: E.tensor_tensor(out=pacc.ap[:, :, 0:Wd], in0=pacc.ap[:, :, 0:Wd],
                                                                      in1=PTt.ap[:, par * 2:par * 2 + 2, 0:Wd], op=ALU.add),
                             [pacc.b(0), pacc.b(1)], [pacc.b(0), pacc.b(1), PTt.b(par * 2), PTt.b(par * 2 + 1)])
                    K.side.step(SIDE_N)
                for m in range(2):
                    K.mm(pSs.b(m)[:, m, 0:Wd], ones, pacc.b(m)[:, m, 0:Wd])
                K.op("act", lambda E: E.activation(out=rl.ap[:, :, 0:Wd], in_=pSs.ap[:, 0:2, 0:Wd], func=AF.Ln),
                     [rl.b(0), rl.b(1)], [pSs.b(0), pSs.b(1)])
                K.op("act", lambda E: E.activation(out=rl.ap[:, :, 0:Wd], in_=rl.ap[:, :, 0:Wd], func=AF.Exp, scale=-1.0),
                     [rl.b(0), rl.b(1)], [rl.b(0), rl.b(1)])
                K.tt("dve", o0[:, 0:Wd], pO.b(0)[:, 0, 0:Wd], rl.b(0)[:, 0, 0:Wd], ALU.mult)
                K.tt("dve", o1[:, 0:Wd], pO.b(1)[:, 1, 0:Wd], rl.b(1)[:, 1, 0:Wd], ALU.mult)
                K.stt(o0[:, 0:Wd], o1[:, 0:Wd], neglam, o0[:, 0:Wd], ALU.mult, ALU.add)
                K.tt("pool", osq[:, 0:Wd], o0[:, 0:Wd], o0[:, 0:Wd], ALU.mult)
                K.mm(pSs.b(2)[:, 2, 0:Wd], ones, osq[:, 0:Wd])
                K.rstd(ors[:, 0:Wd], pSs.b(2)[:, 2, 0:Wd], 128, osq[:, 0:Wd])
                s = nqb % 2
                nqb += 1
                K.stt(aob.b(s)[:, s, 0:Wd], o0[:, 0:Wd], gsub[:, 0:1], ors[:, 0:Wd], ALU.mult, ALU.mult)
                K.dma("sp", MIXT.b(nqb)[h * 128:(h + 1) * 128, t0:t0 + Wd], aob.b(s)[:, s, 0:Wd])
        K.side.flush()
        del K.runners[K.side.thread]
        K.side = None
        K.pop()
        if stage_done(f"att{L}") or stage_done(f"ret{L}") or stage_done(f"lru{L}"):
            break

        K.push()
        woutb = K.sb([128, 8, D], BF16, name="woutb", nbuf=1)
        K.dma("pool", woutb[:, :, :], W["w_out"].b(0).m(lambda a: a[L].rearrange("(k p) c -> p k c", p=128)), max_dma_last_dim=4096)
        rw = K.sb([128, 8, NE], F32, name="rw")
        K.dma("sp", rw[:, :, :], W["router_w"].b(0).m(lambda a: a[L].rearrange("(k p) e -> p k e", p=128)))
        mtp = K.sb([128, 2, 8, 128], BF16, name="mtp", nbuf=2)
        x0 = K.sb([128, 2, D], F32, name="x0", nbuf=2)
        x1 = K.sb([128, 2, D], F32, name="x1", nbuf=2)
        pXp = K.ps([128, 2, 512], F32, name="pXp", nbuf=2)
        junk2 = K.sb([128, 2, D], F32, name="junk2", nbuf=2)
        st2p = K.sb([128, 2, 8], F32, name="st2", nbuf=2)
        tmp2 = K.sb([128, 2, D], F32, name="tmp2", nbuf=2)
        h2f2 = K.sb([128, 2, D], F32, name="h2f", nbuf=2)
        h2b = K.sb([128, 2, D], BF16, name="h2b", nbuf=2)
        pHp = K.ps([128, 2, 512], F32, name="pHp", nbuf=2)
        h2Tp = K.sb([128, 2, 8, 128], F32, name="h2T", nbuf=2)
        pRtp = K.ps([128, 2, 512], F32, name="pRt", nbuf=2)
        exp_ = K.sb([128, 2, NE], F32, name="ex", nbuf=2)
        oblocks = [(TC + 512 * i, 512, 0) for i in range(8)]
        if need_ctx:
            oblocks = [(0, TC, 1)] + oblocks
        for r_ in ([0, 1] if need_ctx else [0]):
            for j_ in (2, 3, 4):
                MODv(r_, j_)

        def out_tile(tok, ti, r, p):
            st2 = st2p.b(p)[:, p]
            h2f = h2f2.b(p)[:, p]
            h2T = h2Tp.b(p)[:, p]
            K.dma("sp", mtp.b(p)[:, p, :, :], MIXT.b(0).m(lambda a: a[:, tok:tok + 128].rearrange("(k p) t -> p k t", p=128)))
            K.dma("sp", x0.b(p)[:, p, :], Xsrc.b(0)[tok:tok + 128, :])
            for hf_ in range(2):
                for k in range(8):
                    K.mm(pXp.b(p)[:, p, :], mtp.b(p)[:, p, k, :], woutb[:, k, hf_ * 512:(hf_ + 1) * 512], start=(k == 0), stop=(k == 7))
                K.tt("dve", x1.b(p)[:, p, hf_ * 512:(hf_ + 1) * 512], pXp.b(p)[:, p, :], MODv(r, 2)[:, hf_ * 512:(hf_ + 1) * 512], ALU.mult)
            K.tt("pool", x1.b(p)[:, p, :], x1.b(p)[:, p, :], x0.b(p)[:, p, :], ALU.add)
            dst = (S1.b(ti)[tok:tok + 128, :] if need_ctx else yout.b(ti)[tok - TC:tok - TC + 128, :])
            K.dma("sp", dst, x1.b(p)[:, p, :])
            K.act(junk2.b(p)[:, p, :], x1.b(p)[:, p, :], AF.Square, accum=st2[:, 0:1])
            K.rstd(st2[:, 2:3], st2[:, 0:1], D, st2[:, 1:2])
            K.stt(tmp2.b(p)[:, p, :], x1.b(p)[:, p, :], st2[:, 2:3], MODv(r, 4), ALU.mult, ALU.mult)
            K.tt("pool", h2f[:, :], tmp2.b(p)[:, p, :], MODv(r, 3), ALU.add)
            K.copy("act", h2b.b(p)[:, p, :], h2f[:, :])
            K.dma("sp", H2d.b(ti)[tok:tok + 128, :], h2b.b(p)[:, p, :])
            for q in range(2):
                for k4 in range(4):
                    k = q * 4 + k4
                    K.tr(pHp.b(p)[:, p, k4 * 128:(k4 + 1) * 128], h2f[:, k * 128:(k + 1) * 128], ident)
                K.copy("act" if q == 0 else "dve", h2T[:, q * 4:(q + 1) * 4, :].m(lambda a: a.rearrange("p k t -> p (k t)")), pHp.b(p)[:, p, :])
            for k in range(8):
                K.mm(pRtp.b(p)[:, p, 0:NE], h2T[:, k, :], rw[:, k, :], start=(k == 0), stop=(k == 7))
            K.reduce(st2[:, 3:4], pRtp.b(p)[:, p, 0:NE], ALU.max)
            K.ts("dve", st2[:, 4:5], st2[:, 3:4], -1.0, None, ALU.mult)
            K.act(exp_.b(p)[:, p, :], pRtp.b(p)[:, p, 0:NE], AF.Exp, bias=st2[:, 4:5], accum=st2[:, 5:6])
            K.recip(st2[:, 6:7], st2[:, 5:6])
            K.ts("dve", PROBSp.b(ti)[:, ti, :], exp_.b(p)[:, p, :], st2[:, 6:7], None, ALU.mult)
            if PRd is not None and L == 0:
                K.dma("sp", PRd.b(ti)[tok:tok + 128, :], PROBSp.b(ti)[:, ti, :])

        def out_tiles(par):
            for (t0, Wd, r) in oblocks:
                for i in range(Wd // 128):
                    tok = t0 + i * 128
                    if (tok // 128) % 2 == par:
                        out_tile(tok, tok // 128, r, par)
        K.corun(lambda: out_tiles(0), lambda: out_tiles(1))
        K.pop()
        if stage_done(f"out{L}"):
            break

        def moe(groups, stream_rows):
            K.push()
            pTp = K.ps([NE, 512], F32, name="pTp")
            pPM = K.ps([128, 32, NE], F32, name="pPM")
            wg = K.sb([128, 2, 2, 8, D], BF16, name="wg", nbuf=4)
            wd = K.sb([128, 8, D], BF16, name="wd")
            wn = ("exp_w_gate", "exp_w_up", "exp_w_down")

            def load_w(e):
                s = e % 2
                for j in range(2):
                    K.dma("pool", wg.b(s * 2 + j)[:, s, j, :, :],
                          W[wn[j]].b(0).m(lambda a: a[L, e].rearrange("(k p) c -> p k c", p=128)), max_dma_last_dim=4096)

            def load_wd(e):
                K.dma("pool", wd[:, :, :], W[wn[2]].b(0).m(lambda a: a[L, e].rearrange("(k p) c -> p k c", p=128)),
                      max_dma_last_dim=4096)

            load_w(0)
            for g in groups:
                row0, ntk, cap = g["row0"], g["ntk"], g["cap"]
                Tn = ntk * 128
                ti0 = row0 // 128
                g["ncj"] = (cap + 127) // 128
                g["cwj"] = min(cap, 128)
                PM = K.sb([128, ntk, NE], F32, name="PM")
                g["PM"] = PM
                K.push()
                PTm = K.sb([NE, Tn], F32, name="PTm")
                for i in range(ntk):
                    K.tr(pTp[:, (i % 4) * 128:(i % 4 + 1) * 128], PROBS[:, ti0 + i, :], ident)
                    if i % 4 == 3 or i == ntk - 1:
                        n = (i % 4 + 1) * 128
                        K.copy("act", PTm[:, (i // 4) * 512:(i // 4) * 512 + n], pTp[:, 0:n])
                K.dma("sp", PTd[:, 0:Tn], PTm[:, :])
                PT8 = K.sb([128, Tn], F32, name="PT8")
                for g8 in range(8):
                    K.dma("sp", PT8[g8 * NE:(g8 + 1) * NE, :], PTd[:, 0:Tn])
                bs = K.sb([128, 8], F32, name="bs")
                K.memset("dve", bs[:, 0:1], 0.0)
                K.memset("dve", bs[:, 1:2], 1.0)
                bj = K.sb([128, Tn], BF16, name="bj")
                for it in range(10):
                    K.tt("dve", bs[:, 2:3], bs[:, 1:2], bs[:, 0:1], ALU.subtract)
                    K.stt(bs[:, 3:4], bs[:, 2:3], cfrac, bs[:, 0:1], ALU.mult, ALU.add)
                    K.ts("dve", bj[:, :], PT8[:, :], bs[:, 3:4], 0.0, ALU.is_ge, ALU.add, accum=bs[:, 4:5])
                    K.ts("dve", bs[:, 5:6], bs[:, 4:5], float(cap), None, ALU.is_ge)
                    K.mm(pPM[:, 0, 0:1], sameE, bs[:, 5:6])
                    K.ts("dve", bs[:, 6:7], pPM[:, 0, 0:1], 0.125, 0.125, ALU.mult, ALU.add)
                    K.stt(bs[:, 1:2], bs[:, 2:3], bs[:, 6:7], bs[:, 0:1], ALU.mult, ALU.add)
                    K.ts("dve", bs[:, 6:7], pPM[:, 0, 0:1], 0.125, None, ALU.mult)
                    K.stt(bs[:, 0:1], bs[:, 2:3], bs[:, 6:7], bs[:, 0:1], ALU.mult, ALU.add)
                K.ts("dve", bj[0:NE, :], PT8[0:NE, :], bs[0:NE, 0:1], None, ALU.is_ge)
                pos = K.sb([NE, Tn], F32, name="pos")
                K.scan(pos[:, :], bj[0:NE, :], bj[0:NE, :], 0.0, op0=ALU.add, op1=ALU.max)
                K.tt("dve", pos[:, :], pos[:, :], bj[0:NE, :], ALU.mult)
                K.ts("dve", pos[:, :], pos[:, :], -1.0, None, ALU.add)
                for i in range(ntk):
                    K.tr(pPM[:, i, :], pos[:, i * 128:(i + 1) * 128], ident[0:NE, 0:NE])
                K.copy("act", PM[:, :, :], pPM[:, 0:ntk, :])
                K.pop()
                vals = K.sb([128, ntk, NE, 5], BF16, name="vals")
                g["vals"] = vals
                K.copy("dve", vals[:, :, :, 0], tidx[:, 0:ntk].m(lambda a: a.unsqueeze(2).to_broadcast([128, ntk, NE])))
                K.copy("dve", vals[:, :, :, 1], pidx.m(lambda a: a.unsqueeze(2).to_broadcast([128, ntk, NE])))
                pr = PROBS[:, ti0:ti0 + ntk, :]
                vf = K.sb([128, ntk, NE], F32, name="vf")
                r1 = K.sb([128, ntk, NE], F32, name="r1")
                K.copy("dve", vals[:, :, :, 2], pr)
                K.copy("dve", vf[:, :, :], vals[:, :, :, 2])
                K.tt("dve", r1[:, :, :], pr, vf[:, :, :], ALU.subtract)
                K.copy("dve", vals[:, :, :, 3], r1[:, :, :])
                K.copy("dve", vf[:, :, :], vals[:, :, :, 3])
                K.tt("dve", r1[:, :, :], r1[:, :, :], vf[:, :, :], ALU.subtract)
                K.copy("dve", vals[:, :, :, 4], r1[:, :, :])
                ncj = g["ncj"]
                g["idxi"] = K.sb([128, 2, 4], I32, name="idxi", nbuf=2)
                g["idxs"] = K.sb([128, 2, 4], I32, name="idxs", nbuf=2)
                g["aff"] = K.sb([128, 2, 4], F32, name="aff", nbuf=2)
                g["Xg"] = K.sb([128, ncj, D], BF16, name="Xg", nbuf=ncj)
                g["XT"] = K.sb([128, 2, 8, cap], BF16, name="XT", nbuf=2)
                g["hid"] = K.sb([128, 8, cap], BF16, name="hid", nbuf=8)
            NSEL = 6
            sel = K.sb([128, NSEL, 512], BF16, name="sel", nbuf=NSEL)
            pPMf = pPM.ap.rearrange("p t e -> p (t e)")
            pIg = [T(pTp.ap[0:5, :]), T(pPMf[0:5, 64:64 + 64])]
            pITg = [T(pPMf[:, 0:32]), T(pPMf[:, 32:40])]
            ilg = [K.sb([8, 512], F32, name="il"), K.sb([8, 64], F32, name="il2")]
            ilT = K.sb([128, 2, 4, 8], F32, name="ilT", nbuf=2)
            idxf = K.sb([128, 2, 4], F32, name="idxf", nbuf=2)
            pXT = K.ps([128, 512], F32, name="pXT")
            pGU = K.ps([128, 2, 512], F32, name="pGU", nbuf=2)
            pC = K.ps([128, 512], F32, name="pC")
            sgl = K.sb([128, 512], F32, name="sgl")
            pY = K.ps([128, 2, 512], F32, name="pY", nbuf=2)
            ysb = K.sb([128, 2, D], F32, name="ysb", nbuf=2)
            cnt = {"sel": 0, "y": 0}
            scb = [[Buf() for _ in range(8)] for _ in range(2)]
            for b_ in scb[0] + scb[1]:
                b_.w = stream_rows.buf.w

            def sel_op(e, gi, i, slot):
                g = groups[gi]
                cap = g["cap"]
                K.ts("dve", sel.b(slot)[:, slot, 0:cap], iota[:, 0:cap], g["PM"][:, i, e:e + 1], None, ALU.is_equal)

            def idx_mm(e, gi, i, slot):
                g = groups[gi]
                cap, ntk = g["cap"], g["ntk"]
                K.mm(pIg[gi][:, 0:cap], g["vals"][:, i, e, :], sel.b(slot)[:, slot, 0:cap], start=(i == 0), stop=(i == ntk - 1))

            def prep_fin(e):
                s = e % 2
                for gi, g in enumerate(groups):
                    cap, ncj, cwj = g["cap"], g["ncj"], g["cwj"]
                    il = ilg[gi]
                    K.copy("act", il[0:5, 0:cap], pIg[gi][:, 0:cap])
                    for jc in range(ncj):
                        K.tr(pITg[gi][0:cwj, jc * 8:jc * 8 + 5], il[0:5, jc * 128:jc * 128 + cwj], ident[0:5, 0:5])
                    iT = ilT.b(gi)[0:cwj, gi, 0:ncj, 0:5]
                    K.copy("act", iT, pITg[gi][0:cwj, 0:ncj * 8].m(lambda a: a.rearrange("p (j f) -> p j f", f=8)[:, :, 0:5]))
                    iF = idxf.b(gi)[0:cwj, gi, 0:ncj]
                    K.stt(iF, ilT.b(gi)[0:cwj, gi, 0:ncj, 0], 128.0, ilT.b(gi)[0:cwj, gi, 0:ncj, 1], ALU.mult, ALU.add)
                    K.ts("dve", iF, iF, float(g["srow0"]), None, ALU.add)
                    K.copy("dve", g["idxs"].b(s)[0:cwj, s, 0:ncj], iF)
                    K.ts("dve", iF, iF, float(g["row0"] - g["srow0"]), None, ALU.add)
                    K.copy("dve", g["idxi"].b(s)[0:cwj, s, 0:ncj], iF)
                    K.tt("dve", g["aff"].b(s)[0:cwj, s, 0:ncj], ilT.b(gi)[0:cwj, gi, 0:ncj, 2], ilT.b(gi)[0:cwj, gi, 0:ncj, 3], ALU.add)
                    K.tt("dve", g["aff"].b(s)[0:cwj, s, 0:ncj], g["aff"].b(s)[0:cwj, s, 0:ncj], ilT.b(gi)[0:cwj, gi, 0:ncj, 4], ALU.add)
                    for jc in range(ncj):
                        iv = g["idxi"].b(s)[0:cwj, s, jc:jc + 1]
                        Xg = g["Xg"]
                        K.dma("pool", Xg.b(jc)[0:cwj, jc, :], H2d.b(0)[:, :], extra_in=[iv],
                              fn=lambda E, jc=jc, iv=iv, Xg=Xg, cwj=cwj: E.indirect_dma_start(
                                  out=Xg.ap[0:cwj, jc, :], out_offset=None, in_=H2d.ap[:, :],
                                  in_offset=bass.IndirectOffsetOnAxis(ap=iv.ap, axis=0)))

            def sel_list(e):
                return [(gi, i) for gi, g in enumerate(groups) for i in range(g["ntk"])]

            def prep_xt(e):
                s = e % 2
                for g in groups:
                    cap, ncj, cwj = g["cap"], g["ncj"], g["cwj"]
                    for k in range(8):
                        tgt = (pXT[:, :], pY.b(0)[:, 0, :], pY.b(1)[:, 1, :])[k % 3]
                        for jc in range(ncj):
                            K.mm(tgt[:, jc * 128:jc * 128 + cwj], g["Xg"].b(jc)[0:cwj, jc, k * 128:(k + 1) * 128], identb[0:cwj, 0:cwj])
                        K.copy("act" if k % 2 == 0 else "dve", g["XT"].b(s)[:, s, k, 0:cap], tgt[:, 0:cap])

            def gate_up(e, pend):
                s = e % 2
                per = (len(pend) + 7) // 8
                for fc in range(8):
                    batch = pend[fc * per:(fc + 1) * per]
                    slots = []
                    for (gi, i) in batch:
                        slot = cnt["sel"] % NSEL
                        cnt["sel"] += 1
                        slots.append(slot)
                        sel_op(e + 1, gi, i, slot)
                    for gi, g in enumerate(groups):
                        cap = g["cap"]
                        for j in range(2):
                            po = pGU.b(j)[:, j, 0:cap] if gi == 0 else pC[:, j * 64:j * 64 + cap]
                            for k in range(8):
                                K.mm(po, wg.b(s * 2 + j)[:, s, j, k, fc * 128:(fc + 1) * 128], g["XT"].b(s)[:, s, k, 0:cap],
                                     start=(k == 0), stop=(k == 7))
                    for (gi, i), slot in zip(batch, slots):
                        idx_mm(e + 1, gi, i, slot)
                    for gi, g in enumerate(groups):
                        cap = g["cap"]
                        pg = pGU.b(0)[:, 0, 0:cap] if gi == 0 else pC[:, 0:cap]
                        pu = pGU.b(1)[:, 1, 0:cap] if gi == 0 else pC[:, 64:64 + cap]
                        K.act(sgl[:, 0:cap], pg, AF.Silu)
                        K.tt("dve", g["hid"].b(fc)[:, fc, 0:cap], sgl[:, 0:cap], pu, ALU.mult)

            def down(e):
                s = e % 2
                nsc = 0
                prevb = [V(None, b_) for b_ in scb[(e + 1) % 2]]
                for g in groups:
                    ncj, cwj = g["ncj"], g["cwj"]
                    g2b = MODv(g["r"], 5)
                    for jc in range(ncj):
                        ys = cnt["y"] % 2
                        cnt["y"] += 1
                        for hf_ in range(2):
                            for fc in range(8):
                                K.mm(pY.b(hf_)[0:cwj, hf_, :], g["hid"].b(fc)[:, fc, jc * 128:jc * 128 + cwj],
                                     wd[:, fc, hf_ * 512:(hf_ + 1) * 512], start=(fc == 0), stop=(fc == 7))
                            K.stt(ysb.b(ys)[0:cwj, ys, hf_ * 512:(hf_ + 1) * 512], pY.b(hf_)[0:cwj, hf_, :],
                                  g["aff"].b(s)[0:cwj, s, jc:jc + 1], g2b[0:cwj, hf_ * 512:(hf_ + 1) * 512], ALU.mult, ALU.mult)
                        iv = g["idxs"].b(s)[0:cwj, s, jc:jc + 1]
                        sv_ = V(stream_rows.ap, scb[e % 2][nsc])
                        nsc += 1
                        K.dma("pool", sv_, ysb.b(ys)[0:cwj, ys, :], extra_in=[iv] + prevb,
                              fn=lambda E, ys=ys, iv=iv, cwj=cwj: E.indirect_dma_start(
                                  out=stream_rows.ap, out_offset=bass.IndirectOffsetOnAxis(ap=iv.ap, axis=0),
                                  in_=ysb.ap[0:cwj, ys, :], in_offset=None, compute_op=ALU.add))

            for (gi, i) in sel_list(0):
                slot = cnt["sel"] % NSEL
                cnt["sel"] += 1
                sel_op(0, gi, i, slot)
                idx_mm(0, gi, i, slot)
            prep_fin(0)
            prep_xt(0)
            for e in range(NE):
                load_wd(e)
                if e + 1 < NE:
                    load_w(e + 1)
                gate_up(e, sel_list(e + 1) if e + 1 < NE else [])
                if e + 1 < NE:
                    prep_fin(e + 1)
                down(e)
                if e + 1 < NE:
                    prep_xt(e + 1)
            K.pop()

        glat = dict(row0=TC, ntk=TL // 128, cap=2 * TL // NE, r=0, srow0=(TC if need_ctx else 0))
        if need_ctx:
            gctx = dict(row0=0, ntk=TC // 128, cap=2 * TC // NE, r=1, srow0=0)
            moe([glat, gctx], S1[:, :])
        else:
            moe([glat], yout[:, :])
        if stage_done(f"moe{L}"):
            break

    K.barrier(["sp"])
    return nc


def _prep_inputs(inputs):
    f = lambda a: np.ascontiguousarray(np.asarray(a, dtype=np.float32))
    x, c, ctx, c_ctx = f(inputs["x"]), f(inputs["c"]), f(inputs["ctx"]), f(inputs["c_ctx"])
    cosT, sinT = host_rope()
    consts = host_consts()
    shared = dict(consts=consts, cosT=cosT, sinT=sinT)
    for n in ("mod_w", "mod_b", "norm1_g", "norm2_g", "w_in", "w_out", "att_q_norm_g", "att_k_norm_g", "att_subln_g",
              "ret_norm_g", "lru_conv_w", "lru_conv_b", "lru_gate_w", "lru_lambda", "lru_norm_g", "router_w",
              "exp_w_gate", "exp_w_up", "exp_w_down"):
        shared[n] = f(inputs[n])
    shared["att_lambda"] = f(inputs["att_lambda"]).reshape(DEPTH, 256)
    shared["ret_log_decay"] = f(inputs["ret_log_decay"]).reshape(DEPTH, 8)
    shared["lru_gate_b"] = f(inputs["lru_gate_b"]).reshape(DEPTH, 4, 256)
    maps = []
    for b in range(x.shape[0]):
        m = dict(shared)
        m["xin"] = np.ascontiguousarray(np.concatenate([ctx[b], x[b]], axis=0))
        m["cvec"] = np.ascontiguousarray(np.stack([c[b], c_ctx], axis=1))
        maps.append(m)
    return maps


_NC_CACHE = {}


def kernel(**inputs):
    maps = _prep_inputs(inputs)
    if "nc" not in _NC_CACHE:
        _NC_CACHE["nc"] = build()
    nc = _NC_CACHE["nc"]
    res = run_bass_kernel_spmd(nc, maps, core_ids=list(range(N_CORES)))
    return np.stack([np.asarray(r["y"], dtype=np.float32) for r in res.results], axis=0)
```

```python
import math
import threading
from contextlib import ExitStack
import numpy as np
import concourse.bass as bass
import concourse.mybir as mybir
from concourse.bass_utils import run_bass_kernel_spmd

F32 = mybir.dt.float32
BF16 = mybir.dt.bfloat16
I32 = mybir.dt.int32
ALU = mybir.AluOpType
AF = mybir.ActivationFunctionType
AX = mybir.AxisListType

D = 1024
TC = 256
TL = 4096
TT = TC + TL
NT = TT // 128
DEPTH = 2
EPS = 1e-6
NE = 16
N_CORES = 8


class Buf:
    __slots__ = ("w", "rs")

    def __init__(self):
        self.w = None
        self.rs = {}


class V:
    def __init__(self, ap, buf):
        self.ap = ap
        self.buf = buf

    def __getitem__(self, k):
        return V(self.ap[k], self.buf)

    def m(self, f):
        return V(f(self.ap), self.buf)


class T:
    def __init__(self, ap, nbuf=1):
        self.ap = ap
        self.bufs = [Buf() for _ in range(nbuf)]

    def __getitem__(self, k):
        return V(self.ap[k], self.bufs[0])

    def b(self, i):
        return V(self.ap, self.bufs[i % len(self.bufs)])


class SideRunner:
    def __init__(self, fn):
        self.go = threading.Semaphore(0)
        self.back = threading.Semaphore(0)
        self.budget = 0
        self.finished = False
        self.exc = None
        self.nops = 0

        def run():
            self.go.acquire()
            try:
                fn()
            except BaseException as e:
                self.exc = e
            self.finished = True
            self.back.release()
        self.thread = threading.Thread(target=run, daemon=True)
        self.thread.start()

    def tick(self):
        while self.budget <= 0:
            self.back.release()
            self.go.acquire()
        self.budget -= 1
        self.nops += 1

    def step(self, n):
        if self.finished:
            return
        self.budget = n
        self.go.release()
        self.back.acquire()
        if self.exc is not None:
            raise self.exc

    def flush(self):
        while not self.finished:
            self.step(10 ** 9)
        if self.exc is not None:
            raise self.exc


class Kern:
    CE = ("pe", "act", "dve", "pool")
    side = None

    def _gate(self):
        r = self.runners.get(threading.current_thread())
        if r is not None:
            r.tick()

    def _costep(self):
        r = self.co.get(threading.current_thread())
        if r is not None:
            r.step(1)

    def spawn(self, fn):
        r = SideRunner(fn)
        self.runners[r.thread] = r
        return r

    def corun(self, fn_a, fn_b):
        r = self.spawn(fn_b)
        me = threading.current_thread()
        self.co[me] = r
        try:
            fn_a()
        finally:
            del self.co[me]
        r.flush()
        del self.runners[r.thread]

    def __init__(self, nc):
        self.nc = nc
        self.eng = dict(pe=nc.tensor, act=nc.scalar, dve=nc.vector, pool=nc.gpsimd, sp=nc.sync)
        self.sems = []
        self.cur = {}
        self.cnt = {}
        self.seen = {e: {} for e in self.eng}
        self.nsem = 0
        for e in self.CE:
            self._new_eng_sem(e)
        self.dpool = {}
        self.dnext = {}
        self.dcnt = {}
        for q, n in (("sp", 16), ("pool", 16), ("act", 6)):
            self.dpool[q] = [self._new_sem() for _ in range(n)]
            self.dnext[q] = 0
            for s in self.dpool[q]:
                self.dcnt[s] = 0
        self.runners = {}
        self.co = {}
        self.stacks = [ExitStack()]
        self.caches = [{}]
        self.uid = 0

    def _new_sem(self):
        h = self.nc.alloc_semaphore(name=f"s{self.nsem}")
        self.nsem += 1
        self.sems.append(h)
        return len(self.sems) - 1

    def _new_eng_sem(self, e):
        self.cur[e] = self._new_sem()
        self.cnt[e] = 0

    def _wait(self, e, ev):
        if ev is None:
            return
        s, v = ev
        if v <= 0 or self.seen[e].get(s, 0) >= v:
            return
        self.eng[e].wait_ge(self.sems[s], v)
        self.seen[e][s] = v

    def _deps(self, e, reads, writes):
        for b in reads:
            self._wait(e, b.w)
        for b in writes:
            self._wait(e, b.w)
            for s, v in list(b.rs.items()):
                self._wait(e, (s, v))

    def _post(self, ev, reads, writes):
        for b in reads:
            if b.rs.get(ev[0], 0) < ev[1]:
                b.rs[ev[0]] = ev[1]
        for b in writes:
            b.w = ev
            b.rs = {}

    @staticmethod
    def _bufs(vs):
        out = []
        for v in vs:
            if isinstance(v, V) and v.buf not in out:
                out.append(v.buf)
        return out

    def op(self, e, fn, outs, ins):
        self._gate()
        reads = self._bufs(ins)
        writes = self._bufs(outs)
        self._deps(e, reads, writes)
        inst = fn(self.eng[e])
        s = self.cur[e]
        self.cnt[e] += 1
        c = self.cnt[e]
        inst.then_inc(self.sems[s], 1)
        if e == "pe":
            self.seen[e][s] = c
        self._post((s, c), reads, writes)
        if c >= 30000:
            self._new_eng_sem(e)
        self._costep()

    def dma(self, q, out, in_, extra_in=(), fn=None, **kw):
        self._gate()
        reads = self._bufs([in_] + list(extra_in))
        writes = self._bufs([out])
        pool = self.dpool[q]
        si = pool[self.dnext[q] % len(pool)]
        self.dnext[q] += 1
        self._wait(q, (si, self.dcnt[si]))
        self._deps(q, reads, writes)
        if fn is None:
            inst = self.eng[q].dma_start(out=out.ap, in_=in_.ap, **kw)
        else:
            inst = fn(self.eng[q])
        self.dcnt[si] += 16
        inst.then_inc(self.sems[si], 16)
        self._post((si, self.dcnt[si]), reads, writes)
        self._costep()

    def barrier(self, engines=None):
        for e in (engines or list(self.eng)):
            for q in self.dpool:
                for si in self.dpool[q]:
                    self._wait(e, (si, self.dcnt[si]))
            for en in self.CE:
                if en != e or en != "pe":
                    self._wait(e, (self.cur[en], self.cnt[en]))

    def push(self):
        self.stacks.append(ExitStack())
        self.caches.append({})

    def pop(self):
        self.barrier()
        self.stacks.pop().close()
        self.caches.pop()

    def sb(self, shape, dtype, nbuf=1, name=None):
        self.uid += 1
        h = self.stacks[-1].enter_context(self.nc.sbuf_tensor(f"{name or 'sb'}_{self.uid}", list(shape), dtype))
        return T(h, nbuf)

    def ps(self, shape, dtype=F32, nbuf=1, name=None):
        self.uid += 1
        h = self.stacks[-1].enter_context(self.nc.psum_tensor(f"{name or 'ps'}_{self.uid}", list(shape), dtype))
        return T(h, nbuf)

    def dr(self, name, shape, dtype, kind=None, nbuf=1):
        if kind is None:
            t = self.nc.dram_tensor(name, list(shape), dtype)
        else:
            t = self.nc.dram_tensor(name, list(shape), dtype, kind=kind)
        return T(t.ap(), nbuf)

    @staticmethod
    def _a(x):
        return x.ap if isinstance(x, V) else x

    def act(self, out, in_, func, bias=None, scale=None, accum=None):
        kw = {}
        if bias is not None:
            kw["bias"] = self._a(bias)
        if scale is not None:
            kw["scale"] = self._a(scale)
        if accum is not None:
            kw["accum_out"] = accum.ap
        self.op("act", lambda E: E.activation(out=out.ap, in_=in_.ap, func=func, **kw),
                [out, accum], [in_, bias, scale])

    def tt(self, e, out, a, b, op):
        self.op(e, lambda E: E.tensor_tensor(out=out.ap, in0=a.ap, in1=b.ap, op=op), [out], [a, b])

    def ts(self, e, out, a, s1, s2=None, op0=ALU.mult, op1=None, accum=None):
        kw = {}
        if op1 is not None:
            kw["op1"] = op1
        if accum is not None:
            kw["accum_out"] = accum.ap
        self.op(e, lambda E: E.tensor_scalar(out=out.ap, in0=a.ap, scalar1=self._a(s1), scalar2=self._a(s2),
                                              op0=op0, **kw), [out, accum], [a, s1, s2])

    def stt(self, out, a, s, b, op0, op1):
        self.op("dve", lambda E: E.scalar_tensor_tensor(out=out.ap, in0=a.ap, scalar=self._a(s), in1=b.ap,
                                                       op0=op0, op1=op1), [out], [a, s, b])

    def copy(self, e, out, in_):
        if e == "act":
            self.op(e, lambda E: E.copy(out=out.ap, in_=in_.ap), [out], [in_])
        else:
            self.op(e, lambda E: E.tensor_copy(out=out.ap, in_=in_.ap), [out], [in_])

    def memset(self, e, out, val):
        self.op(e, lambda E: E.memset(out.ap, val), [out], [])

    def recip(self, out, in_):
        self.op("dve", lambda E: E.reciprocal(out=out.ap, in_=in_.ap), [out], [in_])

    def reduce(self, out, in_, op, axis=AX.X):
        self.op("dve", lambda E: E.tensor_reduce(out=out.ap, in_=in_.ap, axis=axis, op=op), [out], [in_])

    def scan(self, out, d0, d1, init, op0=ALU.mult, op1=ALU.add):
        self.op("dve", lambda E: E.tensor_tensor_scan(out=out.ap, data0=d0.ap, data1=d1.ap, initial=self._a(init),
                                                     op0=op0, op1=op1), [out], [d0, d1, init])

    def mm(self, out, lhsT, rhs, start=True, stop=True):
        self.op("pe", lambda E: E.matmul(out.ap, lhsT.ap, rhs.ap, start=start, stop=stop), [out], [lhsT, rhs])

    def tr(self, out, in_, ident):
        self.op("pe", lambda E: E.transpose(out.ap, in_.ap, ident.ap), [out], [in_, ident])

    def rstd(self, out, ss, n, tmp):
        self.act(tmp, ss, AF.Ln, bias=self.epsc[: ss.ap.shape[0]], scale=1.0 / n)
        self.act(out, tmp, AF.Exp, scale=-0.5)

    def sigmoid(self, out, in_, tmp, nbias=None, scale=1.0):
        if nbias is None:
            self.act(tmp, in_, AF.Exp, scale=-scale)
        else:
            self.act(tmp, in_, AF.Exp, bias=nbias, scale=-scale)
        self.act(tmp, tmp, AF.Ln, bias=self.onec[: in_.ap.shape[0]])
        self.act(out, tmp, AF.Exp, scale=-1.0)


FM_ROWS = [0, 128, 256, 384, 512, 640, 768, 896, 1536, 1664, 1792, 1920, 2560, 2688, 2816, 2944]
NCONST = 128 + 128 + 128 + 512 + 64 + 1 + 1 + 128
import os
SIDE_N, SIDE_A, SIDE_B = [int(v) for v in os.environ.get('SIDE_CFG', '1,0,1').split(',')]


def host_consts():
    c = np.zeros((128, NCONST), np.float32)
    c[:, 0:128] = np.eye(128, dtype=np.float32)
    for i in range(128):
        d = i % 64
        p = d + 16 if (d % 32) < 16 else d - 16
        c[(i // 64) * 64 + p, 128 + i] = 1.0
    c[0:64, 256:320] = 1.0
    c[64:128, 320:384] = 1.0
    c[:, 384:896] = np.arange(512, dtype=np.float32)[None, :]
    c[:, 896:960] = np.arange(64, dtype=np.float32)[None, :]
    c[:, 960] = np.arange(128, dtype=np.float32)
    c[:, 961] = (np.arange(128) // 16 + 1).astype(np.float32) / 8.0
    pp = np.arange(128)
    c[:, 962:1090] = (pp[:, None] % 16 == pp[None, :] % 16).astype(np.float32)
    return c


def host_rope():
    rows = TL // 64
    row = np.repeat(np.arange(rows), 64).astype(np.float32)
    col = np.tile(np.arange(64), rows).astype(np.float32)
    inv = (1.0 / (np.float32(10000.0) ** (np.arange(0, 32, 2, dtype=np.float32) / np.float32(32)))).astype(np.float32)
    ang = np.concatenate([row[:, None] * inv, col[:, None] * inv], axis=-1).astype(np.float32)
    cos = np.cos(ang).astype(np.float32)
    sin = np.sin(ang).astype(np.float32)
    cosT = np.ones((128, TT), np.float32)
    sinT = np.zeros((128, TT), np.float32)
    for r in range(128):
        d = r % 64
        f = d % 16
        a = f if d < 32 else 16 + f
        cosT[r, TC:] = cos[:, a]
        sinT[r, TC:] = -sin[:, a] if (d % 32) < 16 else sin[:, a]
    return cosT, sinT


def build(stop_after=None, dbg=()):
    nc = bass.Bass("TRN2", target_bir_lowering=False)
    K = Kern(nc)

    def dk(name):
        return "ExternalOutput" if name in dbg else None

    xin = K.dr("xin", [TT, D], F32, "ExternalInput")
    cvec = K.dr("cvec", [D, 2], F32, "ExternalInput")
    consts = K.dr("consts", [128, NCONST], F32, "ExternalInput")
    cosD = K.dr("cosT", [128, TT], F32, "ExternalInput")
    sinD = K.dr("sinT", [128, TT], F32, "ExternalInput")
    W = {}
    for n, shp in (("mod_w", [DEPTH, D, 6 * D]), ("mod_b", [DEPTH, 6 * D]), ("norm1_g", [DEPTH, D]), ("norm2_g", [DEPTH, D]),
                   ("w_in", [DEPTH, D, 3072]), ("w_out", [DEPTH, D, D]), ("att_q_norm_g", [DEPTH, 64]),
                   ("att_k_norm_g", [DEPTH, 64]), ("att_lambda", [DEPTH, 256]), ("att_subln_g", [DEPTH, 128]),
                   ("ret_log_decay", [DEPTH, 8]), ("ret_norm_g", [DEPTH, 64]), ("lru_conv_w", [DEPTH, 4, 256]),
                   ("lru_conv_b", [DEPTH, 256]), ("lru_gate_w", [DEPTH, 2, 2, 4, 64, 64]), ("lru_gate_b", [DEPTH, 4, 256]),
                   ("lru_lambda", [DEPTH, 2, 256]), ("lru_norm_g", [DEPTH, 256]), ("router_w", [DEPTH, D, NE]),
                   ("exp_w_gate", [DEPTH, NE, D, D]), ("exp_w_up", [DEPTH, NE, D, D]), ("exp_w_down", [DEPTH, NE, D, D])):
        W[n] = K.dr(n, shp, F32, "ExternalInput")
    yout = K.dr("y", [TL, D], F32, "ExternalOutput", nbuf=NT)
    S1 = K.dr("S1", [TT, D], F32, dk("S1"), nbuf=NT)
    QT = K.dr("QT", [1024, TT], BF16, dk("QT"), nbuf=64)
    KT = K.dr("KT", [1024, TT], BF16, dk("KT"), nbuf=64)
    Vd = K.dr("Vd", [TT, 512], BF16, dk("Vd"), nbuf=NT)
    RQT = K.dr("RQT", [256, TT], BF16, dk("RQT"), nbuf=32)
    RKT = K.dr("RKT", [256, TT], BF16, dk("RKT"), nbuf=32)
    RKd = K.dr("RKd", [TT, 256], BF16, dk("RKd"), nbuf=NT)
    RVd = K.dr("RVd", [TT, 256], BF16, dk("RVd"), nbuf=NT)
    SGd = K.dr("SGd", [TT, 256], F32, dk("SGd"), nbuf=NT)
    XUd = K.dr("XUd", [256, TT], F32, dk("XUd"), nbuf=32)
    GUd = K.dr("GUd", [256, TT], F32, dk("GUd"), nbuf=32)
    MIXT = K.dr("MIXT", [1024, TT], BF16, dk("MIXT"), nbuf=64)
    H2d = K.dr("H2d", [TT, D], BF16, dk("H2d"), nbuf=NT)
    PRd = K.dr("PRd", [TT, NE], F32, dk("PRd"), nbuf=NT) if "PRd" in dbg else None

    cst = K.sb([128, NCONST], F32, name="cst")
    K.dma("sp", cst[:, :], consts[:, :])
    ident = cst[:, 0:128]
    permc = cst[:, 128:256]
    blk1 = cst[:, 256:384]
    iota = cst[:, 384:896]
    tidx = cst[:, 896:960]
    pidx = cst[:, 960:961]
    cfrac = cst[:, 961:962]
    sameE = cst[:, 962:1090]
    PTd = K.dr("PTd", [NE, TL], F32)
    identb_t = K.sb([128, 128], BF16, name="identb")
    K.copy("dve", identb_t[:, :], ident)
    identb = identb_t[:, :]
    ones_t = K.sb([128, 128], F32, name="ones")
    K.memset("dve", ones_t[:, :], 1.0)
    ones = ones_t[:, :]
    onesb_t = K.sb([128, 512], BF16, name="onesb")
    K.memset("dve", onesb_t[:, :], 1.0)
    onesb = onesb_t[:, :]
    epsc_t = K.sb([128, 1], F32, name="epsc")
    K.memset("dve", epsc_t[:, :], EPS)
    K.epsc = epsc_t[:, :]
    onec_t = K.sb([128, 1], F32, name="onec")
    K.memset("dve", onec_t[:, :], 1.0)
    K.onec = onec_t[:, :]
    csil = K.sb([128, 8, 2], F32, name="csil")
    K.dma("sp", csil[:, :, :], cvec.b(0).m(lambda a: a.rearrange("(k p) r -> p k r", p=128)))
    K.act(csil[:, :, :], csil[:, :, :], AF.Silu)
    crep = K.sb([128, 2, 8, 128], BF16, name="crep")
    for r in range(2):
        K.copy("dve", crep[:, r, :, :], csil[:, :, r:r + 1].m(lambda a: a.to_broadcast([128, 8, 128])))
    MODS = K.dr("MODS", [DEPTH, 2, 6 * D], F32, dk("MODS"), nbuf=DEPTH)
    PROBS = K.sb([128, NT, NE], F32, name="PROBS")
    PROBSp = T(PROBS.ap, nbuf=NT)

    def col(dst, src_ap):
        K.dma("sp", dst, V(src_ap.rearrange("(p o) -> p o", o=1), Buf()))

    def bc(dst, src_ap, n=128):
        K.dma("sp", dst, V(src_ap.partition_broadcast(n), Buf()))

    def stage_done(name):
        return stop_after == name

    for L in range(DEPTH):
        need_ctx = L < DEPTH - 1
        Xsrc = xin if L == 0 else S1
        lam_init = 0.8 - 0.6 * math.exp(-0.3 * L)

        def xrows(t0, n):
            if need_ctx:
                return S1[t0:t0 + n, :]
            return yout[t0 - TC:t0 - TC + n, :]

        K.push()
        winb = K.sb([128, 8, 3072], BF16, name="winb", nbuf=1)
        for cgi in range(3):
            K.dma("pool", winb[:, :, cgi * 1024:(cgi + 1) * 1024],
                  W["w_in"].b(0).m(lambda a: a[L, :, cgi * 1024:(cgi + 1) * 1024].rearrange("(k p) c -> p k c", p=128)), max_dma_last_dim=4096)
        K.push()
        modb = K.sb([128, 6 * D], F32, name="modb")
        bc(modb[:, :], W["mod_b"].ap[L])
        n1g = K.sb([128, D], F32, name="n1g")
        bc(n1g[:, :], W["norm1_g"].ap[L])
        n2g = K.sb([128, D], F32, name="n2g")
        bc(n2g[:, :], W["norm2_g"].ap[L])
        MOD = K.sb([128, 2, 6 * D], F32, name="MOD", nbuf=2)
        mw = K.sb([128, 2, 8, 512], BF16, name="mw", nbuf=2)
        pm = K.ps([128, 2, 512], F32, name="pm", nbuf=2)
        for cg in range(12):
            s = cg % 2
            K.dma("pool", mw.b(s)[:, s, :, :],
                  W["mod_w"].b(0).m(lambda a: a[L, :, cg * 512:(cg + 1) * 512].rearrange("(k p) c -> p k c", p=128)), max_dma_last_dim=2048)
            for r in range(2):
                for k in range(8):
                    K.mm(pm.b(r)[:, r, :], crep[:, r, k, :], mw.b(s)[:, s, k, :], start=(k == 0), stop=(k == 7))
                K.tt("dve", MOD.b(r)[:, r, cg * 512:(cg + 1) * 512], pm.b(r)[:, r, :], modb[:, cg * 512:(cg + 1) * 512], ALU.add)
        for r in range(2):
            K.stt(MOD.b(r)[:, r, 1024:2048], MOD.b(r)[:, r, 1024:2048], 1.0, n1g[:, :], ALU.add, ALU.mult)
            K.stt(MOD.b(r)[:, r, 4096:5120], MOD.b(r)[:, r, 4096:5120], 1.0, n2g[:, :], ALU.add, ALU.mult)
        for r in range(2):
            K.dma("sp", MODS.b(L).m(lambda a: a[L, r:r + 1, :]), MOD.b(r)[0:1, r, :])
        K.pop()
        if stage_done(f"mod{L}"):
            break

        def MODv(r, j):
            for cch in reversed(K.caches):
                if ("mod", r, j) in cch:
                    return cch[("mod", r, j)]
            t_ = K.sb([128, D], F32, name="modv")
            K.dma("sp", t_[:, :], MODS.b(L).m(lambda a: a[L, r, j * 1024:(j + 1) * 1024].partition_broadcast(128)))
            K.caches[-1][("mod", r, j)] = t_[:, :]
            return t_[:, :]

        cosS = K.sb([128, 512], F32, name="cosS")
        sinS = K.sb([128, 512], F32, name="sinS")
        gq = K.sb([128, 2], F32, name="gq")
        for j, nm in enumerate(("att_q_norm_g", "att_k_norm_g")):
            for hh in range(2):
                col(gq[hh * 64:(hh + 1) * 64, j:j + 1], W[nm].ap[L])
        permg = K.sb([128, 2, 128], F32, name="permg")
        for j in range(2):
            K.ts("dve", permg[:, j, :], permc, gq[:, j:j + 1], None, ALU.mult)
        xt = K.sb([128, 2, D], F32, name="xt", nbuf=2)
        st = K.sb([128, 8], F32, name="st", nbuf=4)
        tmpn = K.sb([128, D], F32, name="tmpn", nbuf=1)
        hb2 = K.sb([128, 2, 4, D], BF16, name="hb", nbuf=8)
        hT2 = K.sb([128, 2, 8, 512], BF16, name="hT", nbuf=2)
        pT = K.ps([128, 2, 512], F32, name="pT", nbuf=2)
        pF = K.ps([128, 2, 512], F32, name="pF", nbuf=2)
        pF2 = K.ps([128, 2, 512], F32, name="pF2", nbuf=2)
        pS = K.ps([128, 512], F32, name="pS")
        pR = K.ps([128, 512], F32, name="pR")
        raw = K.sb([128, 512], F32, name="raw")
        sq = K.sb([128, 512], F32, name="sq")
        t1 = K.sb([128, 512], F32, name="t1")
        t2 = K.sb([128, 512], F32, name="t2")
        rs = K.sb([128, 512], F32, name="rs")
        ob = K.sb([128, 2, 512], BF16, name="ob", nbuf=2)
        of = K.sb([128, 2, 512], F32, name="of", nbuf=2)
        obt = K.sb([128, 2, 1024], BF16, name="obt", nbuf=2)
        sgt = K.sb([128, 2, 256], F32, name="sgt", nbuf=2)
        tmB = K.sb([128, 256], F32, name="tmB")
        blocks = [(0, TC, 1)] + [(TC + 512 * i, 512, 0) for i in range(8)]
        for r_ in (0, 1):
            MODv(r_, 0)
            MODv(r_, 1)
        cnts = {"fm": 0, "tm": 0}

        def norm_part(bi):
            (t0, Wd, r) = blocks[bi]
            par = bi % 2
            nti = Wd // 128
            for i in range(nti):
                xv = xt.b(i % 2)[:, i % 2, :]
                K.dma("sp", xv, Xsrc.b(0)[t0 + i * 128:t0 + (i + 1) * 128, :])
                sv = st.b(i)
                K.act(tmpn[:, :], xv, AF.Square, accum=sv[:, 0:1])
                K.rstd(sv[:, 2:3], sv[:, 0:1], D, sv[:, 1:2])
                K.stt(tmpn[:, :], xv, sv[:, 2:3], MODv(r, 1), ALU.mult, ALU.mult)
                K.tt("pool", hb2.b(par * 4 + i)[:, par, i, :], tmpn[:, :], MODv(r, 0), ALU.add)
            for k in range(8):
                for i in range(nti):
                    K.mm(pT.b(k)[:, k % 2, i * 128:(i + 1) * 128], hb2.b(par * 4 + i)[:, par, i, k * 128:(k + 1) * 128], identb)
                K.copy("act", hT2.b(par)[:, par, k, 0:Wd], pT.b(k)[:, k % 2, 0:Wd])

        def fm_part(bi):
            (t0, Wd, r) = blocks[bi]
            par = bi % 2
            K.dma("sp", cosS[:, 0:Wd], cosD[:, t0:t0 + Wd])
            K.dma("sp", sinS[:, 0:Wd], sinD[:, t0:t0 + Wd])
            for rc, c0 in enumerate(FM_ROWS):
                s = cnts["fm"] % 2
                cnts["fm"] += 1
                cf = cnts["fm"]
                pf = pF.b(s)[:, s, 0:Wd]
                for k in range(8):
                    K.mm(pf, winb[:, k, c0:c0 + 128], hT2.b(par)[:, par, k, 0:Wd], start=(k == 0), stop=(k == 7))
                if rc < 8:
                    j = 0 if rc < 4 else 1
                    K.copy("act", raw[:, 0:Wd], pf)
                    K.tt("pool", sq[:, 0:Wd], raw[:, 0:Wd], raw[:, 0:Wd], ALU.mult)
                    K.mm(pS[:, 0:Wd], blk1, sq[:, 0:Wd])
                    K.mm(pR[:, 0:Wd], permg[:, j, :], raw[:, 0:Wd])
                    K.rstd(rs[:, 0:Wd], pS[:, 0:Wd], 64, sq[:, 0:Wd])
                    K.stt(t1[:, 0:Wd], raw[:, 0:Wd], gq[:, j:j + 1], cosS[:, 0:Wd], ALU.mult, ALU.mult)
                    K.tt("dve", t2[:, 0:Wd], pR[:, 0:Wd], sinS[:, 0:Wd], ALU.mult)
                    K.tt("pool", t1[:, 0:Wd], t1[:, 0:Wd], t2[:, 0:Wd], ALU.add)
                    K.tt("dve", ob.b(s)[:, s, 0:Wd], t1[:, 0:Wd], rs[:, 0:Wd], ALU.mult)
                    dst = (QT if j == 0 else KT)
                    rr = (rc % 4) * 128
                    K.dma("sp", dst.b(cf)[rr:rr + 128, t0:t0 + Wd], ob.b(s)[:, s, 0:Wd])
                elif rc < 12:
                    j = (rc - 8) // 2
                    K.act(ob.b(s)[:, s, 0:Wd], pf, AF.Copy, scale=(1.0 if j == 0 else 0.125))
                    dst = RQT if j == 0 else RKT
                    rr = ((rc - 8) % 2) * 128
                    K.dma("sp", dst.b(cf)[rr:rr + 128, t0:t0 + Wd], ob.b(s)[:, s, 0:Wd])
                else:
                    j = (rc - 12) // 2
                    K.copy("act", of.b(s)[:, s, 0:Wd], pf)
                    dst = XUd if j == 0 else GUd
                    rr = ((rc - 12) % 2) * 128
                    K.dma("sp", dst.b(cf)[rr:rr + 128, t0:t0 + Wd], of.b(s)[:, s, 0:Wd])

        def tm_part(bi):
            (t0, Wd, r) = blocks[bi]
            par = bi % 2
            for i in range(Wd // 128):
                tok = t0 + i * 128
                ti = tok // 128
                s = cnts["tm"] % 2
                cnts["tm"] += 1
                for g, (c0, cw) in enumerate(((1024, 512), (1792, 512), (2304, 256))):
                    pf = pF2.b(g)[:, g % 2, 0:cw]
                    for k in range(8):
                        K.mm(pf, hT2.b(par)[:, par, k, i * 128:(i + 1) * 128], winb[:, k, c0:c0 + cw], start=(k == 0), stop=(k == 7))
                    if g == 0:
                        K.copy("act", obt.b(s)[:, s, 0:512], pf)
                        K.dma("sp", Vd.b(ti)[tok:tok + 128, :], obt.b(s)[:, s, 0:512])
                    elif g == 1:
                        K.act(obt.b(s)[:, s, 512:768], pF2.b(g)[:, g % 2, 0:256], AF.Copy, scale=0.125)
                        K.copy("dve", obt.b(s)[:, s, 768:1024], pF2.b(g)[:, g % 2, 256:512])
                        K.dma("sp", RKd.b(ti)[tok:tok + 128, :], obt.b(s)[:, s, 512:768])
                        K.dma("sp", RVd.b(ti)[tok:tok + 128, :], obt.b(s)[:, s, 768:1024])
                    else:
                        K.sigmoid(sgt.b(s)[:, s, :], pf, tmB[:, 0:256])
                        K.tt("dve", sgt.b(s)[:, s, :], sgt.b(s)[:, s, :], pf, ALU.mult)
                        K.dma("sp", SGd.b(ti)[tok:tok + 128, :], sgt.b(s)[:, s, :])

        def tm_and_next(bi):
            tm_part(bi)
            if bi + 1 < len(blocks):
                norm_part(bi + 1)

        norm_part(0)
        for bi in range(len(blocks)):
            K.corun(lambda: fm_part(bi), lambda: tm_and_next(bi))
        K.pop()
        if stage_done(f"proj{L}"):
            break

        def side_fn():
            K.push()
            lgr = K.sb([128, 8], F32, name="lgr")
            bc(lgr[:, :], W["ret_log_decay"].ap[L])
            lgc = K.sb([128, 4], F32, name="lgc")
            for dr_ in range(2):
                for hp in range(2):
                    for e in range(2):
                        hd = dr_ * 4 + hp * 2 + e
                        bc(lgc[e * 64:(e + 1) * 64, dr_ * 2 + hp:dr_ * 2 + hp + 1], W["ret_log_decay"].ap[L, hd:hd + 1], n=64)
            rng = K.sb([128, 64], F32, name="rng")
            bc(rng[:, :], W["ret_norm_g"].ap[L])
            pcol = K.sb([128, 4], F32, name="pcol")
            K.copy("dve", pcol[:, 0:1], pidx)
            K.ts("dve", pcol[:, 1:2], pidx, -1.0, 127.0, ALU.mult, ALU.add)
            inner = K.sb([128, 2, 4], F32, name="inner")
            K.act(inner[:, 0, :], lgr[:, 0:4], AF.Exp, scale=pcol[:, 1:2])
            K.act(inner[:, 1, :], lgr[:, 4:8], AF.Exp, scale=pcol[:, 0:1])
            cdt = K.sb([128, 2, 4], F32, name="cdt")
            K.act(cdt[:, :, :].m(lambda a: a.rearrange("p a b -> p (a b)")), lgr[:, :], AF.Exp, scale=128.0)
            crossT = K.sb([128, 2, 2, 128], F32, name="crossT")
            jrow = K.sb([128, 2, 128], F32, name="jrow")
            K.ts("dve", jrow[:, 0, :], iota[:, 0:128], 1.0, None, ALU.add)
            K.ts("dve", jrow[:, 1, :], iota[:, 0:128], -1.0, 128.0, ALU.mult, ALU.add)
            for dr_ in range(2):
                for hp in range(2):
                    K.act(crossT[:, dr_, hp, :], jrow[:, dr_, :], AF.Exp, scale=lgc[:, dr_ * 2 + hp:dr_ * 2 + hp + 1])
            dif = K.sb([128, 128], F32, name="dif")
            K.ts("dve", dif[:, :], iota[:, 0:128], pidx, None, ALU.subtract)
            dpos = K.sb([128, 128], F32, name="dpos")
            dneg = K.sb([128, 128], F32, name="dneg")
            K.ts("dve", dpos[:, :], dif[:, :], 0.0, None, ALU.max)
            K.ts("dve", dneg[:, :], dif[:, :], -1.0, 0.0, ALU.mult, ALU.max)
            mge = K.sb([128, 128], F32, name="mge")
            mlt = K.sb([128, 128], F32, name="mlt")
            K.ts("dve", mge[:, :], dif[:, :], 0.0, None, ALU.is_ge)
            K.ts("dve", mlt[:, :], dif[:, :], 0.0, None, ALU.is_lt)
            Dfb = K.sb([128, 4, 128], F32, name="Dfb")
            dtmp = K.sb([128, 128], F32, name="dtmp")
            for h in range(4):
                K.act(dtmp[:, :], dpos[:, :], AF.Exp, scale=lgr[:, h:h + 1])
                K.tt("dve", Dfb[:, h, :], dtmp[:, :], mge[:, :], ALU.mult)
                K.act(dtmp[:, :], dneg[:, :], AF.Exp, scale=lgr[:, 4 + h:5 + h])
                K.tt("dve", dtmp[:, :], dtmp[:, :], mlt[:, :], ALU.mult)
                K.tt("dve", Dfb[:, h, :], Dfb[:, h, :], dtmp[:, :], ALU.add)
            cdtab = K.sb([128, 2, 256], F32, name="cdtab")
            for dr_ in range(2):
                K.copy("dve", cdtab[:, dr_, :].m(lambda a: a.rearrange("p (h v) -> p h v", v=64)),
                       cdt[:, dr_, :].m(lambda a: a.unsqueeze(2).to_broadcast([128, 4, 64])))
            rk = K.sb([128, 2, 256], BF16, name="rk", nbuf=2)
            rv = K.sb([128, 2, 256], BF16, name="rv", nbuf=2)
            rvi = K.sb([128, 2, 2, 256], BF16, name="rvi", nbuf=4)
            PB = K.ps([128, 2, 512], F32, name="PB", nbuf=6)
            KVb = K.sb([128, NT, 256], F32, name="KVb", nbuf=NT)
            SF = K.sb([128, 2, NT + 1, 256], BF16, name="SF", nbuf=2 * (NT + 1))
            s32 = K.sb([128, 2, 256], F32, name="s32", nbuf=2)
            stmp = K.sb([128, 256], F32, name="stmp")
            K.memset("dve", s32[:, 0, :], 0.0)
            K.memset("dve", s32.b(1)[:, 1, :], 0.0)

            def sfv(dr_, c):
                return SF.b(dr_ * (NT + 1) + c)[:, dr_, c, :]
            K.memset("pool", sfv(0, 0), 0.0)
            for c in range(NT):
                s = c % 2
                K.dma("sp", rk.b(s)[:, s, :], RKd.b(c)[c * 128:(c + 1) * 128, :])
                K.dma("sp", rv.b(s)[:, s, :], RVd.b(c)[c * 128:(c + 1) * 128, :])
                for dr_ in range(2):
                    K.tt("pool" if dr_ == 0 else "dve",
                         rvi.b(s * 2 + dr_)[:, s, dr_, :].m(lambda a: a.rearrange("p (h v) -> p h v", v=64)),
                         rv.b(s)[:, s, :].m(lambda a: a.rearrange("p (h v) -> p h v", v=64)),
                         inner[:, dr_, :].m(lambda a: a.unsqueeze(2).to_broadcast([128, 4, 64])), ALU.mult)
                    for hp in range(2):
                        K.mm(PB.b(dr_)[:, dr_, hp * 128:(hp + 1) * 128], rk.b(s)[:, s, hp * 128:(hp + 1) * 128],
                             rvi.b(s * 2 + dr_)[:, s, dr_, hp * 128:(hp + 1) * 128])
                K.tt("pool", stmp[:, :], s32[:, 0, :], cdtab[:, 0, :], ALU.mult)
                K.tt("dve", s32[:, 0, :], stmp[:, :], PB.b(0)[:, 0, 0:256], ALU.add)
                K.copy("act", sfv(0, c + 1), s32[:, 0, :])
                K.copy("act", KVb.b(c)[:, c, :], PB.b(1)[:, 1, 0:256])
            border = [1, 0] + list(range(NT - 1, 1, -1))
            K.memset("pool", sfv(1, border[0]), 0.0)
            for i in range(len(border) - 1):
                c, cn = border[i], border[i + 1]
                K.tt("pool", stmp[:, :], s32.b(1)[:, 1, :], cdtab[:, 1, :], ALU.mult)
                K.tt("dve", s32.b(1)[:, 1, :], stmp[:, :], KVb.b(c)[:, c, :], ALU.add)
                K.copy("act", sfv(1, cn), s32.b(1)[:, 1, :])
            rq = K.sb([128, 2, 2, 128], BF16, name="rq", nbuf=2)
            rkt = K.sb([128, 2, 2, 128], BF16, name="rkt", nbuf=2)
            sg = K.sb([128, 2, 256], F32, name="sg", nbuf=2)
            qc = K.sb([128, 2, 2, 2, 128], BF16, name="qc", nbuf=4)
            AT = K.sb([128, 2, 4, 128], BF16, name="AT", nbuf=2)
            osb = K.sb([128, 256], F32, name="osb")
            rsq2 = K.sb([128, 2, 256], F32, name="rsq", nbuf=2)
            rss2 = K.sb([128, 2, 12], F32, name="rss", nbuf=2)
            gs2 = K.sb([128, 2, 256], F32, name="gs", nbuf=2)
            ro12 = K.sb([128, 2, 256], F32, name="ro1", nbuf=2)
            osb2 = K.sb([128, 2, 256], F32, name="osb2", nbuf=2)
            rob = K.sb([128, 2, 256], BF16, name="rob", nbuf=2)
            rT = K.sb([128, 2, 256], BF16, name="rT", nbuf=2)
            chunks = list(range(NT)) if need_ctx else list(range(2, NT))
            def ret_chunk(c):
                s = c % 2
                rsq, rss, gs, ro1, osb = rsq2.b(s)[:, s], rss2.b(s)[:, s], gs2.b(s)[:, s], ro12.b(s)[:, s], osb2.b(s)[:, s]
                K.dma("sp", rq.b(s)[:, s, :, :], RQT.b(0).m(lambda a: a[:, c * 128:(c + 1) * 128].rearrange("(k p) t -> p k t", p=128)))
                K.dma("sp", rkt.b(s)[:, s, :, :], RKT.b(0).m(lambda a: a[:, c * 128:(c + 1) * 128].rearrange("(k p) t -> p k t", p=128)))
                K.dma("sp", rv.b(s)[:, s, :], RVd.b(c)[c * 128:(c + 1) * 128, :])
                K.dma("sp", sg.b(s)[:, s, :], SGd.b(c)[c * 128:(c + 1) * 128, :])
                for dr_ in range(2):
                    K.tt("dve" if dr_ == 0 else "pool", qc.b(s * 2 + dr_)[:, s, dr_, :, :], rq.b(s)[:, s, :, :], crossT[:, dr_, :, :], ALU.mult)
                for h in range(4):
                    e, hp = h % 2, h // 2
                    K.mm(PB.b(e)[:, e, hp * 128:(hp + 1) * 128], rkt.b(s)[e * 64:(e + 1) * 64, s, hp, :], rq.b(s)[e * 64:(e + 1) * 64, s, hp, :])
                for e in range(2):
                    K.tt("dve", AT.b(s)[:, s, :, :].m(lambda a: a.rearrange("p (hp e) n -> p hp e n", e=2)[:, :, e, :]),
                         PB.b(e)[:, e, 0:256].m(lambda a: a.rearrange("p (hp n) -> p hp n", n=128)),
                         Dfb[:, :, :].m(lambda a: a.rearrange("p (hp e) n -> p hp e n", e=2)[:, :, e, :]), ALU.mult)
                for h in range(4):
                    e, hp = h % 2, h // 2
                    po = PB.b(2 + e)[:, e, 256 + hp * 64:256 + (hp + 1) * 64]
                    K.mm(po, AT.b(s)[:, s, h, :], rv.b(s)[:, s, h * 64:(h + 1) * 64], start=True, stop=False)
                    for dr_ in range(2):
                        K.mm(po, qc.b(s * 2 + dr_)[e * 64:(e + 1) * 64, s, dr_, hp, :],
                             sfv(dr_, c)[e * 64:(e + 1) * 64, hp * 128 + e * 64:hp * 128 + (e + 1) * 64], start=False, stop=(dr_ == 1))
                for e in range(2):
                    K.copy("act", osb[:, :].m(lambda a: a.rearrange("p (hp e v) -> p hp e v", e=2, v=64)[:, :, e, :]),
                           PB.b(2 + e)[:, e, 256:384].m(lambda a: a.rearrange("p (hp v) -> p hp v", v=64)))
                K.act(rsq[:, :], osb[:, :], AF.Square)
                K.reduce(rss[:, 0:4], rsq[:, :].m(lambda a: a.rearrange("p (h v) -> p h v", v=64)), ALU.add)
                K.rstd(rss[:, 8:12], rss[:, 0:4], 64, rss[:, 4:8])
                K.tt("pool", gs[:, :].m(lambda a: a.rearrange("p (h v) -> p h v", v=64)),
                     sg.b(s)[:, s, :].m(lambda a: a.rearrange("p (h v) -> p h v", v=64)),
                     rng[:, :].m(lambda a: a.unsqueeze(1).to_broadcast([128, 4, 64])), ALU.mult)
                K.tt("dve", ro1[:, :].m(lambda a: a.rearrange("p (h v) -> p h v", v=64)),
                     osb[:, :].m(lambda a: a.rearrange("p (h v) -> p h v", v=64)),
                     rss[:, 8:12].m(lambda a: a.unsqueeze(2).to_broadcast([128, 4, 64])), ALU.mult)
                K.tt("pool", rob.b(s)[:, s, :], ro1[:, :], gs[:, :], ALU.mult)
                for j in range(2):
                    K.mm(PB.b(4 + j)[:, j, 384:512], rob.b(s)[:, s, j * 128:(j + 1) * 128], identb)
                for j in range(2):
                    K.copy("act", rT.b(s)[:, s, j * 128:(j + 1) * 128], PB.b(4 + j)[:, j, 384:512])
                for j in range(2):
                    K.dma("sp", MIXT.b(c * 2 + j)[512 + j * 128:512 + (j + 1) * 128, c * 128:(c + 1) * 128], rT.b(s)[:, s, j * 128:(j + 1) * 128])

            def run_par(par):
                for c in chunks:
                    if c % 2 == par:
                        ret_chunk(c)
            for c in chunks:
                ret_chunk(c)
            K.pop()

            K.push()
            cw = K.sb([128, 2, 4], F32, name="cw")
            cb = K.sb([128, 2], F32, name="cb")
            gb = K.sb([128, 4, 2], F32, name="gb")
            lam = K.sb([128, 4], F32, name="lam")
            lng = K.sb([128, 2], F32, name="lng")
            for c in range(2):
                for j in range(4):
                    col(cw[:, c, j:j + 1], W["lru_conv_w"].ap[L, j, c * 128:(c + 1) * 128])
                col(cb[:, c:c + 1], W["lru_conv_b"].ap[L, c * 128:(c + 1) * 128])
                col(lng[:, c:c + 1], W["lru_norm_g"].ap[L, c * 128:(c + 1) * 128])
                for dg in range(4):
                    col(gb[:, dg, c:c + 1], W["lru_gate_b"].ap[L, dg, c * 128:(c + 1) * 128])
                for dr_ in range(2):
                    col(lam[:, dr_ * 2 + c:dr_ * 2 + c + 1], W["lru_lambda"].ap[L, dr_, c * 128:(c + 1) * 128])
            wbd = K.sb([128, 8, 128], F32, name="wbd")
            K.memset("dve", wbd[:, :, :], 0.0)
            for dr_ in range(2):
                for g in range(2):
                    for c in range(2):
                        for e in range(2):
                            K.dma("sp", wbd[e * 64:(e + 1) * 64, (dr_ * 2 + g) * 2 + c, e * 64:(e + 1) * 64],
                                  W["lru_gate_w"].b(0).m(lambda a: a[L, dr_, g, 2 * c + e, :, :]))
            sp = K.sb([128, 8, 4], F32, name="sp")
            K.ts("dve", sp[:, 5, :], lam[:, :], -1.0, None, ALU.mult)
            K.tt("dve", sp[:, 0, :], lam[:, :], sp[:, 5, :], ALU.max)
            K.act(sp[:, 1, :], sp[:, 0, :], AF.Exp, scale=-1.0)
            K.ts("dve", sp[:, 2, :], sp[:, 1, :], 2.0, None, ALU.add)
            K.recip(sp[:, 2, :], sp[:, 2, :])
            K.tt("dve", sp[:, 2, :], sp[:, 2, :], sp[:, 1, :], ALU.mult)
            K.tt("dve", sp[:, 3, :], sp[:, 2, :], sp[:, 2, :], ALU.mult)
            K.memset("dve", sp[:, 4, :], 1.0 / 15.0)
            for n_ in (13, 11, 9, 7, 5, 3, 1):
                K.tt("dve", sp[:, 4, :], sp[:, 4, :], sp[:, 3, :], ALU.mult)
                K.ts("dve", sp[:, 4, :], sp[:, 4, :], 1.0 / n_, None, ALU.add)
            K.tt("dve", sp[:, 4, :], sp[:, 4, :], sp[:, 2, :], ALU.mult)
            K.ts("dve", sp[:, 5, :], lam[:, :], -1.0, 0.0, ALU.mult, ALU.max)
            K.stt(sp[:, 6, :], sp[:, 4, :], 2.0, sp[:, 5, :], ALU.mult, ALU.add)
            K.ts("dve", sp[:, 7, :], sp[:, 6, :], -8.0, None, ALU.mult)
            ccoef = sp[:, 7, :]
            PADL = TC + 3
            xu = K.sb([128, 2, TT + 6], F32, name="xu")
            K.memset("pool", xu[:, :, :], 0.0)
            for c in range(2):
                K.dma("sp", xu[:, c, 2:2 + TC], XUd.b(0)[c * 128:(c + 1) * 128, 0:TC])
                K.dma("sp", xu[:, c, PADL + 2:PADL + 2 + TL], XUd.b(0)[c * 128:(c + 1) * 128, TC:TT])
            u = K.sb([128, 2, TT], F32, name="u")
            for c in range(2):
                for (pb, t0, Ln) in ((2, 0, TC), (PADL + 2, TC, TL)):
                    e = "dve"
                    K.ts(e, u[:, c, t0:t0 + Ln], xu[:, c, pb - 2:pb - 2 + Ln], cw[:, c, 0:1], cb[:, c:c + 1], ALU.mult, ALU.add)
                    for j in range(1, 4):
                        K.stt(u[:, c, t0:t0 + Ln], xu[:, c, pb - 2 + j:pb - 2 + j + Ln], cw[:, c, j:j + 1], u[:, c, t0:t0 + Ln], ALU.mult, ALU.add)
            hf = xu
            pG = K.ps([128, 2, 512], F32, name="pG", nbuf=2)
            gr_2 = K.sb([128, 2, 512], F32, name="gr", nbuf=2)
            gtmp_2 = K.sb([128, 2, 512], F32, name="gtmp", nbuf=2)
            ngb = K.sb([128, 4, 2], F32, name="ngb")
            K.ts("dve", ngb[:, :, :], gb[:, :, :], -1.0, None, ALU.mult)
            gi_2 = K.sb([128, 2, 512], F32, name="gi", nbuf=2)
            ga_2 = K.sb([128, 2, 512], F32, name="ga", nbuf=2)
            gw_2 = K.sb([128, 2, 512], F32, name="gw", nbuf=2)
            gbv_2 = K.sb([128, 2, 512], F32, name="gbv", nbuf=2)
            ar_2 = K.sb([128, 2, 512], F32, name="ar", nbuf=2)
            br_2 = K.sb([128, 2, 512], F32, name="br", nbuf=2)
            hbr = K.sb([128, 2, 512], F32, name="hbr", nbuf=2)
            hst = K.sb([128, 2], F32, name="hst", nbuf=2)
            yc = K.sb([128, 2, 512], F32, name="yc", nbuf=2)
            gu = K.sb([128, 2, 512], F32, name="gu", nbuf=2)
            g2_2 = K.sb([128, 2, 512], F32, name="g2", nbuf=2)
            ysq = K.sb([128, 2, 512], F32, name="ysq", nbuf=2)
            yrs = K.sb([128, 512], F32, name="yrs")
            yob = K.sb([128, 2, 512], BF16, name="yob", nbuf=2)

            def tmps(c):
                return [t_.b(c)[:, c] for t_ in (gr_2, gtmp_2, gi_2, ga_2, gw_2, gbv_2, ar_2, br_2, g2_2)]

            def gates(dr_, c, t0, Wd):
                gr, gtmp, gi, ga, gw, gbv, ar, br, g2 = tmps(c)
                for g, dst in ((0, gr), (1, gi)):
                    K.mm(pG.b(c)[:, c, 0:Wd], wbd[:, (dr_ * 2 + g) * 2 + c, :], u[:, c, t0:t0 + Wd])
                    K.sigmoid(dst[:, 0:Wd], pG.b(c)[:, c, 0:Wd], gtmp[:, 0:Wd], nbias=ngb[:, dr_ * 2 + g, c:c + 1])
                K.act(ga[:, 0:Wd], gr[:, 0:Wd], AF.Exp, scale=ccoef[:, dr_ * 2 + c:dr_ * 2 + c + 1])
                K.tt("pool", gw[:, 0:Wd], ga[:, 0:Wd], ga[:, 0:Wd], ALU.mult)
                K.ts("dve", gw[:, 0:Wd], gw[:, 0:Wd], -1.0, 1.0, ALU.mult, ALU.add)
                K.act(gw[:, 0:Wd], gw[:, 0:Wd], AF.Ln)
                K.act(gw[:, 0:Wd], gw[:, 0:Wd], AF.Exp, scale=0.5)
                K.tt("pool", gbv[:, 0:Wd], gi[:, 0:Wd], u[:, c, t0:t0 + Wd], ALU.mult)
                K.tt("dve", gbv[:, 0:Wd], gbv[:, 0:Wd], gw[:, 0:Wd], ALU.mult)

            lblocks = [(0, TC)] + [(TC + 512 * i, 512) for i in range(8)]

            def fwd_chain(c):
                gr, gtmp, gi, ga, gw, gbv, ar, br, g2 = tmps(c)
                for bi, (t0, Wd) in enumerate(lblocks):
                    gates(0, c, t0, Wd)
                    init = 0.0 if bi == 0 else hf.b(0)[:, c, t0 - 1:t0]
                    K.scan(hf.b(0)[:, c, t0:t0 + Wd], ga[:, 0:Wd], gbv[:, 0:Wd], init)
            K.corun(lambda: fwd_chain(0), lambda: fwd_chain(1))
            bblocks = [lblocks[0]] + lblocks[:0:-1]
            nyo = 0

            def bwd_blk(c, bi, t0, Wd, emit):
                gr, gtmp, gi, ga, gw, gbv, ar, br, g2 = tmps(c)
                gates(1, c, t0, Wd)
                K.copy("dve", ar[:, 0:Wd], ga[:, 0:Wd].m(lambda a: a[:, ::-1]))
                K.copy("dve", br[:, 0:Wd], gbv[:, 0:Wd].m(lambda a: a[:, ::-1]))
                init = 0.0 if bi == 0 else hst.b(c)[:, c:c + 1]
                K.scan(hbr.b(c)[:, c, 0:Wd], ar[:, 0:Wd], br[:, 0:Wd], init)
                K.copy("pool", hst.b(c)[:, c:c + 1], hbr.b(c)[:, c, Wd - 1:Wd])
                if not emit:
                    return
                K.dma("sp", gu.b(c)[:, c, 0:Wd], GUd.b(0)[c * 128:(c + 1) * 128, t0:t0 + Wd])
                K.tt("dve", yc.b(c)[:, c, 0:Wd], hf.b(0)[:, c, t0:t0 + Wd], hbr.b(c)[:, c, 0:Wd].m(lambda a: a[:, ::-1]), ALU.add)
                guv = gu.b(c)[:, c, 0:Wd]
                K.tt("pool", g2[:, 0:Wd], guv, guv, ALU.mult)
                K.ts("dve", g2[:, 0:Wd], g2[:, 0:Wd], 0.044715, 1.0, ALU.mult, ALU.add)
                K.tt("pool", g2[:, 0:Wd], g2[:, 0:Wd], guv, ALU.mult)
                K.sigmoid(g2[:, 0:Wd], g2[:, 0:Wd], gtmp[:, 0:Wd], scale=2.0 * math.sqrt(2.0 / math.pi))
                K.tt("pool", g2[:, 0:Wd], g2[:, 0:Wd], guv, ALU.mult)
                K.tt("dve", yc.b(c)[:, c, 0:Wd], yc.b(c)[:, c, 0:Wd], g2[:, 0:Wd], ALU.mult)
                K.tt("pool", ysq.b(c)[:, c, 0:Wd], yc.b(c)[:, c, 0:Wd], yc.b(c)[:, c, 0:Wd], ALU.mult)

            for bi, (t0, Wd) in enumerate(bblocks):
                emit = need_ctx or t0 >= TC
                K.corun(lambda: bwd_blk(0, bi, t0, Wd, emit), lambda: bwd_blk(1, bi, t0, Wd, emit))
                if not emit:
                    continue
                for c in range(2):
                    K.mm(pG.b(0)[:, 0, 0:Wd], ones, ysq.b(c)[:, c, 0:Wd], start=(c == 0), stop=(c == 1))
                K.rstd(yrs[:, 0:Wd], pG.b(0)[:, 0, 0:Wd], 256, ysq.b(0)[:, 0, 0:Wd])
                for c in range(2):
                    K.stt(yob.b(c)[:, c, 0:Wd], yc.b(c)[:, c, 0:Wd], lng[:, c:c + 1], yrs[:, 0:Wd], ALU.mult, ALU.mult)
                    nyo += 1
                    K.dma("sp", MIXT.b(nyo)[768 + c * 128:768 + (c + 1) * 128, t0:t0 + Wd], yob.b(c)[:, c, 0:Wd])
            K.pop()


        K.push()
        lamb = K.sb([128, 256], F32, name="lamb")
        bc(lamb[:, :], W["att_lambda"].ap[L])
        lt = K.sb([128, 8], F32, name="lt")
        lj = K.sb([128, 64], F32, name="lj")
        K.tt("dve", lj[:, :], lamb[:, 0:64], lamb[:, 64:128], ALU.mult)
        K.reduce(lt[:, 0:1], lj[:, :], ALU.add)
        K.tt("dve", lj[:, :], lamb[:, 128:192], lamb[:, 192:256], ALU.mult)
        K.reduce(lt[:, 1:2], lj[:, :], ALU.add)
        K.act(lt[:, 2:4], lt[:, 0:2], AF.Exp)
        K.tt("dve", lt[:, 4:5], lt[:, 3:4], lt[:, 2:3], ALU.subtract)
        K.ts("dve", lt[:, 5:6], lt[:, 4:5], -lam_init, None, ALU.add)
        neglam = lt[:, 5:6]
        gsub = K.sb([128, 1], F32, name="gsub")
        col(gsub[:, :], W["att_subln_g"].ap[L])
        K.ts("dve", gsub[:, :], gsub[:, :], 1.0 - lam_init, None, ALU.mult)
        QTh = K.sb([128, TT], BF16, name="QTh")
        KTh = K.sb([128, TT], BF16, name="KTh")
        Vh = K.sb([128, NT, 128], BF16, name="Vh")
        pSs = K.ps([128, 4, 512], F32, name="pSs", nbuf=4)
        pO = K.ps([128, 2, 512], F32, name="pO", nbuf=2)
        PTt = K.sb([128, 4, 512], BF16, name="PTt", nbuf=4)
        rl = K.sb([128, 2, 512], F32, name="rl", nbuf=2)
        pacc = K.sb([128, 2, 512], F32, name="pacc", nbuf=2)
        o0 = K.sb([128, 512], F32, name="o0")
        o1 = K.sb([128, 512], F32, name="o1")
        osq = K.sb([128, 512], F32, name="osq")
        ors = K.sb([128, 512], F32, name="ors")
        aob = K.sb([128, 2, 512], BF16, name="aob", nbuf=2)
        qblocks = [(TC + 512 * i, 512, list(range(NT))) for i in range(8)]
        if need_ctx:
            qblocks = [(0, TC, [0, 1])] + qblocks
        nqb = 0
        K.side = K.spawn(side_fn)
        for h in range(4):
            K.dma("sp", QTh[:, :], QT.b(0)[h * 128:(h + 1) * 128, :])
            K.dma("sp", KTh[:, :], KT.b(0)[h * 128:(h + 1) * 128, :])
            K.dma("sp", Vh[:, :, :], Vd.b(0).m(lambda a: a[:, h * 128:(h + 1) * 128].rearrange("(c p) v -> p c v", p=128)))
            for (t0, Wd, kcs) in qblocks:
                def st_mm(ci):
                    kc = kcs[ci]
                    for m in range(2):
                        sl = (ci % 2) * 2 + m
                        K.mm(pSs.b(sl)[:, sl, 0:Wd], KTh[m * 64:(m + 1) * 64, kc * 128:(kc + 1) * 128],
                             QTh[m * 64:(m + 1) * 64, t0:t0 + Wd])
                st_mm(0)
                for ci, kc in enumerate(kcs):
                    if ci + 1 < len(kcs):
                        st_mm(ci + 1)
                    K.side.step(SIDE_A)
                    par = ci % 2
                    K.op("act", lambda E, par=par: E.activation(out=PTt.ap[:, par * 2:par * 2 + 2, 0:Wd], in_=pSs.ap[:, par * 2:par * 2 + 2, 0:Wd],
                                                                func=AF.Exp, scale=0.125),
                         [PTt.b(par * 2), PTt.b(par * 2 + 1)], [pSs.b(par * 2), pSs.b(par * 2 + 1)])
                    K.side.step(SIDE_B)
                    for m in range(2):
                        sl = par * 2 + m
                        K.mm(pO.b(m)[:, m, 0:Wd], Vh[:, kc, :], PTt.b(sl)[:, sl, 0:Wd], start=(ci == 0), stop=(ci == len(kcs) - 1))
                    if ci == 0:
                        K.op("dve", lambda E, par=par: E.tensor_copy(out=pacc.ap[:, :, 0:Wd], in_=PTt.ap[:, par * 2:par * 2 + 2, 0:Wd]),
                             [pacc.b(0), pacc.b(1)], [PTt.b(par * 2), PTt.b(par * 2 + 1)])
                    else:
                        K.op("dve", lambda E, par=par: E.tensor_tensor(out=pacc.ap[:, :, 0:Wd], in0=pacc.ap[:, :, 0:Wd],
                                                                      in1=PTt.ap[:, par * 2:par * 2 + 2, 0:Wd], op=ALU.add),
                             [pacc.b(0), pacc.b(1)], [pacc.b(0), pacc.b(1), PTt.b(par * 2), PTt.b(par * 2 + 1)])
                    K.side.step(SIDE_N)
                for m in range(2):
                    K.mm(pSs.b(m)[:, m, 0:Wd], ones, pacc.b(m)[:, m, 0:Wd])
                K.op("act", lambda E: E.activation(out=rl.ap[:, :, 0:Wd], in_=pSs.ap[:, 0:2, 0:Wd], func=AF.Ln),
                     [rl.b(0), rl.b(1)], [pSs.b(0), pSs.b(1)])
                K.op("act", lambda E: E.activation(out=rl.ap[:, :, 0:Wd], in_=rl.ap[:, :, 0:Wd], func=AF.Exp, scale=-1.0),
                     [rl.b(0), rl.b(1)], [rl.b(0), rl.b(1)])
                K.tt("dve", o0[:, 0:Wd], pO.b(0)[:, 0, 0:Wd], rl.b(0)[:, 0, 0:Wd], ALU.mult)
                K.tt("dve", o1[:, 0:Wd], pO.b(1)[:, 1, 0:Wd], rl.b(1)[:, 1, 0:Wd], ALU.mult)
                K.stt(o0[:, 0:Wd], o1[:, 0:Wd], neglam, o0[:, 0:Wd], ALU.mult, ALU.add)
                K.tt("pool", osq[:, 0:Wd], o0[:, 0:Wd], o0[:, 0:Wd], ALU.mult)
                K.mm(pSs.b(2)[:, 2, 0:Wd], ones, osq[:, 0:Wd])
                K.rstd(ors[:, 0:Wd], pSs.b(2)[:, 2, 0:Wd], 128, osq[:, 0:Wd])
                s = nqb % 2
                nqb += 1
                K.stt(aob.b(s)[:, s, 0:Wd], o0[:, 0:Wd], gsub[:, 0:1], ors[:, 0:Wd], ALU.mult, ALU.mult)
                K.dma("sp", MIXT.b(nqb)[h * 128:(h + 1) * 128, t0:t0 + Wd], aob.b(s)[:, s, 0:Wd])
        K.side.flush()
        del K.runners[K.side.thread]
        K.side = None
        K.pop()
        if stage_done(f"att{L}") or stage_done(f"ret{L}") or stage_done(f"lru{L}"):
            break

        K.push()
        woutb = K.sb([128, 8, D], BF16, name="woutb", nbuf=1)
        K.dma("pool", woutb[:, :, :], W["w_out"].b(0).m(lambda a: a[L].rearrange("(k p) c -> p k c", p=128)), max_dma_last_dim=4096)
        rw = K.sb([128, 8, NE], F32, name="rw")
        K.dma("sp", rw[:, :, :], W["router_w"].b(0).m(lambda a: a[L].rearrange("(k p) e -> p k e", p=128)))
        mtp = K.sb([128, 2, 8, 128], BF16, name="mtp", nbuf=2)
        x0 = K.sb([128, 2, D], F32, name="x0", nbuf=2)
        x1 = K.sb([128, 2, D], F32, name="x1", nbuf=2)
        pXp = K.ps([128, 2, 512], F32, name="pXp", nbuf=2)
        junk2 = K.sb([128, 2, D], F32, name="junk2", nbuf=2)
        st2p = K.sb([128, 2, 8], F32, name="st2", nbuf=2)
        tmp2 = K.sb([128, 2, D], F32, name="tmp2", nbuf=2)
        h2f2 = K.sb([128, 2, D], F32, name="h2f", nbuf=2)
        h2b = K.sb([128, 2, D], BF16, name="h2b", nbuf=2)
        pHp = K.ps([128, 2, 512], F32, name="pHp", nbuf=2)
        h2Tp = K.sb([128, 2, 8, 128], F32, name="h2T", nbuf=2)
        pRtp = K.ps([128, 2, 512], F32, name="pRt", nbuf=2)
        exp_ = K.sb([128, 2, NE], F32, name="ex", nbuf=2)
        oblocks = [(TC + 512 * i, 512, 0) for i in range(8)]
        if need_ctx:
            oblocks = [(0, TC, 1)] + oblocks
        for r_ in ([0, 1] if need_ctx else [0]):
            for j_ in (2, 3, 4):
                MODv(r_, j_)

        def out_tile(tok, ti, r, p):
            st2 = st2p.b(p)[:, p]
            h2f = h2f2.b(p)[:, p]
            h2T = h2Tp.b(p)[:, p]
            K.dma("sp", mtp.b(p)[:, p, :, :], MIXT.b(0).m(lambda a: a[:, tok:tok + 128].rearrange("(k p) t -> p k t", p=128)))
            K.dma("sp", x0.b(p)[:, p, :], Xsrc.b(0)[tok:tok + 128, :])
            for hf_ in range(2):
                for k in range(8):
                    K.mm(pXp.b(p)[:, p, :], mtp.b(p)[:, p, k, :], woutb[:, k, hf_ * 512:(hf_ + 1) * 512], start=(k == 0), stop=(k == 7))
                K.tt("dve", x1.b(p)[:, p, hf_ * 512:(hf_ + 1) * 512], pXp.b(p)[:, p, :], MODv(r, 2)[:, hf_ * 512:(hf_ + 1) * 512], ALU.mult)
            K.tt("pool", x1.b(p)[:, p, :], x1.b(p)[:, p, :], x0.b(p)[:, p, :], ALU.add)
            dst = (S1.b(ti)[tok:tok + 128, :] if need_ctx else yout.b(ti)[tok - TC:tok - TC + 128, :])
            K.dma("sp", dst, x1.b(p)[:, p, :])
            K.act(junk2.b(p)[:, p, :], x1.b(p)[:, p, :], AF.Square, accum=st2[:, 0:1])
            K.rstd(st2[:, 2:3], st2[:, 0:1], D, st2[:, 1:2])
            K.stt(tmp2.b(p)[:, p, :], x1.b(p)[:, p, :], st2[:, 2:3], MODv(r, 4), ALU.mult, ALU.mult)
            K.tt("pool", h2f[:, :], tmp2.b(p)[:, p, :], MODv(r, 3), ALU.add)
            K.copy("act", h2b.b(p)[:, p, :], h2f[:, :])
            K.dma("sp", H2d.b(ti)[tok:tok + 128, :], h2b.b(p)[:, p, :])
            for q in range(2):
                for k4 in range(4):
                    k = q * 4 + k4
                    K.tr(pHp.b(p)[:, p, k4 * 128:(k4 + 1) * 128], h2f[:, k * 128:(k + 1) * 128], ident)
                K.copy("act" if q == 0 else "dve", h2T[:, q * 4:(q + 1) * 4, :].m(lambda a: a.rearrange("p k t -> p (k t)")), pHp.b(p)[:, p, :])
            for k in range(8):
                K.mm(pRtp.b(p)[:, p, 0:NE], h2T[:, k, :], rw[:, k, :], start=(k == 0), stop=(k == 7))
            K.reduce(st2[:, 3:4], pRtp.b(p)[:, p, 0:NE], ALU.max)
            K.ts("dve", st2[:, 4:5], st2[:, 3:4], -1.0, None, ALU.mult)
            K.act(exp_.b(p)[:, p, :], pRtp.b(p)[:, p, 0:NE], AF.Exp, bias=st2[:, 4:5], accum=st2[:, 5:6])
            K.recip(st2[:, 6:7], st2[:, 5:6])
            K.ts("dve", PROBSp.b(ti)[:, ti, :], exp_.b(p)[:, p, :], st2[:, 6:7], None, ALU.mult)
            if PRd is not None and L == 0:
                K.dma("sp", PRd.b(ti)[tok:tok + 128, :], PROBSp.b(ti)[:, ti, :])

        def out_tiles(par):
            for (t0, Wd, r) in oblocks:
                for i in range(Wd // 128):
                    tok = t0 + i * 128
                    if (tok // 128) % 2 == par:
                        out_tile(tok, tok // 128, r, par)
        K.corun(lambda: out_tiles(0), lambda: out_tiles(1))
        K.pop()
        if stage_done(f"out{L}"):
            break

        def moe(groups, stream_rows):
            K.push()
            pTp = K.ps([NE, 512], F32, name="pTp")
            pPM = K.ps([128, 32, NE], F32, name="pPM")
            wg = K.sb([128, 2, 2, 8, D], BF16, name="wg", nbuf=4)
            wd = K.sb([128, 8, D], BF16, name="wd")
            wn = ("exp_w_gate", "exp_w_up", "exp_w_down")

            def load_w(e):
                s = e % 2
                for j in range(2):
                    K.dma("pool", wg.b(s * 2 + j)[:, s, j, :, :],
                          W[wn[j]].b(0).m(lambda a: a[L, e].rearrange("(k p) c -> p k c", p=128)), max_dma_last_dim=4096)

            def load_wd(e):
                K.dma("pool", wd[:, :, :], W[wn[2]].b(0).m(lambda a: a[L, e].rearrange("(k p) c -> p k c", p=128)),
                      max_dma_last_dim=4096)

            load_w(0)
            for g in groups:
                row0, ntk, cap = g["row0"], g["ntk"], g["cap"]
                Tn = ntk * 128
                ti0 = row0 // 128
                g["ncj"] = (cap + 127) // 128
                g["cwj"] = min(cap, 128)
                PM = K.sb([128, ntk, NE], F32, name="PM")
                g["PM"] = PM
                K.push()
                PTm = K.sb([NE, Tn], F32, name="PTm")
                for i in range(ntk):
                    K.tr(pTp[:, (i % 4) * 128:(i % 4 + 1) * 128], PROBS[:, ti0 + i, :], ident)
                    if i % 4 == 3 or i == ntk - 1:
                        n = (i % 4 + 1) * 128
                        K.copy("act", PTm[:, (i // 4) * 512:(i // 4) * 512 + n], pTp[:, 0:n])
                K.dma("sp", PTd[:, 0:Tn], PTm[:, :])
                PT8 = K.sb([128, Tn], F32, name="PT8")
                for g8 in range(8):
                    K.dma("sp", PT8[g8 * NE:(g8 + 1) * NE, :], PTd[:, 0:Tn])
                bs = K.sb([128, 8], F32, name="bs")
                K.memset("dve", bs[:, 0:1], 0.0)
                K.memset("dve", bs[:, 1:2], 1.0)
                bj = K.sb([128, Tn], BF16, name="bj")
                for it in range(10):
                    K.tt("dve", bs[:, 2:3], bs[:, 1:2], bs[:, 0:1], ALU.subtract)
                    K.stt(bs[:, 3:4], bs[:, 2:3], cfrac, bs[:, 0:1], ALU.mult, ALU.add)
                    K.ts("dve", bj[:, :], PT8[:, :], bs[:, 3:4], 0.0, ALU.is_ge, ALU.add, accum=bs[:, 4:5])
                    K.ts("dve", bs[:, 5:6], bs[:, 4:5], float(cap), None, ALU.is_ge)
                    K.mm(pPM[:, 0, 0:1], sameE, bs[:, 5:6])
                    K.ts("dve", bs[:, 6:7], pPM[:, 0, 0:1], 0.125, 0.125, ALU.mult, ALU.add)
                    K.stt(bs[:, 1:2], bs[:, 2:3], bs[:, 6:7], bs[:, 0:1], ALU.mult, ALU.add)
                    K.ts("dve", bs[:, 6:7], pPM[:, 0, 0:1], 0.125, None, ALU.mult)
                    K.stt(bs[:, 0:1], bs[:, 2:3], bs[:, 6:7], bs[:, 0:1], ALU.mult, ALU.add)
                K.ts("dve", bj[0:NE, :], PT8[0:NE, :], bs[0:NE, 0:1], None, ALU.is_ge)
                pos = K.sb([NE, Tn], F32, name="pos")
                K.scan(pos[:, :], bj[0:NE, :], bj[0:NE, :], 0.0, op0=ALU.add, op1=ALU.max)
                K.tt("dve", pos[:, :], pos[:, :], bj[0:NE, :], ALU.mult)
                K.ts("dve", pos[:, :], pos[:, :], -1.0, None, ALU.add)
                for i in range(ntk):
                    K.tr(pPM[:, i, :], pos[:, i * 128:(i + 1) * 128], ident[0:NE, 0:NE])
                K.copy("act", PM[:, :, :], pPM[:, 0:ntk, :])
                K.pop()
                vals = K.sb([128, ntk, NE, 5], BF16, name="vals")
                g["vals"] = vals
                K.copy("dve", vals[:, :, :, 0], tidx[:, 0:ntk].m(lambda a: a.unsqueeze(2).to_broadcast([128, ntk, NE])))
                K.copy("dve", vals[:, :, :, 1], pidx.m(lambda a: a.unsqueeze(2).to_broadcast([128, ntk, NE])))
                pr = PROBS[:, ti0:ti0 + ntk, :]
                vf = K.sb([128, ntk, NE], F32, name="vf")
                r1 = K.sb([128, ntk, NE], F32, name="r1")
                K.copy("dve", vals[:, :, :, 2], pr)
                K.copy("dve", vf[:, :, :], vals[:, :, :, 2])
                K.tt("dve", r1[:, :, :], pr, vf[:, :, :], ALU.subtract)
                K.copy("dve", vals[:, :, :, 3], r1[:, :, :])
                K.copy("dve", vf[:, :, :], vals[:, :, :, 3])
                K.tt("dve", r1[:, :, :], r1[:, :, :], vf[:, :, :], ALU.subtract)
                K.copy("dve", vals[:, :, :, 4], r1[:, :, :])
                ncj = g["ncj"]
                g["idxi"] = K.sb([128, 2, 4], I32, name="idxi", nbuf=2)
                g["idxs"] = K.sb([128, 2, 4], I32, name="idxs", nbuf=2)
                g["aff"] = K.sb([128, 2, 4], F32, name="aff", nbuf=2)
                g["Xg"] = K.sb([128, ncj, D], BF16, name="Xg", nbuf=ncj)
                g["XT"] = K.sb([128, 2, 8, cap], BF16, name="XT", nbuf=2)
                g["hid"] = K.sb([128, 8, cap], BF16, name="hid", nbuf=8)
            NSEL = 6
            sel = K.sb([128, NSEL, 512], BF16, name="sel", nbuf=NSEL)
            pPMf = pPM.ap.rearrange("p t e -> p (t e)")
            pIg = [T(pTp.ap[0:5, :]), T(pPMf[0:5, 64:64 + 64])]
            pITg = [T(pPMf[:, 0:32]), T(pPMf[:, 32:40])]
            ilg = [K.sb([8, 512], F32, name="il"), K.sb([8, 64], F32, name="il2")]
            ilT = K.sb([128, 2, 4, 8], F32, name="ilT", nbuf=2)
            idxf = K.sb([128, 2, 4], F32, name="idxf", nbuf=2)
            pXT = K.ps([128, 512], F32, name="pXT")
            pGU = K.ps([128, 2, 512], F32, name="pGU", nbuf=2)
            pC = K.ps([128, 512], F32, name="pC")
            sgl = K.sb([128, 512], F32, name="sgl")
            pY = K.ps([128, 2, 512], F32, name="pY", nbuf=2)
            ysb = K.sb([128, 2, D], F32, name="ysb", nbuf=2)
            cnt = {"sel": 0, "y": 0}
            scb = [[Buf() for _ in range(8)] for _ in range(2)]
            for b_ in scb[0] + scb[1]:
                b_.w = stream_rows.buf.w

            def sel_op(e, gi, i, slot):
                g = groups[gi]
                cap = g["cap"]
                K.ts("dve", sel.b(slot)[:, slot, 0:cap], iota[:, 0:cap], g["PM"][:, i, e:e + 1], None, ALU.is_equal)

            def idx_mm(e, gi, i, slot):
                g = groups[gi]
                cap, ntk = g["cap"], g["ntk"]
                K.mm(pIg[gi][:, 0:cap], g["vals"][:, i, e, :], sel.b(slot)[:, slot, 0:cap], start=(i == 0), stop=(i == ntk - 1))

            def prep_fin(e):
                s = e % 2
                for gi, g in enumerate(groups):
                    cap, ncj, cwj = g["cap"], g["ncj"], g["cwj"]
                    il = ilg[gi]
                    K.copy("act", il[0:5, 0:cap], pIg[gi][:, 0:cap])
                    for jc in range(ncj):
                        K.tr(pITg[gi][0:cwj, jc * 8:jc * 8 + 5], il[0:5, jc * 128:jc * 128 + cwj], ident[0:5, 0:5])
                    iT = ilT.b(gi)[0:cwj, gi, 0:ncj, 0:5]
                    K.copy("act", iT, pITg[gi][0:cwj, 0:ncj * 8].m(lambda a: a.rearrange("p (j f) -> p j f", f=8)[:, :, 0:5]))
                    iF = idxf.b(gi)[0:cwj, gi, 0:ncj]
                    K.stt(iF, ilT.b(gi)[0:cwj, gi, 0:ncj, 0], 128.0, ilT.b(gi)[0:cwj, gi, 0:ncj, 1], ALU.mult, ALU.add)
                    K.ts("dve", iF, iF, float(g["srow0"]), None, ALU.add)
                    K.copy("dve", g["idxs"].b(s)[0:cwj, s, 0:ncj], iF)
                    K.ts("dve", iF, iF, float(g["row0"] - g["srow0"]), None, ALU.add)
                    K.copy("dve", g["idxi"].b(s)[0:cwj, s, 0:ncj], iF)
                    K.tt("dve", g["aff"].b(s)[0:cwj, s, 0:ncj], ilT.b(gi)[0:cwj, gi, 0:ncj, 2], ilT.b(gi)[0:cwj, gi, 0:ncj, 3], ALU.add)
                    K.tt("dve", g["aff"].b(s)[0:cwj, s, 0:ncj], g["aff"].b(s)[0:cwj, s, 0:ncj], ilT.b(gi)[0:cwj, gi, 0:ncj, 4], ALU.add)
                    for jc in range(ncj):
                        iv = g["idxi"].b(s)[0:cwj, s, jc:jc + 1]
                        Xg = g["Xg"]
                        K.dma("pool", Xg.b(jc)[0:cwj, jc, :], H2d.b(0)[:, :], extra_in=[iv],
                              fn=lambda E, jc=jc, iv=iv, Xg=Xg, cwj=cwj: E.indirect_dma_start(
                                  out=Xg.ap[0:cwj, jc, :], out_offset=None, in_=H2d.ap[:, :],
                                  in_offset=bass.IndirectOffsetOnAxis(ap=iv.ap, axis=0)))

            def sel_list(e):
                return [(gi, i) for gi, g in enumerate(groups) for i in range(g["ntk"])]

            def prep_xt(e):
                s = e % 2
                for g in groups:
                    cap, ncj, cwj = g["cap"], g["ncj"], g["cwj"]
                    for k in range(8):
                        tgt = (pXT[:, :], pY.b(0)[:, 0, :], pY.b(1)[:, 1, :])[k % 3]
                        for jc in range(ncj):
                            K.mm(tgt[:, jc * 128:jc * 128 + cwj], g["Xg"].b(jc)[0:cwj, jc, k * 128:(k + 1) * 128], identb[0:cwj, 0:cwj])
                        K.copy("act" if k % 2 == 0 else "dve", g["XT"].b(s)[:, s, k, 0:cap], tgt[:, 0:cap])

            def gate_up(e, pend):
                s = e % 2
                per = (len(pend) + 7) // 8
                for fc in range(8):
                    batch = pend[fc * per:(fc + 1) * per]
                    slots = []
                    for (gi, i) in batch:
                        slot = cnt["sel"] % NSEL
                        cnt["sel"] += 1
                        slots.append(slot)
                        sel_op(e + 1, gi, i, slot)
                    for gi, g in enumerate(groups):
                        cap = g["cap"]
                        for j in range(2):
                            po = pGU.b(j)[:, j, 0:cap] if gi == 0 else pC[:, j * 64:j * 64 + cap]
                            for k in range(8):
                                K.mm(po, wg.b(s * 2 + j)[:, s, j, k, fc * 128:(fc + 1) * 128], g["XT"].b(s)[:, s, k, 0:cap],
                                     start=(k == 0), stop=(k == 7))
                    for (gi, i), slot in zip(batch, slots):
                        idx_mm(e + 1, gi, i, slot)
                    for gi, g in enumerate(groups):
                        cap = g["cap"]
                        pg = pGU.b(0)[:, 0, 0:cap] if gi == 0 else pC[:, 0:cap]
                        pu = pGU.b(1)[:, 1, 0:cap] if gi == 0 else pC[:, 64:64 + cap]
                        K.act(sgl[:, 0:cap], pg, AF.Silu)
                        K.tt("dve", g["hid"].b(fc)[:, fc, 0:cap], sgl[:, 0:cap], pu, ALU.mult)

            def down(e):
                s = e % 2
                nsc = 0
                prevb = [V(None, b_) for b_ in scb[(e + 1) % 2]]
                for g in groups:
                    ncj, cwj = g["ncj"], g["cwj"]
                    g2b = MODv(g["r"], 5)
                    for jc in range(ncj):
                        ys = cnt["y"] % 2
                        cnt["y"] += 1
                        for hf_ in range(2):
                            for fc in range(8):
                                K.mm(pY.b(hf_)[0:cwj, hf_, :], g["hid"].b(fc)[:, fc, jc * 128:jc * 128 + cwj],
                                     wd[:, fc, hf_ * 512:(hf_ + 1) * 512], start=(fc == 0), stop=(fc == 7))
                            K.stt(ysb.b(ys)[0:cwj, ys, hf_ * 512:(hf_ + 1) * 512], pY.b(hf_)[0:cwj, hf_, :],
                                  g["aff"].b(s)[0:cwj, s, jc:jc + 1], g2b[0:cwj, hf_ * 512:(hf_ + 1) * 512], ALU.mult, ALU.mult)
                        iv = g["idxs"].b(s)[0:cwj, s, jc:jc + 1]
                        sv_ = V(stream_rows.ap, scb[e % 2][nsc])
                        nsc += 1
                        K.dma("pool", sv_, ysb.b(ys)[0:cwj, ys, :], extra_in=[iv] + prevb,
                              fn=lambda E, ys=ys, iv=iv, cwj=cwj: E.indirect_dma_start(
                                  out=stream_rows.ap, out_offset=bass.IndirectOffsetOnAxis(ap=iv.ap, axis=0),
                                  in_=ysb.ap[0:cwj, ys, :], in_offset=None, compute_op=ALU.add))

            for (gi, i) in sel_list(0):
                slot = cnt["sel"] % NSEL
                cnt["sel"] += 1
                sel_op(0, gi, i, slot)
                idx_mm(0, gi, i, slot)
            prep_fin(0)
            prep_xt(0)
            for e in range(NE):
                load_wd(e)
                if e + 1 < NE:
                    load_w(e + 1)
                gate_up(e, sel_list(e + 1) if e + 1 < NE else [])
                if e + 1 < NE:
                    prep_fin(e + 1)
                down(e)
                if e + 1 < NE:
                    prep_xt(e + 1)
            K.pop()

        glat = dict(row0=TC, ntk=TL // 128, cap=2 * TL // NE, r=0, srow0=(TC if need_ctx else 0))
        if need_ctx:
            gctx = dict(row0=0, ntk=TC // 128, cap=2 * TC // NE, r=1, srow0=0)
            moe([glat, gctx], S1[:, :])
        else:
            moe([glat], yout[:, :])
        if stage_done(f"moe{L}"):
            break

    K.barrier(["sp"])
    return nc


def _prep_inputs(inputs):
    f = lambda a: np.ascontiguousarray(np.asarray(a, dtype=np.float32))
    x, c, ctx, c_ctx = f(inputs["x"]), f(inputs["c"]), f(inputs["ctx"]), f(inputs["c_ctx"])
    cosT, sinT = host_rope()
    consts = host_consts()
    shared = dict(consts=consts, cosT=cosT, sinT=sinT)
    for n in ("mod_w", "mod_b", "norm1_g", "norm2_g", "w_in", "w_out", "att_q_norm_g", "att_k_norm_g", "att_subln_g",
              "ret_norm_g", "lru_conv_w", "lru_conv_b", "lru_gate_w", "lru_lambda", "lru_norm_g", "router_w",
              "exp_w_gate", "exp_w_up", "exp_w_down"):
        shared[n] = f(inputs[n])
    shared["att_lambda"] = f(inputs["att_lambda"]).reshape(DEPTH, 256)
    shared["ret_log_decay"] = f(inputs["ret_log_decay"]).reshape(DEPTH, 8)
    shared["lru_gate_b"] = f(inputs["lru_gate_b"]).reshape(DEPTH, 4, 256)
    maps = []
    for b in range(x.shape[0]):
        m = dict(shared)
        m["xin"] = np.ascontiguousarray(np.concatenate([ctx[b], x[b]], axis=0))
        m["cvec"] = np.ascontiguousarray(np.stack([c[b], c_ctx], axis=1))
        maps.append(m)
    return maps


_NC_CACHE = {}


def kernel(**inputs):
    maps = _prep_inputs(inputs)
    if "nc" not in _NC_CACHE:
        _NC_CACHE["nc"] = build()
    nc = _NC_CACHE["nc"]
    res = run_bass_kernel_spmd(nc, maps, core_ids=list(range(N_CORES)))
    return np.stack([np.asarray(r["y"], dtype=np.float32) for r in res.results], axis=0)
```

```python
import math
import threading
from contextlib import ExitStack
import numpy as np
import concourse.bass as bass
import concourse.mybir as mybir
from concourse.bass_utils import run_bass_kernel_spmd

F32 = mybir.dt.float32
BF16 = mybir.dt.bfloat16
I32 = mybir.dt.int32
ALU = mybir.AluOpType
AF = mybir.ActivationFunctionType
AX = mybir.AxisListType

D = 1024
TC = 256
TL = 4096
TT = TC + TL
NT = TT // 128
DEPTH = 2
EPS = 1e-6
NE = 16
N_CORES = 8


class Buf:
    __slots__ = ("w", "rs")

    def __init__(self):
        self.w = None
        self.rs = {}


class V:
    def __init__(self, ap, buf):
        self.ap = ap
        self.buf = buf

    def __getitem__(self, k):
        return V(self.ap[k], self.buf)

    def m(self, f):
        return V(f(self.ap), self.buf)


class T:
    def __init__(self, ap, nbuf=1):
        self.ap = ap
        self.bufs = [Buf() for _ in range(nbuf)]

    def __getitem__(self, k):
        return V(self.ap[k], self.bufs[0])

    def b(self, i):
        return V(self.ap, self.bufs[i % len(self.bufs)])


class SideRunner:
    def __init__(self, fn):
        self.go = threading.Semaphore(0)
        self.back = threading.Semaphore(0)
        self.budget = 0
        self.finished = False
        self.exc = None
        self.nops = 0

        def run():
            self.go.acquire()
            try:
                fn()
            except BaseException as e:
                self.exc = e
            self.finished = True
            self.back.release()
        self.thread = threading.Thread(target=run, daemon=True)
        self.thread.start()

    def tick(self):
        while self.budget <= 0:
            self.back.release()
            self.go.acquire()
        self.budget -= 1
        self.nops += 1

    def step(self, n):
        if self.finished:
            return
        self.budget = n
        self.go.release()
        self.back.acquire()
        if self.exc is not None:
            raise self.exc

    def flush(self):
        while not self.finished:
            self.step(10 ** 9)
        if self.exc is not None:
            raise self.exc


class Kern:
    CE = ("pe", "act", "dve", "pool")
    side = None

    def _gate(self):
        r = self.runners.get(threading.current_thread())
        if r is not None:
            r.tick()

    def _costep(self):
        r = self.co.get(threading.current_thread())
        if r is not None:
            r.step(1)

    def spawn(self, fn):
        r = SideRunner(fn)
        self.runners[r.thread] = r
        return r

    def corun(self, fn_a, fn_b):
        r = self.spawn(fn_b)
        me = threading.current_thread()
        self.co[me] = r
        try:
            fn_a()
        finally:
            del self.co[me]
        r.flush()
        del self.runners[r.thread]

    def __init__(self, nc):
        self.nc = nc
        self.eng = dict(pe=nc.tensor, act=nc.scalar, dve=nc.vector, pool=nc.gpsimd, sp=nc.sync)
        self.sems = []
        self.cur = {}
        self.cnt = {}
        self.seen = {e: {} for e in self.eng}
        self.nsem = 0
        for e in self.CE:
            self._new_eng_sem(e)
        self.dpool = {}
        self.dnext = {}
        self.dcnt = {}
        for q, n in (("sp", 16), ("pool", 16), ("act", 6)):
            self.dpool[q] = [self._new_sem() for _ in range(n)]
            self.dnext[q] = 0
            for s in self.dpool[q]:
                self.dcnt[s] = 0
        self.runners = {}
        self.co = {}
        self.stacks = [ExitStack()]
        self.caches = [{}]
        self.uid = 0

    def _new_sem(self):
        h = self.nc.alloc_semaphore(name=f"s{self.nsem}")
        self.nsem += 1
        self.sems.append(h)
        return len(self.sems) - 1

    def _new_eng_sem(self, e):
        self.cur[e] = self._new_sem()
        self.cnt[e] = 0

    def _wait(self, e, ev):
        if ev is None:
            return
        s, v = ev
        if v <= 0 or self.seen[e].get(s, 0) >= v:
            return
        self.eng[e].wait_ge(self.sems[s], v)
        self.seen[e][s] = v

    def _deps(self, e, reads, writes):
        for b in reads:
            self._wait(e, b.w)
        for b in writes:
            self._wait(e, b.w)
            for s, v in list(b.rs.items()):
                self._wait(e, (s, v))

    def _post(self, ev, reads, writes):
        for b in reads:
            if b.rs.get(ev[0], 0) < ev[1]:
                b.rs[ev[0]] = ev[1]
        for b in writes:
            b.w = ev
            b.rs = {}

    @staticmethod
    def _bufs(vs):
        out = []
        for v in vs:
            if isinstance(v, V) and v.buf not in out:
                out.append(v.buf)
        return out

    def op(self, e, fn, outs, ins):
        self._gate()
        reads = self._bufs(ins)
        writes = self._bufs(outs)
        self._deps(e, reads, writes)
        inst = fn(self.eng[e])
        s = self.cur[e]
        self.cnt[e] += 1
        c = self.cnt[e]
        inst.then_inc(self.sems[s], 1)
        if e == "pe":
            self.seen[e][s] = c
        self._post((s, c), reads, writes)
        if c >= 30000:
            self._new_eng_sem(e)
        self._costep()

    def dma(self, q, out, in_, extra_in=(), fn=None, **kw):
        self._gate()
        reads = self._bufs([in_] + list(extra_in))
        writes = self._bufs([out])
        pool = self.dpool[q]
        si = pool[self.dnext[q] % len(pool)]
        self.dnext[q] += 1
        self._wait(q, (si, self.dcnt[si]))
        self._deps(q, reads, writes)
        if fn is None:
            inst = self.eng[q].dma_start(out=out.ap, in_=in_.ap, **kw)
        else:
            inst = fn(self.eng[q])
        self.dcnt[si] += 16
        inst.then_inc(self.sems[si], 16)
        self._post((si, self.dcnt[si]), reads, writes)
        self._costep()

    def barrier(self, engines=None):
        for e in (engines or list(self.eng)):
            for q in self.dpool:
                for si in self.dpool[q]:
                    self._wait(e, (si, self.dcnt[si]))
            for en in self.CE:
                if en != e or en != "pe":
                    self._wait(e, (self.cur[en], self.cnt[en]))

    def push(self):
        self.stacks.append(ExitStack())
        self.caches.append({})

    def pop(self):
        self.barrier()
        self.stacks.pop().close()
        self.caches.pop()

    def sb(self, shape, dtype, nbuf=1, name=None):
        self.uid += 1
        h = self.stacks[-1].enter_context(self.nc.sbuf_tensor(f"{name or 'sb'}_{self.uid}", list(shape), dtype))
        return T(h, nbuf)

    def ps(self, shape, dtype=F32, nbuf=1, name=None):
        self.uid += 1
        h = self.stacks[-1].enter_context(self.nc.psum_tensor(f"{name or 'ps'}_{self.uid}", list(shape), dtype))
        return T(h, nbuf)

    def dr(self, name, shape, dtype, kind=None, nbuf=1):
        if kind is None:
            t = self.nc.dram_tensor(name, list(shape), dtype)
        else:
            t = self.nc.dram_tensor(name, list(shape), dtype, kind=kind)
        return T(t.ap(), nbuf)

    @staticmethod
    def _a(x):
        return x.ap if isinstance(x, V) else x

    def act(self, out, in_, func, bias=None, scale=None, accum=None):
        kw = {}
        if bias is not None:
            kw["bias"] = self._a(bias)
        if scale is not None:
            kw["scale"] = self._a(scale)
        if accum is not None:
            kw["accum_out"] = accum.ap
        self.op("act", lambda E: E.activation(out=out.ap, in_=in_.ap, func=func, **kw),
                [out, accum], [in_, bias, scale])

    def tt(self, e, out, a, b, op):
        self.op(e, lambda E: E.tensor_tensor(out=out.ap, in0=a.ap, in1=b.ap, op=op), [out], [a, b])

    def ts(self, e, out, a, s1, s2=None, op0=ALU.mult, op1=None, accum=None):
        kw = {}
        if op1 is not None:
            kw["op1"] = op1
        if accum is not None:
            kw["accum_out"] = accum.ap
        self.op(e, lambda E: E.tensor_scalar(out=out.ap, in0=a.ap, scalar1=self._a(s1), scalar2=self._a(s2),
                                              op0=op0, **kw), [out, accum], [a, s1, s2])

    def stt(self, out, a, s, b, op0, op1):
        self.op("dve", lambda E: E.scalar_tensor_tensor(out=out.ap, in0=a.ap, scalar=self._a(s), in1=b.ap,
                                                       op0=op0, op1=op1), [out], [a, s, b])

    def copy(self, e, out, in_):
        if e == "act":
            self.op(e, lambda E: E.copy(out=out.ap, in_=in_.ap), [out], [in_])
        else:
            self.op(e, lambda E: E.tensor_copy(out=out.ap, in_=in_.ap), [out], [in_])

    def memset(self, e, out, val):
        self.op(e, lambda E: E.memset(out.ap, val), [out], [])

    def recip(self, out, in_):
        self.op("dve", lambda E: E.reciprocal(out=out.ap, in_=in_.ap), [out], [in_])

    def reduce(self, out, in_, op, axis=AX.X):
        self.op("dve", lambda E: E.tensor_reduce(out=out.ap, in_=in_.ap, axis=axis, op=op), [out], [in_])

    def scan(self, out, d0, d1, init, op0=ALU.mult, op1=ALU.add):
        self.op("dve", lambda E: E.tensor_tensor_scan(out=out.ap, data0=d0.ap, data1=d1.ap, initial=self._a(init),
                                                     op0=op0, op1=op1), [out], [d0, d1, init])

    def mm(self, out, lhsT, rhs, start=True, stop=True):
        self.op("pe", lambda E: E.matmul(out.ap, lhsT.ap, rhs.ap, start=start, stop=stop), [out], [lhsT, rhs])

    def tr(self, out, in_, ident):
        self.op("pe", lambda E: E.transpose(out.ap, in_.ap, ident.ap), [out], [in_, ident])

    def rstd(self, out, ss, n, tmp):
        self.act(tmp, ss, AF.Ln, bias=self.epsc[: ss.ap.shape[0]], scale=1.0 / n)
        self.act(out, tmp, AF.Exp, scale=-0.5)

    def sigmoid(self, out, in_, tmp, nbias=None, scale=1.0):
        if nbias is None:
            self.act(tmp, in_, AF.Exp, scale=-scale)
        else:
            self.act(tmp, in_, AF.Exp, bias=nbias, scale=-scale)
        self.act(tmp, tmp, AF.Ln, bias=self.onec[: in_.ap.shape[0]])
        self.act(out, tmp, AF.Exp, scale=-1.0)


FM_ROWS = [0, 128, 256, 384, 512, 640, 768, 896, 1536, 1664, 1792, 1920, 2560, 2688, 2816, 2944]
NCONST = 128 + 128 + 128 + 512 + 64 + 1 + 1 + 128
import os
SIDE_N, SIDE_A, SIDE_B = [int(v) for v in os.environ.get('SIDE_CFG', '0,0,2').split(',')]


def host_consts():
    c = np.zeros((128, NCONST), np.float32)
    c[:, 0:128] = np.eye(128, dtype=np.float32)
    for i in range(128):
        d = i % 64
        p = d + 16 if (d % 32) < 16 else d - 16
        c[(i // 64) * 64 + p, 128 + i] = 1.0
    c[0:64, 256:320] = 1.0
    c[64:128, 320:384] = 1.0
    c[:, 384:896] = np.arange(512, dtype=np.float32)[None, :]
    c[:, 896:960] = np.arange(64, dtype=np.float32)[None, :]
    c[:, 960] = np.arange(128, dtype=np.float32)
    c[:, 961] = (np.arange(128) // 16 + 1).astype(np.float32) / 8.0
    pp = np.arange(128)
    c[:, 962:1090] = (pp[:, None] % 16 == pp[None, :] % 16).astype(np.float32)
    return c


def host_rope():
    rows = TL // 64
    row = np.repeat(np.arange(rows), 64).astype(np.float32)
    col = np.tile(np.arange(64), rows).astype(np.float32)
    inv = (1.0 / (np.float32(10000.0) ** (np.arange(0, 32, 2, dtype=np.float32) / np.float32(32)))).astype(np.float32)
    ang = np.concatenate([row[:, None] * inv, col[:, None] * inv], axis=-1).astype(np.float32)
    cos = np.cos(ang).astype(np.float32)
    sin = np.sin(ang).astype(np.float32)
    cosT = np.ones((128, TT), np.float32)
    sinT = np.zeros((128, TT), np.float32)
    for r in range(128):
        d = r % 64
        f = d % 16
        a = f if d < 32 else 16 + f
        cosT[r, TC:] = cos[:, a]
        sinT[r, TC:] = -sin[:, a] if (d % 32) < 16 else sin[:, a]
    return cosT, sinT


def build(stop_after=None, dbg=()):
    nc = bass.Bass("TRN2", target_bir_lowering=False)
    K = Kern(nc)

    def dk(name):
        return "ExternalOutput" if name in dbg else None

    xin = K.dr("xin", [TT, D], F32, "ExternalInput")
    cvec = K.dr("cvec", [D, 2], F32, "ExternalInput")
    consts = K.dr("consts", [128, NCONST], F32, "ExternalInput")
    cosD = K.dr("cosT", [128, TT], F32, "ExternalInput")
    sinD = K.dr("sinT", [128, TT], F32, "ExternalInput")
    W = {}
    for n, shp in (("mod_w", [DEPTH, D, 6 * D]), ("mod_b", [DEPTH, 6 * D]), ("norm1_g", [DEPTH, D]), ("norm2_g", [DEPTH, D]),
                   ("w_in", [DEPTH, D, 3072]), ("w_out", [DEPTH, D, D]), ("att_q_norm_g", [DEPTH, 64]),
                   ("att_k_norm_g", [DEPTH, 64]), ("att_lambda", [DEPTH, 256]), ("att_subln_g", [DEPTH, 128]),
                   ("ret_log_decay", [DEPTH, 8]), ("ret_norm_g", [DEPTH, 64]), ("lru_conv_w", [DEPTH, 4, 256]),
                   ("lru_conv_b", [DEPTH, 256]), ("lru_gate_w", [DEPTH, 2, 2, 4, 64, 64]), ("lru_gate_b", [DEPTH, 4, 256]),
                   ("lru_lambda", [DEPTH, 2, 256]), ("lru_norm_g", [DEPTH, 256]), ("router_w", [DEPTH, D, NE]),
                   ("exp_w_gate", [DEPTH, NE, D, D]), ("exp_w_up", [DEPTH, NE, D, D]), ("exp_w_down", [DEPTH, NE, D, D])):
        W[n] = K.dr(n, shp, F32, "ExternalInput")
    yout = K.dr("y", [TL, D], F32, "ExternalOutput", nbuf=NT)
    S1 = K.dr("S1", [TT, D], F32, dk("S1"), nbuf=NT)
    QT = K.dr("QT", [1024, TT], BF16, dk("QT"), nbuf=64)
    KT = K.dr("KT", [1024, TT], BF16, dk("KT"), nbuf=64)
    Vd = K.dr("Vd", [TT, 512], BF16, dk("Vd"), nbuf=NT)
    RQT = K.dr("RQT", [256, TT], BF16, dk("RQT"), nbuf=32)
    RKT = K.dr("RKT", [256, TT], BF16, dk("RKT"), nbuf=32)
    RKd = K.dr("RKd", [TT, 256], BF16, dk("RKd"), nbuf=NT)
    RVd = K.dr("RVd", [TT, 256], BF16, dk("RVd"), nbuf=NT)
    SGd = K.dr("SGd", [TT, 256], F32, dk("SGd"), nbuf=NT)
    XUd = K.dr("XUd", [256, TT], F32, dk("XUd"), nbuf=32)
    GUd = K.dr("GUd", [256, TT], F32, dk("GUd"), nbuf=32)
    MIXT = K.dr("MIXT", [1024, TT], BF16, dk("MIXT"), nbuf=64)
    H2d = K.dr("H2d", [TT, D], BF16, dk("H2d"), nbuf=NT)
    PRd = K.dr("PRd", [TT, NE], F32, dk("PRd"), nbuf=NT) if "PRd" in dbg else None

    cst = K.sb([128, NCONST], F32, name="cst")
    K.dma("sp", cst[:, :], consts[:, :])
    ident = cst[:, 0:128]
    permc = cst[:, 128:256]
    blk1 = cst[:, 256:384]
    iota = cst[:, 384:896]
    tidx = cst[:, 896:960]
    pidx = cst[:, 960:961]
    cfrac = cst[:, 961:962]
    sameE = cst[:, 962:1090]
    PTd = K.dr("PTd", [NE, TL], F32)
    identb_t = K.sb([128, 128], BF16, name="identb")
    K.copy("dve", identb_t[:, :], ident)
    identb = identb_t[:, :]
    ones_t = K.sb([128, 128], F32, name="ones")
    K.memset("dve", ones_t[:, :], 1.0)
    ones = ones_t[:, :]
    onesb_t = K.sb([128, 512], BF16, name="onesb")
    K.memset("dve", onesb_t[:, :], 1.0)
    onesb = onesb_t[:, :]
    epsc_t = K.sb([128, 1], F32, name="epsc")
    K.memset("dve", epsc_t[:, :], EPS)
    K.epsc = epsc_t[:, :]
    onec_t = K.sb([128, 1], F32, name="onec")
    K.memset("dve", onec_t[:, :], 1.0)
    K.onec = onec_t[:, :]
    csil = K.sb([128, 8, 2], F32, name="csil")
    K.dma("sp", csil[:, :, :], cvec.b(0).m(lambda a: a.rearrange("(k p) r -> p k r", p=128)))
    K.act(csil[:, :, :], csil[:, :, :], AF.Silu)
    crep = K.sb([128, 2, 8, 128], BF16, name="crep")
    for r in range(2):
        K.copy("dve", crep[:, r, :, :], csil[:, :, r:r + 1].m(lambda a: a.to_broadcast([128, 8, 128])))
    MODS = K.dr("MODS", [DEPTH, 2, 6 * D], F32, dk("MODS"), nbuf=DEPTH)
    PROBS = K.sb([128, NT, NE], F32, name="PROBS")
    PROBSp = T(PROBS.ap, nbuf=NT)

    def col(dst, src_ap):
        K.dma("sp", dst, V(src_ap.rearrange("(p o) -> p o", o=1), Buf()))

    def bc(dst, src_ap, n=128):
        K.dma("sp", dst, V(src_ap.partition_broadcast(n), Buf()))

    def stage_done(name):
        return stop_after == name

    for L in range(DEPTH):
        need_ctx = L < DEPTH - 1
        Xsrc = xin if L == 0 else S1
        lam_init = 0.8 - 0.6 * math.exp(-0.3 * L)

        def xrows(t0, n):
            if need_ctx:
                return S1[t0:t0 + n, :]
            return yout[t0 - TC:t0 - TC + n, :]

        K.push()
        winb = K.sb([128, 8, 3072], BF16, name="winb", nbuf=1)
        for cgi in range(3):
            K.dma("pool", winb[:, :, cgi * 1024:(cgi + 1) * 1024],
                  W["w_in"].b(0).m(lambda a: a[L, :, cgi * 1024:(cgi + 1) * 1024].rearrange("(k p) c -> p k c", p=128)), max_dma_last_dim=4096)
        K.push()
        modb = K.sb([128, 6 * D], F32, name="modb")
        bc(modb[:, :], W["mod_b"].ap[L])
        n1g = K.sb([128, D], F32, name="n1g")
        bc(n1g[:, :], W["norm1_g"].ap[L])
        n2g = K.sb([128, D], F32, name="n2g")
        bc(n2g[:, :], W["norm2_g"].ap[L])
        MOD = K.sb([128, 2, 6 * D], F32, name="MOD", nbuf=2)
        mw = K.sb([128, 2, 8, 512], BF16, name="mw", nbuf=2)
        pm = K.ps([128, 2, 512], F32, name="pm", nbuf=2)
        for cg in range(12):
            s = cg % 2
            K.dma("pool", mw.b(s)[:, s, :, :],
                  W["mod_w"].b(0).m(lambda a: a[L, :, cg * 512:(cg + 1) * 512].rearrange("(k p) c -> p k c", p=128)), max_dma_last_dim=2048)
            for r in range(2):
                for k in range(8):
                    K.mm(pm.b(r)[:, r, :], crep[:, r, k, :], mw.b(s)[:, s, k, :], start=(k == 0), stop=(k == 7))
                K.tt("dve", MOD.b(r)[:, r, cg * 512:(cg + 1) * 512], pm.b(r)[:, r, :], modb[:, cg * 512:(cg + 1) * 512], ALU.add)
        for r in range(2):
            K.stt(MOD.b(r)[:, r, 1024:2048], MOD.b(r)[:, r, 1024:2048], 1.0, n1g[:, :], ALU.add, ALU.mult)
            K.stt(MOD.b(r)[:, r, 4096:5120], MOD.b(r)[:, r, 4096:5120], 1.0, n2g[:, :], ALU.add, ALU.mult)
        for r in range(2):
            K.dma("sp", MODS.b(L).m(lambda a: a[L, r:r + 1, :]), MOD.b(r)[0:1, r, :])
        K.pop()
        if stage_done(f"mod{L}"):
            break

        def MODv(r, j):
            for cch in reversed(K.caches):
                if ("mod", r, j) in cch:
                    return cch[("mod", r, j)]
            t_ = K.sb([128, D], F32, name="modv")
            K.dma("sp", t_[:, :], MODS.b(L).m(lambda a: a[L, r, j * 1024:(j + 1) * 1024].partition_broadcast(128)))
            K.caches[-1][("mod", r, j)] = t_[:, :]
            return t_[:, :]

        cosS = K.sb([128, 512], F32, name="cosS")
        sinS = K.sb([128, 512], F32, name="sinS")
        gq = K.sb([128, 2], F32, name="gq")
        for j, nm in enumerate(("att_q_norm_g", "att_k_norm_g")):
            for hh in range(2):
                col(gq[hh * 64:(hh + 1) * 64, j:j + 1], W[nm].ap[L])
        permg = K.sb([128, 2, 128], F32, name="permg")
        for j in range(2):
            K.ts("dve", permg[:, j, :], permc, gq[:, j:j + 1], None, ALU.mult)
        xt = K.sb([128, 2, D], F32, name="xt", nbuf=2)
        st = K.sb([128, 8], F32, name="st", nbuf=4)
        tmpn = K.sb([128, D], F32, name="tmpn", nbuf=1)
        hb2 = K.sb([128, 2, 4, D], BF16, name="hb", nbuf=8)
        hT2 = K.sb([128, 2, 8, 512], BF16, name="hT", nbuf=2)
        pT = K.ps([128, 2, 512], F32, name="pT", nbuf=2)
        pF = K.ps([128, 2, 512], F32, name="pF", nbuf=2)
        pF2 = K.ps([128, 2, 512], F32, name="pF2", nbuf=2)
        pS = K.ps([128, 512], F32, name="pS")
        pR = K.ps([128, 512], F32, name="pR")
        raw = K.sb([128, 512], F32, name="raw")
        sq = K.sb([128, 512], F32, name="sq")
        t1 = K.sb([128, 512], F32, name="t1")
        t2 = K.sb([128, 512], F32, name="t2")
        rs = K.sb([128, 512], F32, name="rs")
        ob = K.sb([128, 2, 512], BF16, name="ob", nbuf=2)
        of = K.sb([128, 2, 512], F32, name="of", nbuf=2)
        obt = K.sb([128, 2, 1024], BF16, name="obt", nbuf=2)
        sgt = K.sb([128, 2, 256], F32, name="sgt", nbuf=2)
        tmB = K.sb([128, 256], F32, name="tmB")
        blocks = [(0, TC, 1)] + [(TC + 512 * i, 512, 0) for i in range(8)]
        for r_ in (0, 1):
            MODv(r_, 0)
            MODv(r_, 1)
        cnts = {"fm": 0, "tm": 0}

        def norm_part(bi):
            (t0, Wd, r) = blocks[bi]
            par = bi % 2
            nti = Wd // 128
            for i in range(nti):
                xv = xt.b(i % 2)[:, i % 2, :]
                K.dma("sp", xv, Xsrc.b(0)[t0 + i * 128:t0 + (i + 1) * 128, :])
                sv = st.b(i)
                K.act(tmpn[:, :], xv, AF.Square, accum=sv[:, 0:1])
                K.rstd(sv[:, 2:3], sv[:, 0:1], D, sv[:, 1:2])
                K.stt(tmpn[:, :], xv, sv[:, 2:3], MODv(r, 1), ALU.mult, ALU.mult)
                K.tt("pool", hb2.b(par * 4 + i)[:, par, i, :], tmpn[:, :], MODv(r, 0), ALU.add)
            for k in range(8):
                for i in range(nti):
                    K.mm(pT.b(k)[:, k % 2, i * 128:(i + 1) * 128], hb2.b(par * 4 + i)[:, par, i, k * 128:(k + 1) * 128], identb)
                K.copy("act", hT2.b(par)[:, par, k, 0:Wd], pT.b(k)[:, k % 2, 0:Wd])

        def fm_part(bi):
            (t0, Wd, r) = blocks[bi]
            par = bi % 2
            K.dma("sp", cosS[:, 0:Wd], cosD[:, t0:t0 + Wd])
            K.dma("sp", sinS[:, 0:Wd], sinD[:, t0:t0 + Wd])
            for rc, c0 in enumerate(FM_ROWS):
                s = cnts["fm"] % 2
                cnts["fm"] += 1
                cf = cnts["fm"]
                pf = pF.b(s)[:, s, 0:Wd]
                for k in range(8):
                    K.mm(pf, winb[:, k, c0:c0 + 128], hT2.b(par)[:, par, k, 0:Wd], start=(k == 0), stop=(k == 7))
                if rc < 8:
                    j = 0 if rc < 4 else 1
                    K.copy("act", raw[:, 0:Wd], pf)
                    K.tt("pool", sq[:, 0:Wd], raw[:, 0:Wd], raw[:, 0:Wd], ALU.mult)
                    K.mm(pS[:, 0:Wd], blk1, sq[:, 0:Wd])
                    K.mm(pR[:, 0:Wd], permg[:, j, :], raw[:, 0:Wd])
                    K.rstd(rs[:, 0:Wd], pS[:, 0:Wd], 64, sq[:, 0:Wd])
                    K.stt(t1[:, 0:Wd], raw[:, 0:Wd], gq[:, j:j + 1], cosS[:, 0:Wd], ALU.mult, ALU.mult)
                    K.tt("dve", t2[:, 0:Wd], pR[:, 0:Wd], sinS[:, 0:Wd], ALU.mult)
                    K.tt("pool", t1[:, 0:Wd], t1[:, 0:Wd], t2[:, 0:Wd], ALU.add)
                    K.tt("dve", ob.b(s)[:, s, 0:Wd], t1[:, 0:Wd], rs[:, 0:Wd], ALU.mult)
                    dst = (QT if j == 0 else KT)
                    rr = (rc % 4) * 128
                    K.dma("sp", dst.b(cf)[rr:rr + 128, t0:t0 + Wd], ob.b(s)[:, s, 0:Wd])
                elif rc < 12:
                    j = (rc - 8) // 2
                    K.act(ob.b(s)[:, s, 0:Wd], pf, AF.Copy, scale=(1.0 if j == 0 else 0.125))
                    dst = RQT if j == 0 else RKT
                    rr = ((rc - 8) % 2) * 128
                    K.dma("sp", dst.b(cf)[rr:rr + 128, t0:t0 + Wd], ob.b(s)[:, s, 0:Wd])
                else:
                    j = (rc - 12) // 2
                    K.copy("act", of.b(s)[:, s, 0:Wd], pf)
                    dst = XUd if j == 0 else GUd
                    rr = ((rc - 12) % 2) * 128
                    K.dma("sp", dst.b(cf)[rr:rr + 128, t0:t0 + Wd], of.b(s)[:, s, 0:Wd])

        def tm_part(bi):
            (t0, Wd, r) = blocks[bi]
            par = bi % 2
            for i in range(Wd // 128):
                tok = t0 + i * 128
                ti = tok // 128
                s = cnts["tm"] % 2
                cnts["tm"] += 1
                for g, (c0, cw) in enumerate(((1024, 512), (1792, 512), (2304, 256))):
                    pf = pF2.b(g)[:, g % 2, 0:cw]
                    for k in range(8):
                        K.mm(pf, hT2.b(par)[:, par, k, i * 128:(i + 1) * 128], winb[:, k, c0:c0 + cw], start=(k == 0), stop=(k == 7))
                    if g == 0:
                        K.copy("act", obt.b(s)[:, s, 0:512], pf)
                        K.dma("sp", Vd.b(ti)[tok:tok + 128, :], obt.b(s)[:, s, 0:512])
                    elif g == 1:
                        K.act(obt.b(s)[:, s, 512:768], pF2.b(g)[:, g % 2, 0:256], AF.Copy, scale=0.125)
                        K.copy("dve", obt.b(s)[:, s, 768:1024], pF2.b(g)[:, g % 2, 256:512])
                        K.dma("sp", RKd.b(ti)[tok:tok + 128, :], obt.b(s)[:, s, 512:768])
                        K.dma("sp", RVd.b(ti)[tok:tok + 128, :], obt.b(s)[:, s, 768:1024])
                    else:
                        K.sigmoid(sgt.b(s)[:, s, :], pf, tmB[:, 0:256])
                        K.tt("dve", sgt.b(s)[:, s, :], sgt.b(s)[:, s, :], pf, ALU.mult)
                        K.dma("sp", SGd.b(ti)[tok:tok + 128, :], sgt.b(s)[:, s, :])

        def tm_and_next(bi):
            tm_part(bi)
            if bi + 1 < len(blocks):
                norm_part(bi + 1)

        norm_part(0)
        for bi in range(len(blocks)):
            K.corun(lambda: fm_part(bi), lambda: tm_and_next(bi))
        K.pop()
        if stage_done(f"proj{L}"):
            break

        def side_fn():
            K.push()
            lgr = K.sb([128, 8], F32, name="lgr")
            bc(lgr[:, :], W["ret_log_decay"].ap[L])
            lgc = K.sb([128, 4], F32, name="lgc")
            for dr_ in range(2):
                for hp in range(2):
                    for e in range(2):
                        hd = dr_ * 4 + hp * 2 + e
                        bc(lgc[e * 64:(e + 1) * 64, dr_ * 2 + hp:dr_ * 2 + hp + 1], W["ret_log_decay"].ap[L, hd:hd + 1], n=64)
            rng = K.sb([128, 64], F32, name="rng")
            bc(rng[:, :], W["ret_norm_g"].ap[L])
            pcol = K.sb([128, 4], F32, name="pcol")
            K.copy("dve", pcol[:, 0:1], pidx)
            K.ts("dve", pcol[:, 1:2], pidx, -1.0, 127.0, ALU.mult, ALU.add)
            inner = K.sb([128, 2, 4], F32, name="inner")
            K.act(inner[:, 0, :], lgr[:, 0:4], AF.Exp, scale=pcol[:, 1:2])
            K.act(inner[:, 1, :], lgr[:, 4:8], AF.Exp, scale=pcol[:, 0:1])
            cdt = K.sb([128, 2, 4], F32, name="cdt")
            K.act(cdt[:, :, :].m(lambda a: a.rearrange("p a b -> p (a b)")), lgr[:, :], AF.Exp, scale=128.0)
            crossT = K.sb([128, 2, 2, 128], F32, name="crossT")
            jrow = K.sb([128, 2, 128], F32, name="jrow")
            K.ts("dve", jrow[:, 0, :], iota[:, 0:128], 1.0, None, ALU.add)
            K.ts("dve", jrow[:, 1, :], iota[:, 0:128], -1.0, 128.0, ALU.mult, ALU.add)
            for dr_ in range(2):
                for hp in range(2):
                    K.act(crossT[:, dr_, hp, :], jrow[:, dr_, :], AF.Exp, scale=lgc[:, dr_ * 2 + hp:dr_ * 2 + hp + 1])
            dif = K.sb([128, 128], F32, name="dif")
            K.ts("dve", dif[:, :], iota[:, 0:128], pidx, None, ALU.subtract)
            dpos = K.sb([128, 128], F32, name="dpos")
            dneg = K.sb([128, 128], F32, name="dneg")
            K.ts("dve", dpos[:, :], dif[:, :], 0.0, None, ALU.max)
            K.ts("dve", dneg[:, :], dif[:, :], -1.0, 0.0, ALU.mult, ALU.max)
            mge = K.sb([128, 128], F32, name="mge")
            mlt = K.sb([128, 128], F32, name="mlt")
            K.ts("dve", mge[:, :], dif[:, :], 0.0, None, ALU.is_ge)
            K.ts("dve", mlt[:, :], dif[:, :], 0.0, None, ALU.is_lt)
            Dfb = K.sb([128, 4, 128], F32, name="Dfb")
            dtmp = K.sb([128, 128], F32, name="dtmp")
            for h in range(4):
                K.act(dtmp[:, :], dpos[:, :], AF.Exp, scale=lgr[:, h:h + 1])
                K.tt("dve", Dfb[:, h, :], dtmp[:, :], mge[:, :], ALU.mult)
                K.act(dtmp[:, :], dneg[:, :], AF.Exp, scale=lgr[:, 4 + h:5 + h])
                K.tt("dve", dtmp[:, :], dtmp[:, :], mlt[:, :], ALU.mult)
                K.tt("dve", Dfb[:, h, :], Dfb[:, h, :], dtmp[:, :], ALU.add)
            cdtab = K.sb([128, 2, 256], F32, name="cdtab")
            for dr_ in range(2):
                K.copy("dve", cdtab[:, dr_, :].m(lambda a: a.rearrange("p (h v) -> p h v", v=64)),
                       cdt[:, dr_, :].m(lambda a: a.unsqueeze(2).to_broadcast([128, 4, 64])))
            rk = K.sb([128, 2, 256], BF16, name="rk", nbuf=2)
            rv = K.sb([128, 2, 256], BF16, name="rv", nbuf=2)
            rvi = K.sb([128, 2, 2, 256], BF16, name="rvi", nbuf=4)
            PB = K.ps([128, 2, 512], F32, name="PB", nbuf=6)
            KVb = K.sb([128, NT, 256], F32, name="KVb", nbuf=NT)
            SF = K.sb([128, 2, NT + 1, 256], BF16, name="SF", nbuf=2 * (NT + 1))
            s32 = K.sb([128, 2, 256], F32, name="s32", nbuf=2)
            stmp = K.sb([128, 256], F32, name="stmp")
            K.memset("dve", s32[:, 0, :], 0.0)
            K.memset("dve", s32.b(1)[:, 1, :], 0.0)

            def sfv(dr_, c):
                return SF.b(dr_ * (NT + 1) + c)[:, dr_, c, :]
            K.memset("pool", sfv(0, 0), 0.0)
            for c in range(NT):
                s = c % 2
                K.dma("sp", rk.b(s)[:, s, :], RKd.b(c)[c * 128:(c + 1) * 128, :])
                K.dma("sp", rv.b(s)[:, s, :], RVd.b(c)[c * 128:(c + 1) * 128, :])
                for dr_ in range(2):
                    K.tt("pool" if dr_ == 0 else "dve",
                         rvi.b(s * 2 + dr_)[:, s, dr_, :].m(lambda a: a.rearrange("p (h v) -> p h v", v=64)),
                         rv.b(s)[:, s, :].m(lambda a: a.rearrange("p (h v) -> p h v", v=64)),
                         inner[:, dr_, :].m(lambda a: a.unsqueeze(2).to_broadcast([128, 4, 64])), ALU.mult)
                    for hp in range(2):
                        K.mm(PB.b(dr_)[:, dr_, hp * 128:(hp + 1) * 128], rk.b(s)[:, s, hp * 128:(hp + 1) * 128],
                             rvi.b(s * 2 + dr_)[:, s, dr_, hp * 128:(hp + 1) * 128])
                K.tt("pool", stmp[:, :], s32[:, 0, :], cdtab[:, 0, :], ALU.mult)
                K.tt("dve", s32[:, 0, :], stmp[:, :], PB.b(0)[:, 0, 0:256], ALU.add)
                K.copy("act", sfv(0, c + 1), s32[:, 0, :])
                K.copy("act", KVb.b(c)[:, c, :], PB.b(1)[:, 1, 0:256])
            border = [1, 0] + list(range(NT - 1, 1, -1))
            K.memset("pool", sfv(1, border[0]), 0.0)
            for i in range(len(border) - 1):
                c, cn = border[i], border[i + 1]
                K.tt("pool", stmp[:, :], s32.b(1)[:, 1, :], cdtab[:, 1, :], ALU.mult)
                K.tt("dve", s32.b(1)[:, 1, :], stmp[:, :], KVb.b(c)[:, c, :], ALU.add)
                K.copy("act", sfv(1, cn), s32.b(1)[:, 1, :])
            rq = K.sb([128, 2, 2, 128], BF16, name="rq", nbuf=2)
            rkt = K.sb([128, 2, 2, 128], BF16, name="rkt", nbuf=2)
            sg = K.sb([128, 2, 256], F32, name="sg", nbuf=2)
            qc = K.sb([128, 2, 2, 2, 128], BF16, name="qc", nbuf=4)
            AT = K.sb([128, 2, 4, 128], BF16, name="AT", nbuf=2)
            osb = K.sb([128, 256], F32, name="osb")
            rsq2 = K.sb([128, 2, 256], F32, name="rsq", nbuf=2)
            rss2 = K.sb([128, 2, 12], F32, name="rss", nbuf=2)
            gs2 = K.sb([128, 2, 256], F32, name="gs", nbuf=2)
            ro12 = K.sb([128, 2, 256], F32, name="ro1", nbuf=2)
            osb2 = K.sb([128, 2, 256], F32, name="osb2", nbuf=2)
            rob = K.sb([128, 2, 256], BF16, name="rob", nbuf=2)
            rT = K.sb([128, 2, 256], BF16, name="rT", nbuf=2)
            chunks = list(range(NT)) if need_ctx else list(range(2, NT))
            def ret_chunk(c):
                s = c % 2
                rsq, rss, gs, ro1, osb = rsq2.b(s)[:, s], rss2.b(s)[:, s], gs2.b(s)[:, s], ro12.b(s)[:, s], osb2.b(s)[:, s]
                K.dma("sp", rq.b(s)[:, s, :, :], RQT.b(0).m(lambda a: a[:, c * 128:(c + 1) * 128].rearrange("(k p) t -> p k t", p=128)))
                K.dma("sp", rkt.b(s)[:, s, :, :], RKT.b(0).m(lambda a: a[:, c * 128:(c + 1) * 128].rearrange("(k p) t -> p k t", p=128)))
                K.dma("sp", rv.b(s)[:, s, :], RVd.b(c)[c * 128:(c + 1) * 128, :])
                K.dma("sp", sg.b(s)[:, s, :], SGd.b(c)[c * 128:(c + 1) * 128, :])
                for dr_ in range(2):
                    K.tt("dve" if dr_ == 0 else "pool", qc.b(s * 2 + dr_)[:, s, dr_, :, :], rq.b(s)[:, s, :, :], crossT[:, dr_, :, :], ALU.mult)
                for h in range(4):
                    e, hp = h % 2, h // 2
                    K.mm(PB.b(e)[:, e, hp * 128:(hp + 1) * 128], rkt.b(s)[e * 64:(e + 1) * 64, s, hp, :], rq.b(s)[e * 64:(e + 1) * 64, s, hp, :])
                for e in range(2):
                    K.tt("dve", AT.b(s)[:, s, :, :].m(lambda a: a.rearrange("p (hp e) n -> p hp e n", e=2)[:, :, e, :]),
                         PB.b(e)[:, e, 0:256].m(lambda a: a.rearrange("p (hp n) -> p hp n", n=128)),
                         Dfb[:, :, :].m(lambda a: a.rearrange("p (hp e) n -> p hp e n", e=2)[:, :, e, :]), ALU.mult)
                for h in range(4):
                    e, hp = h % 2, h // 2
                    po = PB.b(2 + e)[:, e, 256 + hp * 64:256 + (hp + 1) * 64]
                    K.mm(po, AT.b(s)[:, s, h, :], rv.b(s)[:, s, h * 64:(h + 1) * 64], start=True, stop=False)
                    for dr_ in range(2):
                        K.mm(po, qc.b(s * 2 + dr_)[e * 64:(e + 1) * 64, s, dr_, hp, :],
                             sfv(dr_, c)[e * 64:(e + 1) * 64, hp * 128 + e * 64:hp * 128 + (e + 1) * 64], start=False, stop=(dr_ == 1))
                for e in range(2):
                    K.copy("act", osb[:, :].m(lambda a: a.rearrange("p (hp e v) -> p hp e v", e=2, v=64)[:, :, e, :]),
                           PB.b(2 + e)[:, e, 256:384].m(lambda a: a.rearrange("p (hp v) -> p hp v", v=64)))
                K.act(rsq[:, :], osb[:, :], AF.Square)
                K.reduce(rss[:, 0:4], rsq[:, :].m(lambda a: a.rearrange("p (h v) -> p h v", v=64)), ALU.add)
                K.rstd(rss[:, 8:12], rss[:, 0:4], 64, rss[:, 4:8])
                K.tt("pool", gs[:, :].m(lambda a: a.rearrange("p (h v) -> p h v", v=64)),
                     sg.b(s)[:, s, :].m(lambda a: a.rearrange("p (h v) -> p h v", v=64)),
                     rng[:, :].m(lambda a: a.unsqueeze(1).to_broadcast([128, 4, 64])), ALU.mult)
                K.tt("dve", ro1[:, :].m(lambda a: a.rearrange("p (h v) -> p h v", v=64)),
                     osb[:, :].m(lambda a: a.rearrange("p (h v) -> p h v", v=64)),
                     rss[:, 8:12].m(lambda a: a.unsqueeze(2).to_broadcast([128, 4, 64])), ALU.mult)
                K.tt("pool", rob.b(s)[:, s, :], ro1[:, :], gs[:, :], ALU.mult)
                for j in range(2):
                    K.mm(PB.b(4 + j)[:, j, 384:512], rob.b(s)[:, s, j * 128:(j + 1) * 128], identb)
                for j in range(2):
                    K.copy("act", rT.b(s)[:, s, j * 128:(j + 1) * 128], PB.b(4 + j)[:, j, 384:512])
                for j in range(2):
                    K.dma("sp", MIXT.b(c * 2 + j)[512 + j * 128:512 + (j + 1) * 128, c * 128:(c + 1) * 128], rT.b(s)[:, s, j * 128:(j + 1) * 128])

            def run_par(par):
                for c in chunks:
                    if c % 2 == par:
                        ret_chunk(c)
            for c in chunks:
                ret_chunk(c)
            K.pop()

            K.push()
            cw = K.sb([128, 2, 4], F32, name="cw")
            cb = K.sb([128, 2], F32, name="cb")
            gb = K.sb([128, 4, 2], F32, name="gb")
            lam = K.sb([128, 4], F32, name="lam")
            lng = K.sb([128, 2], F32, name="lng")
            for c in range(2):
                for j in range(4):
                    col(cw[:, c, j:j + 1], W["lru_conv_w"].ap[L, j, c * 128:(c + 1) * 128])
                col(cb[:, c:c + 1], W["lru_conv_b"].ap[L, c * 128:(c + 1) * 128])
                col(lng[:, c:c + 1], W["lru_norm_g"].ap[L, c * 128:(c + 1) * 128])
                for dg in range(4):
                    col(gb[:, dg, c:c + 1], W["lru_gate_b"].ap[L, dg, c * 128:(c + 1) * 128])
                for dr_ in range(2):
                    col(lam[:, dr_ * 2 + c:dr_ * 2 + c + 1], W["lru_lambda"].ap[L, dr_, c * 128:(c + 1) * 128])
            wbd = K.sb([128, 8, 128], F32, name="wbd")
            K.memset("dve", wbd[:, :, :], 0.0)
            for dr_ in range(2):
                for g in range(2):
                    for c in range(2):
                        for e in range(2):
                            K.dma("sp", wbd[e * 64:(e + 1) * 64, (dr_ * 2 + g) * 2 + c, e * 64:(e + 1) * 64],
                                  W["lru_gate_w"].b(0).m(lambda a: a[L, dr_, g, 2 * c + e, :, :]))
            sp = K.sb([128, 8, 4], F32, name="sp")
            K.ts("dve", sp[:, 5, :], lam[:, :], -1.0, None, ALU.mult)
            K.tt("dve", sp[:, 0, :], lam[:, :], sp[:, 5, :], ALU.max)
            K.act(sp[:, 1, :], sp[:, 0, :], AF.Exp, scale=-1.0)
            K.ts("dve", sp[:, 2, :], sp[:, 1, :], 2.0, None, ALU.add)
            K.recip(sp[:, 2, :], sp[:, 2, :])
            K.tt("dve", sp[:, 2, :], sp[:, 2, :], sp[:, 1, :], ALU.mult)
            K.tt("dve", sp[:, 3, :], sp[:, 2, :], sp[:, 2, :], ALU.mult)
            K.memset("dve", sp[:, 4, :], 1.0 / 15.0)
            for n_ in (13, 11, 9, 7, 5, 3, 1):
                K.tt("dve", sp[:, 4, :], sp[:, 4, :], sp[:, 3, :], ALU.mult)
                K.ts("dve", sp[:, 4, :], sp[:, 4, :], 1.0 / n_, None, ALU.add)
            K.tt("dve", sp[:, 4, :], sp[:, 4, :], sp[:, 2, :], ALU.mult)
            K.ts("dve", sp[:, 5, :], lam[:, :], -1.0, 0.0, ALU.mult, ALU.max)
            K.stt(sp[:, 6, :], sp[:, 4, :], 2.0, sp[:, 5, :], ALU.mult, ALU.add)
            K.ts("dve", sp[:, 7, :], sp[:, 6, :], -8.0, None, ALU.mult)
            ccoef = sp[:, 7, :]
            PADL = TC + 3
            xu = K.sb([128, 2, TT + 6], F32, name="xu")
            K.memset("pool", xu[:, :, :], 0.0)
            for c in range(2):
                K.dma("sp", xu[:, c, 2:2 + TC], XUd.b(0)[c * 128:(c + 1) * 128, 0:TC])
                K.dma("sp", xu[:, c, PADL + 2:PADL + 2 + TL], XUd.b(0)[c * 128:(c + 1) * 128, TC:TT])
            u = K.sb([128, 2, TT], F32, name="u")
            for c in range(2):
                for (pb, t0, Ln) in ((2, 0, TC), (PADL + 2, TC, TL)):
                    e = "dve"
                    K.ts(e, u[:, c, t0:t0 + Ln], xu[:, c, pb - 2:pb - 2 + Ln], cw[:, c, 0:1], cb[:, c:c + 1], ALU.mult, ALU.add)
                    for j in range(1, 4):
                        K.stt(u[:, c, t0:t0 + Ln], xu[:, c, pb - 2 + j:pb - 2 + j + Ln], cw[:, c, j:j + 1], u[:, c, t0:t0 + Ln], ALU.mult, ALU.add)
            hf = xu
            pG = K.ps([128, 2, 512], F32, name="pG", nbuf=2)
            gr_2 = K.sb([128, 2, 512], F32, name="gr", nbuf=2)
            gtmp_2 = K.sb([128, 2, 512], F32, name="gtmp", nbuf=2)
            ngb = K.sb([128, 4, 2], F32, name="ngb")
            K.ts("dve", ngb[:, :, :], gb[:, :, :], -1.0, None, ALU.mult)
            gi_2 = K.sb([128, 2, 512], F32, name="gi", nbuf=2)
            ga_2 = K.sb([128, 2, 512], F32, name="ga", nbuf=2)
            gw_2 = K.sb([128, 2, 512], F32, name="gw", nbuf=2)
            gbv_2 = K.sb([128, 2, 512], F32, name="gbv", nbuf=2)
            ar_2 = K.sb([128, 2, 512], F32, name="ar", nbuf=2)
            br_2 = K.sb([128, 2, 512], F32, name="br", nbuf=2)
            hbr = K.sb([128, 2, 512], F32, name="hbr", nbuf=2)
            hst = K.sb([128, 2], F32, name="hst", nbuf=2)
            yc = K.sb([128, 2, 512], F32, name="yc", nbuf=2)
            gu = K.sb([128, 2, 512], F32, name="gu", nbuf=2)
            g2_2 = K.sb([128, 2, 512], F32, name="g2", nbuf=2)
            ysq = K.sb([128, 2, 512], F32, name="ysq", nbuf=2)
            yrs = K.sb([128, 512], F32, name="yrs")
            yob = K.sb([128, 2, 512], BF16, name="yob", nbuf=2)

            def tmps(c):
                return [t_.b(c)[:, c] for t_ in (gr_2, gtmp_2, gi_2, ga_2, gw_2, gbv_2, ar_2, br_2, g2_2)]

            def gates(dr_, c, t0, Wd):
                gr, gtmp, gi, ga, gw, gbv, ar, br, g2 = tmps(c)
                for g, dst in ((0, gr), (1, gi)):
                    K.mm(pG.b(c)[:, c, 0:Wd], wbd[:, (dr_ * 2 + g) * 2 + c, :], u[:, c, t0:t0 + Wd])
                    K.sigmoid(dst[:, 0:Wd], pG.b(c)[:, c, 0:Wd], gtmp[:, 0:Wd], nbias=ngb[:, dr_ * 2 + g, c:c + 1])
                K.act(ga[:, 0:Wd], gr[:, 0:Wd], AF.Exp, scale=ccoef[:, dr_ * 2 + c:dr_ * 2 + c + 1])
                K.tt("pool", gw[:, 0:Wd], ga[:, 0:Wd], ga[:, 0:Wd], ALU.mult)
                K.ts("dve", gw[:, 0:Wd], gw[:, 0:Wd], -1.0, 1.0, ALU.mult, ALU.add)
                K.act(gw[:, 0:Wd], gw[:, 0:Wd], AF.Ln)
                K.act(gw[:, 0:Wd], gw[:, 0:Wd], AF.Exp, scale=0.5)
                K.tt("pool", gbv[:, 0:Wd], gi[:, 0:Wd], u[:, c, t0:t0 + Wd], ALU.mult)
                K.tt("dve", gbv[:, 0:Wd], gbv[:, 0:Wd], gw[:, 0:Wd], ALU.mult)

            lblocks = [(0, TC)] + [(TC + 512 * i, 512) for i in range(8)]

            def fwd_chain(c):
                gr, gtmp, gi, ga, gw, gbv, ar, br, g2 = tmps(c)
                for bi, (t0, Wd) in enumerate(lblocks):
                    gates(0, c, t0, Wd)
                    init = 0.0 if bi == 0 else hf.b(0)[:, c, t0 - 1:t0]
                    K.scan(hf.b(0)[:, c, t0:t0 + Wd], ga[:, 0:Wd], gbv[:, 0:Wd], init)
            K.corun(lambda: fwd_chain(0), lambda: fwd_chain(1))
            bblocks = [lblocks[0]] + lblocks[:0:-1]
            nyo = 0

            def bwd_blk(c, bi, t0, Wd, emit):
                gr, gtmp, gi, ga, gw, gbv, ar, br, g2 = tmps(c)
                gates(1, c, t0, Wd)
                K.copy("dve", ar[:, 0:Wd], ga[:, 0:Wd].m(lambda a: a[:, ::-1]))
                K.copy("dve", br[:, 0:Wd], gbv[:, 0:Wd].m(lambda a: a[:, ::-1]))
                init = 0.0 if bi == 0 else hst.b(c)[:, c:c + 1]
                K.scan(hbr.b(c)[:, c, 0:Wd], ar[:, 0:Wd], br[:, 0:Wd], init)
                K.copy("pool", hst.b(c)[:, c:c + 1], hbr.b(c)[:, c, Wd - 1:Wd])
                if not emit:
                    return
                K.dma("sp", gu.b(c)[:, c, 0:Wd], GUd.b(0)[c * 128:(c + 1) * 128, t0:t0 + Wd])
                K.tt("dve", yc.b(c)[:, c, 0:Wd], hf.b(0)[:, c, t0:t0 + Wd], hbr.b(c)[:, c, 0:Wd].m(lambda a: a[:, ::-1]), ALU.add)
                guv = gu.b(c)[:, c, 0:Wd]
                K.tt("pool", g2[:, 0:Wd], guv, guv, ALU.mult)
                K.ts("dve", g2[:, 0:Wd], g2[:, 0:Wd], 0.044715, 1.0, ALU.mult, ALU.add)
                K.tt("pool", g2[:, 0:Wd], g2[:, 0:Wd], guv, ALU.mult)
                K.sigmoid(g2[:, 0:Wd], g2[:, 0:Wd], gtmp[:, 0:Wd], scale=2.0 * math.sqrt(2.0 / math.pi))
                K.tt("pool", g2[:, 0:Wd], g2[:, 0:Wd], guv, ALU.mult)
                K.tt("dve", yc.b(c)[:, c, 0:Wd], yc.b(c)[:, c, 0:Wd], g2[:, 0:Wd], ALU.mult)
                K.tt("pool", ysq.b(c)[:, c, 0:Wd], yc.b(c)[:, c, 0:Wd], yc.b(c)[:, c, 0:Wd], ALU.mult)

            for bi, (t0, Wd) in enumerate(bblocks):
                emit = need_ctx or t0 >= TC
                K.corun(lambda: bwd_blk(0, bi, t0, Wd, emit), lambda: bwd_blk(1, bi, t0, Wd, emit))
                if not emit:
                    continue
                for c in range(2):
                    K.mm(pG.b(0)[:, 0, 0:Wd], ones, ysq.b(c)[:, c, 0:Wd], start=(c == 0), stop=(c == 1))
                K.rstd(yrs[:, 0:Wd], pG.b(0)[:, 0, 0:Wd], 256, ysq.b(0)[:, 0, 0:Wd])
                for c in range(2):
                    K.stt(yob.b(c)[:, c, 0:Wd], yc.b(c)[:, c, 0:Wd], lng[:, c:c + 1], yrs[:, 0:Wd], ALU.mult, ALU.mult)
                    nyo += 1
                    K.dma("sp", MIXT.b(nyo)[768 + c * 128:768 + (c + 1) * 128, t0:t0 + Wd], yob.b(c)[:, c, 0:Wd])
            K.pop()


        K.push()
        lamb = K.sb([128, 256], F32, name="lamb")
        bc(lamb[:, :], W["att_lambda"].ap[L])
        lt = K.sb([128, 8], F32, name="lt")
        lj = K.sb([128, 64], F32, name="lj")
        K.tt("dve", lj[:, :], lamb[:, 0:64], lamb[:, 64:128], ALU.mult)
        K.reduce(lt[:, 0:1], lj[:, :], ALU.add)
        K.tt("dve", lj[:, :], lamb[:, 128:192], lamb[:, 192:256], ALU.mult)
        K.reduce(lt[:, 1:2], lj[:, :], ALU.add)
        K.act(lt[:, 2:4], lt[:, 0:2], AF.Exp)
        K.tt("dve", lt[:, 4:5], lt[:, 3:4], lt[:, 2:3], ALU.subtract)
        K.ts("dve", lt[:, 5:6], lt[:, 4:5], -lam_init, None, ALU.add)
        neglam = lt[:, 5:6]
        gsub = K.sb([128, 1], F32, name="gsub")
        col(gsub[:, :], W["att_subln_g"].ap[L])
        K.ts("dve", gsub[:, :], gsub[:, :], 1.0 - lam_init, None, ALU.mult)
        QTh = K.sb([128, TT], BF16, name="QTh")
        KTh = K.sb([128, TT], BF16, name="KTh")
        Vh = K.sb([128, NT, 128], BF16, name="Vh")
        pSs = K.ps([128, 4, 512], F32, name="pSs", nbuf=4)
        pO = K.ps([128, 2, 512], F32, name="pO", nbuf=2)
        PTt = K.sb([128, 4, 512], BF16, name="PTt", nbuf=4)
        rl = K.sb([128, 2, 512], F32, name="rl", nbuf=2)
        pacc = K.sb([128, 2, 512], F32, name="pacc", nbuf=2)
        o0 = K.sb([128, 512], F32, name="o0")
        o1 = K.sb([128, 512], F32, name="o1")
        osq = K.sb([128, 512], F32, name="osq")
        ors = K.sb([128, 512], F32, name="ors")
        aob = K.sb([128, 2, 512], BF16, name="aob", nbuf=2)
        qblocks = [(TC + 512 * i, 512, list(range(NT))) for i in range(8)]
        if need_ctx:
            qblocks = [(0, TC, [0, 1])] + qblocks
        nqb = 0
        K.side = K.spawn(side_fn)
        for h in range(4):
            K.dma("sp", QTh[:, :], QT.b(0)[h * 128:(h + 1) * 128, :])
            K.dma("sp", KTh[:, :], KT.b(0)[h * 128:(h + 1) * 128, :])
            K.dma("sp", Vh[:, :, :], Vd.b(0).m(lambda a: a[:, h * 128:(h + 1) * 128].rearrange("(c p) v -> p c v", p=128)))
            for (t0, Wd, kcs) in qblocks:
                def st_mm(ci):
                    kc = kcs[ci]
                    for m in range(2):
                        sl = (ci % 2) * 2 + m
                        K.mm(pSs.b(sl)[:, sl, 0:Wd], KTh[m * 64:(m + 1) * 64, kc * 128:(kc + 1) * 128],
                             QTh[m * 64:(m + 1) * 64, t0:t0 + Wd])
                st_mm(0)
                for ci, kc in enumerate(kcs):
                    if ci + 1 < len(kcs):
                        st_mm(ci + 1)
                    K.side.step(SIDE_A)
                    par = ci % 2
                    K.op("act", lambda E, par=par: E.activation(out=PTt.ap[:, par * 2:par * 2 + 2, 0:Wd], in_=pSs.ap[:, par * 2:par * 2 + 2, 0:Wd],
                                                                func=AF.Exp, scale=0.125),
                         [PTt.b(par * 2), PTt.b(par * 2 + 1)], [pSs.b(par * 2), pSs.b(par * 2 + 1)])
                    K.side.step(SIDE_B)
                    for m in range(2):
                        sl = par * 2 + m
                        K.mm(pO.b(m)[:, m, 0:Wd], Vh[:, kc, :], PTt.b(sl)[:, sl, 0:Wd], start=(ci == 0), stop=(ci == len(kcs) - 1))
                    if ci == 0:
                        K.op("dve", lambda E, par=par: E.tensor_copy(out=pacc.ap[:, :, 0:Wd], in_=PTt.ap[:, par * 2:par * 2 + 2, 0:Wd]),
                             [pacc.b(0), pacc.b(1)], [PTt.b(par * 2), PTt.b(par * 2 + 1)])
                    else:
                        K.op("dve", lambda E, par=par: E.tensor_tensor(out=pacc.ap[:, :, 0:Wd], in0=pacc.ap[:, :, 0:Wd],
                                                                      in1=PTt.ap[:, par * 2:par * 2 + 2, 0:Wd], op=ALU.add),
                             [pacc.b(0), pacc.b(1)], [pacc.b(0), pacc.b(1), PTt.b(par * 2), PTt.b(par * 2 + 1)])
                    K.side.step(SIDE_N)
                for m in range(2):
                    K.mm(pSs.b(m)[:, m, 0:Wd], ones, pacc.b(m)[:, m, 0:Wd])
                K.op("act", lambda E: E.activation(out=rl.ap[:, :, 0:Wd], in_=pSs.ap[:, 0:2, 0:Wd], func=AF.Ln),
                     [rl.b(0), rl.b(1)], [pSs.b(0), pSs.b(1)])
                K.op("act", lambda E: E.activation(out=rl.ap[:, :, 0:Wd], in_=rl.ap[:, :, 0:Wd], func=AF.Exp, scale=-1.0),
                     [rl.b(0), rl.b(1)], [rl.b(0), rl.b(1)])
                K.tt("dve", o0[:, 0:Wd], pO.b(0)[:, 0, 0:Wd], rl.b(0)[:, 0, 0:Wd], ALU.mult)
                K.tt("dve", o1[:, 0:Wd], pO.b(1)[:, 1, 0:Wd], rl.b(1)[:, 1, 0:Wd], ALU.mult)
                K.stt(o0[:, 0:Wd], o1[:, 0:Wd], neglam, o0[:, 0:Wd], ALU.mult, ALU.add)
                K.tt("pool", osq[:, 0:Wd], o0[:, 0:Wd], o0[:, 0:Wd], ALU.mult)
                K.mm(pSs.b(2)[:, 2, 0:Wd], ones, osq[:, 0:Wd])
                K.rstd(ors[:, 0:Wd], pSs.b(2)[:, 2, 0:Wd], 128, osq[:, 0:Wd])
                s = nqb % 2
                nqb += 1
                K.stt(aob.b(s)[:, s, 0:Wd], o0[:, 0:Wd], gsub[:, 0:1], ors[:, 0:Wd], ALU.mult, ALU.mult)
                K.dma("sp", MIXT.b(nqb)[h * 128:(h + 1) * 128, t0:t0 + Wd], aob.b(s)[:, s, 0:Wd])
        K.side.flush()
        del K.runners[K.side.thread]
        K.side = None
        K.pop()
        if stage_done(f"att{L}") or stage_done(f"ret{L}") or stage_done(f"lru{L}"):
            break

        K.push()
        woutb = K.sb([128, 8, D], BF16, name="woutb", nbuf=1)
        K.dma("pool", woutb[:, :, :], W["w_out"].b(0).m(lambda a: a[L].rearrange("(k p) c -> p k c", p=128)), max_dma_last_dim=4096)
        rw = K.sb([128, 8, NE], F32, name="rw")
        K.dma("sp", rw[:, :, :], W["router_w"].b(0).m(lambda a: a[L].rearrange("(k p) e -> p k e", p=128)))
        mtp = K.sb([128, 2, 8, 128], BF16, name="mtp", nbuf=2)
        x0 = K.sb([128, 2, D], F32, name="x0", nbuf=2)
        x1 = K.sb([128, 2, D], F32, name="x1", nbuf=2)
        pXp = K.ps([128, 2, 512], F32, name="pXp", nbuf=2)
        junk2 = K.sb([128, 2, D], F32, name="junk2", nbuf=2)
        st2p = K.sb([128, 2, 8], F32, name="st2", nbuf=2)
        tmp2 = K.sb([128, 2, D], F32, name="tmp2", nbuf=2)
        h2f2 = K.sb([128, 2, D], F32, name="h2f", nbuf=2)
        h2b = K.sb([128, 2, D], BF16, name="h2b", nbuf=2)
        pHp = K.ps([128, 2, 512], F32, name="pHp", nbuf=2)
        h2Tp = K.sb([128, 2, 8, 128], F32, name="h2T", nbuf=2)
        pRtp = K.ps([128, 2, 512], F32, name="pRt", nbuf=2)
        exp_ = K.sb([128, 2, NE], F32, name="ex", nbuf=2)
        oblocks = [(TC + 512 * i, 512, 0) for i in range(8)]
        if need_ctx:
            oblocks = [(0, TC, 1)] + oblocks
        for r_ in ([0, 1] if need_ctx else [0]):
            for j_ in (2, 3, 4):
                MODv(r_, j_)

        def out_tile(tok, ti, r, p):
            st2 = st2p.b(p)[:, p]
            h2f = h2f2.b(p)[:, p]
            h2T = h2Tp.b(p)[:, p]
            K.dma("sp", mtp.b(p)[:, p, :, :], MIXT.b(0).m(lambda a: a[:, tok:tok + 128].rearrange("(k p) t -> p k t", p=128)))
            K.dma("sp", x0.b(p)[:, p, :], Xsrc.b(0)[tok:tok + 128, :])
            for hf_ in range(2):
                for k in range(8):
                    K.mm(pXp.b(p)[:, p, :], mtp.b(p)[:, p, k, :], woutb[:, k, hf_ * 512:(hf_ + 1) * 512], start=(k == 0), stop=(k == 7))
                K.tt("dve", x1.b(p)[:, p, hf_ * 512:(hf_ + 1) * 512], pXp.b(p)[:, p, :], MODv(r, 2)[:, hf_ * 512:(hf_ + 1) * 512], ALU.mult)
            K.tt("pool", x1.b(p)[:, p, :], x1.b(p)[:, p, :], x0.b(p)[:, p, :], ALU.add)
            dst = (S1.b(ti)[tok:tok + 128, :] if need_ctx else yout.b(ti)[tok - TC:tok - TC + 128, :])
            K.dma("sp", dst, x1.b(p)[:, p, :])
            K.act(junk2.b(p)[:, p, :], x1.b(p)[:, p, :], AF.Square, accum=st2[:, 0:1])
            K.rstd(st2[:, 2:3], st2[:, 0:1], D, st2[:, 1:2])
            K.stt(tmp2.b(p)[:, p, :], x1.b(p)[:, p, :], st2[:, 2:3], MODv(r, 4), ALU.mult, ALU.mult)
            K.tt("pool", h2f[:, :], tmp2.b(p)[:, p, :], MODv(r, 3), ALU.add)
            K.copy("act", h2b.b(p)[:, p, :], h2f[:, :])
            K.dma("sp", H2d.b(ti)[tok:tok + 128, :], h2b.b(p)[:, p, :])
            for q in range(2):
                for k4 in range(4):
                    k = q * 4 + k4
                    K.tr(pHp.b(p)[:, p, k4 * 128:(k4 + 1) * 128], h2f[:, k * 128:(k + 1) * 128], ident)
                K.copy("act" if q == 0 else "dve", h2T[:, q * 4:(q + 1) * 4, :].m(lambda a: a.rearrange("p k t -> p (k t)")), pHp.b(p)[:, p, :])
            for k in range(8):
                K.mm(pRtp.b(p)[:, p, 0:NE], h2T[:, k, :], rw[:, k, :], start=(k == 0), stop=(k == 7))
            K.reduce(st2[:, 3:4], pRtp.b(p)[:, p, 0:NE], ALU.max)
            K.ts("dve", st2[:, 4:5], st2[:, 3:4], -1.0, None, ALU.mult)
            K.act(exp_.b(p)[:, p, :], pRtp.b(p)[:, p, 0:NE], AF.Exp, bias=st2[:, 4:5], accum=st2[:, 5:6])
            K.recip(st2[:, 6:7], st2[:, 5:6])
            K.ts("dve", PROBSp.b(ti)[:, ti, :], exp_.b(p)[:, p, :], st2[:, 6:7], None, ALU.mult)
            if PRd is not None and L == 0:
                K.dma("sp", PRd.b(ti)[tok:tok + 128, :], PROBSp.b(ti)[:, ti, :])

        def out_tiles(par):
            for (t0, Wd, r) in oblocks:
                for i in range(Wd // 128):
                    tok = t0 + i * 128
                    if (tok // 128) % 2 == par:
                        out_tile(tok, tok // 128, r, par)
        K.corun(lambda: out_tiles(0), lambda: out_tiles(1))
        K.pop()
        if stage_done(f"out{L}"):
            break

        def moe(groups, stream_rows):
            K.push()
            pTp = K.ps([NE, 512], F32, name="pTp")
            pPM = K.ps([128, 32, NE], F32, name="pPM")
            wg = K.sb([128, 2, 2, 8, D], BF16, name="wg", nbuf=4)
            wd = K.sb([128, 8, D], BF16, name="wd")
            wn = ("exp_w_gate", "exp_w_up", "exp_w_down")

            def load_w(e):
                s = e % 2
                for j in range(2):
                    K.dma("pool", wg.b(s * 2 + j)[:, s, j, :, :],
                          W[wn[j]].b(0).m(lambda a: a[L, e].rearrange("(k p) c -> p k c", p=128)), max_dma_last_dim=4096)

            def load_wd(e):
                K.dma("pool", wd[:, :, :], W[wn[2]].b(0).m(lambda a: a[L, e].rearrange("(k p) c -> p k c", p=128)),
                      max_dma_last_dim=4096)

            load_w(0)
            for g in groups:
                row0, ntk, cap = g["row0"], g["ntk"], g["cap"]
                Tn = ntk * 128
                ti0 = row0 // 128
                g["ncj"] = (cap + 127) // 128
                g["cwj"] = min(cap, 128)
                PM = K.sb([128, ntk, NE], F32, name="PM")
                g["PM"] = PM
                K.push()
                PTm = K.sb([NE, Tn], F32, name="PTm")
                for i in range(ntk):
                    K.tr(pTp[:, (i % 4) * 128:(i % 4 + 1) * 128], PROBS[:, ti0 + i, :], ident)
                    if i % 4 == 3 or i == ntk - 1:
                        n = (i % 4 + 1) * 128
                        K.copy("act", PTm[:, (i // 4) * 512:(i // 4) * 512 + n], pTp[:, 0:n])
                K.dma("sp", PTd[:, 0:Tn], PTm[:, :])
                PT8 = K.sb([128, Tn], F32, name="PT8")
                for g8 in range(8):
                    K.dma("sp", PT8[g8 * NE:(g8 + 1) * NE, :], PTd[:, 0:Tn])
                bs = K.sb([128, 8], F32, name="bs")
                K.memset("dve", bs[:, 0:1], 0.0)
                K.memset("dve", bs[:, 1:2], 1.0)
                bj = K.sb([128, Tn], BF16, name="bj")
                for it in range(10):
                    K.tt("dve", bs[:, 2:3], bs[:, 1:2], bs[:, 0:1], ALU.subtract)
                    K.stt(bs[:, 3:4], bs[:, 2:3], cfrac, bs[:, 0:1], ALU.mult, ALU.add)
                    K.ts("dve", bj[:, :], PT8[:, :], bs[:, 3:4], 0.0, ALU.is_ge, ALU.add, accum=bs[:, 4:5])
                    K.ts("dve", bs[:, 5:6], bs[:, 4:5], float(cap), None, ALU.is_ge)
                    K.mm(pPM[:, 0, 0:1], sameE, bs[:, 5:6])
                    K.ts("dve", bs[:, 6:7], pPM[:, 0, 0:1], 0.125, 0.125, ALU.mult, ALU.add)
                    K.stt(bs[:, 1:2], bs[:, 2:3], bs[:, 6:7], bs[:, 0:1], ALU.mult, ALU.add)
                    K.ts("dve", bs[:, 6:7], pPM[:, 0, 0:1], 0.125, None, ALU.mult)
                    K.stt(bs[:, 0:1], bs[:, 2:3], bs[:, 6:7], bs[:, 0:1], ALU.mult, ALU.add)
                K.ts("dve", bj[0:NE, :], PT8[0:NE, :], bs[0:NE, 0:1], None, ALU.is_ge)
                pos = K.sb([NE, Tn], F32, name="pos")
                K.scan(pos[:, :], bj[0:NE, :], bj[0:NE, :], 0.0, op0=ALU.add, op1=ALU.max)
                K.tt("dve", pos[:, :], pos[:, :], bj[0:NE, :], ALU.mult)
                K.ts("dve", pos[:, :], pos[:, :], -1.0, None, ALU.add)
                for i in range(ntk):
                    K.tr(pPM[:, i, :], pos[:, i * 128:(i + 1) * 128], ident[0:NE, 0:NE])
                K.copy("act", PM[:, :, :], pPM[:, 0:ntk, :])
                K.pop()
                vals = K.sb([128, ntk, NE, 5], BF16, name="vals")
                g["vals"] = vals
                K.copy("dve", vals[:, :, :, 0], tidx[:, 0:ntk].m(lambda a: a.unsqueeze(2).to_broadcast([128, ntk, NE])))
                K.copy("dve", vals[:, :, :, 1], pidx.m(lambda a: a.unsqueeze(2).to_broadcast([128, ntk, NE])))
                pr = PROBS[:, ti0:ti0 + ntk, :]
                vf = K.sb([128, ntk, NE], F32, name="vf")
                r1 = K.sb([128, ntk, NE], F32, name="r1")
                K.copy("dve", vals[:, :, :, 2], pr)
                K.copy("dve", vf[:, :, :], vals[:, :, :, 2])
                K.tt("dve", r1[:, :, :], pr, vf[:, :, :], ALU.subtract)
                K.copy("dve", vals[:, :, :, 3], r1[:, :, :])
                K.copy("dve", vf[:, :, :], vals[:, :, :, 3])
                K.tt("dve", r1[:, :, :], r1[:, :, :], vf[:, :, :], ALU.subtract)
                K.copy("dve", vals[:, :, :, 4], r1[:, :, :])
                ncj = g["ncj"]
                g["idxi"] = K.sb([128, 2, 4], I32, name="idxi", nbuf=2)
                g["idxs"] = K.sb([128, 2, 4], I32, name="idxs", nbuf=2)
                g["aff"] = K.sb([128, 2, 4], F32, name="aff", nbuf=2)
                g["Xg"] = K.sb([128, ncj, D], BF16, name="Xg", nbuf=ncj)
                g["XT"] = K.sb([128, 2, 8, cap], BF16, name="XT", nbuf=2)
                g["hid"] = K.sb([128, 8, cap], BF16, name="hid", nbuf=8)
            NSEL = 6
            sel = K.sb([128, NSEL, 512], BF16, name="sel", nbuf=NSEL)
            pPMf = pPM.ap.rearrange("p t e -> p (t e)")
            pIg = [T(pTp.ap[0:5, :]), T(pPMf[0:5, 64:64 + 64])]
            pITg = [T(pPMf[:, 0:32]), T(pPMf[:, 32:40])]
            ilg = [K.sb([8, 512], F32, name="il"), K.sb([8, 64], F32, name="il2")]
            ilT = K.sb([128, 2, 4, 8], F32, name="ilT", nbuf=2)
            idxf = K.sb([128, 2, 4], F32, name="idxf", nbuf=2)
            pXT = K.ps([128, 512], F32, name="pXT")
            pGU = K.ps([128, 2, 512], F32, name="pGU", nbuf=2)
            pC = K.ps([128, 512], F32, name="pC")
            sgl = K.sb([128, 512], F32, name="sgl")
            pY = K.ps([128, 2, 512], F32, name="pY", nbuf=2)
            ysb = K.sb([128, 2, D], F32, name="ysb", nbuf=2)
            cnt = {"sel": 0, "y": 0}
            scb = [[Buf() for _ in range(8)] for _ in range(2)]
            for b_ in scb[0] + scb[1]:
                b_.w = stream_rows.buf.w

            def sel_op(e, gi, i, slot):
                g = groups[gi]
                cap = g["cap"]
                K.ts("dve", sel.b(slot)[:, slot, 0:cap], iota[:, 0:cap], g["PM"][:, i, e:e + 1], None, ALU.is_equal)

            def idx_mm(e, gi, i, slot):
                g = groups[gi]
                cap, ntk = g["cap"], g["ntk"]
                K.mm(pIg[gi][:, 0:cap], g["vals"][:, i, e, :], sel.b(slot)[:, slot, 0:cap], start=(i == 0), stop=(i == ntk - 1))

            def prep_fin(e):
                s = e % 2
                for gi, g in enumerate(groups):
                    cap, ncj, cwj = g["cap"], g["ncj"], g["cwj"]
                    il = ilg[gi]
                    K.copy("act", il[0:5, 0:cap], pIg[gi][:, 0:cap])
                    for jc in range(ncj):
                        K.tr(pITg[gi][0:cwj, jc * 8:jc * 8 + 5], il[0:5, jc * 128:jc * 128 + cwj], ident[0:5, 0:5])
                    iT = ilT.b(gi)[0:cwj, gi, 0:ncj, 0:5]
                    K.copy("act", iT, pITg[gi][0:cwj, 0:ncj * 8].m(lambda a: a.rearrange("p (j f) -> p j f", f=8)[:, :, 0:5]))
                    iF = idxf.b(gi)[0:cwj, gi, 0:ncj]
                    K.stt(iF, ilT.b(gi)[0:cwj, gi, 0:ncj, 0], 128.0, ilT.b(gi)[0:cwj, gi, 0:ncj, 1], ALU.mult, ALU.add)
                    K.ts("dve", iF, iF, float(g["srow0"]), None, ALU.add)
                    K.copy("dve", g["idxs"].b(s)[0:cwj, s, 0:ncj], iF)
                    K.ts("dve", iF, iF, float(g["row0"] - g["srow0"]), None, ALU.add)
                    K.copy("dve", g["idxi"].b(s)[0:cwj, s, 0:ncj], iF)
                    K.tt("dve", g["aff"].b(s)[0:cwj, s, 0:ncj], ilT.b(gi)[0:cwj, gi, 0:ncj, 2], ilT.b(gi)[0:cwj, gi, 0:ncj, 3], ALU.add)
                    K.tt("dve", g["aff"].b(s)[0:cwj, s, 0:ncj], g["aff"].b(s)[0:cwj, s, 0:ncj], ilT.b(gi)[0:cwj, gi, 0:ncj, 4], ALU.add)
                    for jc in range(ncj):
                        iv = g["idxi"].b(s)[0:cwj, s, jc:jc + 1]
                        Xg = g["Xg"]
                        K.dma("pool", Xg.b(jc)[0:cwj, jc, :], H2d.b(0)[:, :], extra_in=[iv],
                              fn=lambda E, jc=jc, iv=iv, Xg=Xg, cwj=cwj: E.indirect_dma_start(
                                  out=Xg.ap[0:cwj, jc, :], out_offset=None, in_=H2d.ap[:, :],
                                  in_offset=bass.IndirectOffsetOnAxis(ap=iv.ap, axis=0)))

            def sel_list(e):
                return [(gi, i) for gi, g in enumerate(groups) for i in range(g["ntk"])]

            def prep_xt(e):
                s = e % 2
                for g in groups:
                    cap, ncj, cwj = g["cap"], g["ncj"], g["cwj"]
                    for k in range(8):
                        tgt = (pXT[:, :], pY.b(0)[:, 0, :], pY.b(1)[:, 1, :])[k % 3]
                        for jc in range(ncj):
                            K.mm(tgt[:, jc * 128:jc * 128 + cwj], g["Xg"].b(jc)[0:cwj, jc, k * 128:(k + 1) * 128], identb[0:cwj, 0:cwj])
                        K.copy("act" if k % 2 == 0 else "dve", g["XT"].b(s)[:, s, k, 0:cap], tgt[:, 0:cap])

            def gate_up(e, pend):
                s = e % 2
                per = (len(pend) + 7) // 8
                for fc in range(8):
                    batch = pend[fc * per:(fc + 1) * per]
                    slots = []
                    for (gi, i) in batch:
                        slot = cnt["sel"] % NSEL
                        cnt["sel"] += 1
                        slots.append(slot)
                        sel_op(e + 1, gi, i, slot)
                    for gi, g in enumerate(groups):
                        cap = g["cap"]
                        for j in range(2):
                            po = pGU.b(j)[:, j, 0:cap] if gi == 0 else pC[:, j * 64:j * 64 + cap]
                            for k in range(8):
                                K.mm(po, wg.b(s * 2 + j)[:, s, j, k, fc * 128:(fc + 1) * 128], g["XT"].b(s)[:, s, k, 0:cap],
                                     start=(k == 0), stop=(k == 7))
                    for (gi, i), slot in zip(batch, slots):
                        idx_mm(e + 1, gi, i, slot)
                    for gi, g in enumerate(groups):
                        cap = g["cap"]
                        pg = pGU.b(0)[:, 0, 0:cap] if gi == 0 else pC[:, 0:cap]
                        pu = pGU.b(1)[:, 1, 0:cap] if gi == 0 else pC[:, 64:64 + cap]
                        K.act(sgl[:, 0:cap], pg, AF.Silu)
                        K.tt("dve", g["hid"].b(fc)[:, fc, 0:cap], sgl[:, 0:cap], pu, ALU.mult)

            def down(e):
                s = e % 2
                nsc = 0
                prevb = [V(None, b_) for b_ in scb[(e + 1) % 2]]
                for g in groups:
                    ncj, cwj = g["ncj"], g["cwj"]
                    g2b = MODv(g["r"], 5)
                    for jc in range(ncj):
                        ys = cnt["y"] % 2
                        cnt["y"] += 1
                        for hf_ in range(2):
                            for fc in range(8):
                                K.mm(pY.b(hf_)[0:cwj, hf_, :], g["hid"].b(fc)[:, fc, jc * 128:jc * 128 + cwj],
                                     wd[:, fc, hf_ * 512:(hf_ + 1) * 512], start=(fc == 0), stop=(fc == 7))
                            K.stt(ysb.b(ys)[0:cwj, ys, hf_ * 512:(hf_ + 1) * 512], pY.b(hf_)[0:cwj, hf_, :],
                                  g["aff"].b(s)[0:cwj, s, jc:jc + 1], g2b[0:cwj, hf_ * 512:(hf_ + 1) * 512], ALU.mult, ALU.mult)
                        iv = g["idxs"].b(s)[0:cwj, s, jc:jc + 1]
                        sv_ = V(stream_rows.ap, scb[e % 2][nsc])
                        nsc += 1
                        K.dma("pool", sv_, ysb.b(ys)[0:cwj, ys, :], extra_in=[iv] + prevb,
                              fn=lambda E, ys=ys, iv=iv, cwj=cwj: E.indirect_dma_start(
                                  out=stream_rows.ap, out_offset=bass.IndirectOffsetOnAxis(ap=iv.ap, axis=0),
                                  in_=ysb.ap[0:cwj, ys, :], in_offset=None, compute_op=ALU.add))

            for (gi, i) in sel_list(0):
                slot = cnt["sel"] % NSEL
                cnt["sel"] += 1
                sel_op(0, gi, i, slot)
                idx_mm(0, gi, i, slot)
            prep_fin(0)
            prep_xt(0)
            for e in range(NE):
                load_wd(e)
                if e + 1 < NE:
                    load_w(e + 1)
                gate_up(e, sel_list(e + 1) if e + 1 < NE else [])
                if e + 1 < NE:
                    prep_fin(e + 1)
                down(e)
                if e + 1 < NE:
                    prep_xt(e + 1)
            K.pop()

        glat = dict(row0=TC, ntk=TL // 128, cap=2 * TL // NE, r=0, srow0=(TC if need_ctx else 0))
        if need_ctx:
            gctx = dict(row0=0, ntk=TC // 128, cap=2 * TC // NE, r=1, srow0=0)
            moe([glat, gctx], S1[:, :])
        else:
            moe([glat], yout[:, :])
        if stage_done(f"moe{L}"):
            break

    K.barrier(["sp"])
    return nc


def _prep_inputs(inputs):
    f = lambda a: np.ascontiguousarray(np.asarray(a, dtype=np.float32))
    x, c, ctx, c_ctx = f(inputs["x"]), f(inputs["c"]), f(inputs["ctx"]), f(inputs["c_ctx"])
    cosT, sinT = host_rope()
    consts = host_consts()
    shared = dict(consts=consts, cosT=cosT, sinT=sinT)
    for n in ("mod_w", "mod_b", "norm1_g", "norm2_g", "w_in", "w_out", "att_q_norm_g", "att_k_norm_g", "att_subln_g",
              "ret_norm_g", "lru_conv_w", "lru_conv_b", "lru_gate_w", "lru_lambda", "lru_norm_g", "router_w",
              "exp_w_gate", "exp_w_up", "exp_w_down"):
        shared[n] = f(inputs[n])
    shared["att_lambda"] = f(inputs["att_lambda"]).reshape(DEPTH, 256)
    shared["ret_log_decay"] = f(inputs["ret_log_decay"]).reshape(DEPTH, 8)
    shared["lru_gate_b"] = f(inputs["lru_gate_b"]).reshape(DEPTH, 4, 256)
    maps = []
    for b in range(x.shape[0]):
        m = dict(shared)
        m["xin"] = np.ascontiguousarray(np.concatenate([ctx[b], x[b]], axis=0))
        m["cvec"] = np.ascontiguousarray(np.stack([c[b], c_ctx], axis=1))
        maps.append(m)
    return maps


_NC_CACHE = {}


def kernel(**inputs):
    maps = _prep_inputs(inputs)
    if "nc" not in _NC_CACHE:
        _NC_CACHE["nc"] = build()
    nc = _NC_CACHE["nc"]
    res = run_bass_kernel_spmd(nc, maps, core_ids=list(range(N_CORES)))
    return np.stack([np.asarray(r["y"], dtype=np.float32) for r in res.results], axis=0)
```
